# Optimizing a Trainium2 kernel written in Bass

```python
import math
import jax, jax.numpy as jnp
from jax import lax
import numpy as np

D_MODEL = 1024
BATCH = 16
SEQ = 4096
DEPTH = 1

ATTN_HEADS = 4
ATTN_HEAD_DIM = 64
ATTN_V_DIM = 2 * ATTN_HEAD_DIM
ATTN_WIDTH = ATTN_HEADS * ATTN_V_DIM
QK_COLS = ATTN_HEADS * 2 * ATTN_HEAD_DIM
Q_BLOCK = 128
CONV_CHANNELS = D_MODEL // 2
CONV_WIDTH = 31
N_BRANCHES = 2
IN_COLS = 2 * QK_COLS + ATTN_WIDTH + 2 * CONV_CHANNELS + N_BRANCHES * D_MODEL
N_GROUPS = 4
EXPERTS_PER_GROUP = 8
N_EXPERTS = N_GROUPS * EXPERTS_PER_GROUP
EXPERT_FF = D_MODEL // 2
TOP_K = 2
MOE_BLOCK = 128
PLE_DIM = 256
EPS = 1e-6

kernel_name = "hybrid_diffattn_conformer_hmoe_block"


def rmsnorm(x, g):
    x32 = x.astype(jnp.float32)
    y = x32 * lax.rsqrt(jnp.mean(x32 * x32, axis=-1, keepdims=True) + EPS)
    return (y * g.astype(jnp.float32)).astype(x.dtype)


def layernorm(x, g, b):
    x32 = x.astype(jnp.float32)
    mu = jnp.mean(x32, axis=-1, keepdims=True)
    var = jnp.mean(jnp.square(x32 - mu), axis=-1, keepdims=True)
    y = (x32 - mu) * lax.rsqrt(var + EPS)
    return (y * g.astype(jnp.float32) + b.astype(jnp.float32)).astype(x.dtype)


def diff_attention(q, k, v, lam):
    B, H, _, S, d = q.shape
    nqb = S // Q_BLOCK
    scale = 1.0 / math.sqrt(d)
    q_blocks = jnp.moveaxis(q.reshape(B, H, 2, nqb, Q_BLOCK, d), 3, 0)
    kpos = jnp.arange(S)

    def one_block(args):
        qb, bi = args
        qpos = bi * Q_BLOCK + jnp.arange(Q_BLOCK)
        mask = kpos[None, :] <= qpos[:, None]
        s = jnp.einsum('bhmqd,bhmkd->bhmqk', qb, k).astype(jnp.float32) * scale
        s = jnp.where(mask, s, -jnp.inf)
        probs = jax.nn.softmax(s, axis=-1)
        a = probs[:, :, 0] - lam * probs[:, :, 1]
        return jnp.einsum('bhqk,bhkv->bhqv', a.astype(v.dtype), v)

    out = lax.map(one_block, (q_blocks, jnp.arange(nqb)))
    return jnp.transpose(out, (1, 0, 3, 2, 4)).reshape(B, S, H, v.shape[-1])


def hierarchical_moe(t, w_rg, b_rg, w_re, b_re, w1, w3, w2):
    N, D = t.shape
    g_logits = (t @ w_rg + b_rg).astype(jnp.float32)
    g_probs = jax.nn.softmax(g_logits, axis=-1)
    g_idx = jnp.argmax(g_logits, axis=-1)
    g_gate = jnp.take_along_axis(g_probs, g_idx[:, None], axis=-1)
    e_logits = (t @ w_re + b_re).astype(jnp.float32).reshape(N, N_GROUPS, EXPERTS_PER_GROUP)
    e_logits = jnp.take_along_axis(e_logits, g_idx[:, None, None], axis=1)[:, 0]
    e_probs = jax.nn.softmax(e_logits, axis=-1)
    top_p, top_e = lax.top_k(e_probs, TOP_K)
    top_p = top_p / jnp.sum(top_p, axis=-1, keepdims=True)
    weights = g_gate * top_p
    expert_id = g_idx[:, None] * EXPERTS_PER_GROUP + top_e

    A = N * TOP_K
    flat_e = expert_id.reshape(-1)
    flat_t = jnp.repeat(jnp.arange(N), TOP_K)
    flat_w = weights.reshape(-1).astype(t.dtype)
    order = jnp.argsort(flat_e)
    sorted_e = flat_e[order]
    counts = jnp.bincount(flat_e, length=N_EXPERTS)
    padded = ((counts + MOE_BLOCK - 1) // MOE_BLOCK) * MOE_BLOCK
    start = jnp.cumsum(counts) - counts
    pend = jnp.cumsum(padded)
    pstart = pend - padded
    dest = pstart[sorted_e] + (jnp.arange(A) - start[sorted_e])
    R = ((A + MOE_BLOCK - 1) // MOE_BLOCK) * MOE_BLOCK + N_EXPERTS * MOE_BLOCK
    nb = R // MOE_BLOCK
    row_tok = jnp.full((R,), N, dtype=jnp.int32).at[dest].set(flat_t[order].astype(jnp.int32))
    row_w = jnp.zeros((R,), t.dtype).at[dest].set(flat_w[order])
    block_e = jnp.minimum(jnp.searchsorted(pend, jnp.arange(nb) * MOE_BLOCK, side='right'),
                          N_EXPERTS - 1)
    t_pad = jnp.concatenate([t, jnp.zeros((1, D), t.dtype)], axis=0)
    xs = t_pad[row_tok].reshape(nb, MOE_BLOCK, D)

    def expert_block(args):
        xb, e = args
        hdn = jax.nn.silu(xb @ w1[e]) * (xb @ w3[e])
        return hdn @ w2[e]

    ys = lax.map(expert_block, (xs, block_e)).reshape(R, D)
    out = jnp.zeros((N + 1, D), ys.dtype).at[row_tok].add(ys * row_w[:, None])
    return out[:N]


def setup_inputs(seed: int = 0) -> dict:
    key = jax.random.key(seed)
    ks = jax.random.split(key, 32)
    f32 = jnp.float32
    L, D, C, d = DEPTH, D_MODEL, CONV_CHANNELS, ATTN_HEAD_DIM

    def nrm(k, shape, scale):
        return jax.random.normal(k, shape, f32) * scale

    def gain(k, shape):
        return 1.0 + 0.02 * jax.random.normal(k, shape, f32)

    return {
        "x": nrm(ks[0], (BATCH, SEQ, D), 1.0),
        "p": nrm(ks[1], (L, BATCH, SEQ, PLE_DIM), 1.0),
        "norm_mix": gain(ks[2], (L, D)),
        "w_in": nrm(ks[3], (L, D, IN_COLS), D ** -0.5),
        "b_conv_in": nrm(ks[4], (L, 2 * C), 0.02),
        "b_gate": nrm(ks[5], (L, N_BRANCHES * D), 0.02),
        "q_norm": gain(ks[6], (L, 2, d)),
        "k_norm": gain(ks[7], (L, 2, d)),
        "lambda_q1": nrm(ks[8], (L, d), 0.1),
        "lambda_k1": nrm(ks[9], (L, d), 0.1),
        "lambda_q2": nrm(ks[10], (L, d), 0.1),
        "lambda_k2": nrm(ks[11], (L, d), 0.1),
        "subln": gain(ks[12], (L, ATTN_V_DIM)),
        "w_attn_out": nrm(ks[13], (L, ATTN_WIDTH, D), ATTN_WIDTH ** -0.5),
        "conv_w": nrm(ks[14], (L, CONV_WIDTH, C), CONV_WIDTH ** -0.5),
        "conv_b": nrm(ks[15], (L, C), 0.02),
        "conv_ln_g": gain(ks[16], (L, C)),
        "conv_ln_b": nrm(ks[17], (L, C), 0.02),
        "w_conv_out": nrm(ks[18], (L, C, D), C ** -0.5),
        "b_conv_out": nrm(ks[19], (L, D), 0.02),
        "w_o": nrm(ks[20], (L, D, D), D ** -0.5),
        "norm_ffn": gain(ks[21], (L, D)),
        "w_router_group": nrm(ks[22], (L, D, N_GROUPS), D ** -0.5),
        "b_router_group": nrm(ks[23], (L, N_GROUPS), 0.01),
        "w_router_expert": nrm(ks[24], (L, D, N_EXPERTS), D ** -0.5),
        "b_router_expert": nrm(ks[25], (L, N_EXPERTS), 0.01),
        "w1": nrm(ks[26], (L, N_EXPERTS, D, EXPERT_FF), D ** -0.5),
        "w3": nrm(ks[27], (L, N_EXPERTS, D, EXPERT_FF), D ** -0.5),
        "w2": nrm(ks[28], (L, N_EXPERTS, EXPERT_FF, D), EXPERT_FF ** -0.5),
        "norm_ple": gain(ks[29], (L, D)),
        "w_ple_gate": nrm(ks[30], (L, D, D), D ** -0.5),
        "b_ple_gate": nrm(ks[31], (L, D), 0.02),
        "w_ple_proj": nrm(jax.random.fold_in(key, 99), (L, PLE_DIM, D), PLE_DIM ** -0.5),
    }


def reference(x, p, norm_mix, w_in, b_conv_in, b_gate, q_norm, k_norm,
              lambda_q1, lambda_k1, lambda_q2, lambda_k2, subln, w_attn_out,
              conv_w, conv_b, conv_ln_g, conv_ln_b, w_conv_out, b_conv_out, w_o,
              norm_ffn, w_router_group, b_router_group, w_router_expert, b_router_expert,
              w1, w3, w2, norm_ple, w_ple_gate, b_ple_gate, w_ple_proj):
    B, S, D = x.shape
    H, d, C = ATTN_HEADS, ATTN_HEAD_DIM, CONV_CHANNELS
    h = x
    for i in range(DEPTH):
        lambda_init = 0.8 - 0.6 * math.exp(-0.3 * i)
        u = rmsnorm(h, norm_mix[i])
        proj = u @ w_in[i]
        q, k, v, c, g = jnp.split(
            proj, np.cumsum([QK_COLS, QK_COLS, ATTN_WIDTH, 2 * C]).tolist(), axis=-1)

        q = rmsnorm(q.reshape(B, S, H, 2, d), q_norm[i])
        k = rmsnorm(k.reshape(B, S, H, 2, d), k_norm[i])
        q = jnp.transpose(q, (0, 2, 3, 1, 4))
        k = jnp.transpose(k, (0, 2, 3, 1, 4))
        v = jnp.transpose(v.reshape(B, S, H, ATTN_V_DIM), (0, 2, 1, 3))
        lam = (jnp.exp(jnp.sum(lambda_q1[i].astype(jnp.float32) * lambda_k1[i].astype(jnp.float32)))
               - jnp.exp(jnp.sum(lambda_q2[i].astype(jnp.float32) * lambda_k2[i].astype(jnp.float32)))
               + lambda_init)
        attn = diff_attention(q, k, v, lam)
        attn = rmsnorm(attn, subln[i]) * (1.0 - lambda_init)
        y_a = attn.reshape(B, S, ATTN_WIDTH) @ w_attn_out[i]

        c = c + b_conv_in[i]
        c = c[..., :C] * jax.nn.sigmoid(c[..., C:])
        c = lax.conv_general_dilated(
            c, conv_w[i][:, None, :], window_strides=(1,), padding=[(CONV_WIDTH - 1, 0)],
            dimension_numbers=('NWC', 'WIO', 'NWC'), feature_group_count=C) + conv_b[i]
        c = jax.nn.silu(layernorm(c, conv_ln_g[i], conv_ln_b[i]))
        y_b = c @ w_conv_out[i] + b_conv_out[i]

        gates = jax.nn.sigmoid(g + b_gate[i])
        merged = gates[..., :D] * y_a + gates[..., D:] * y_b
        h = h + merged @ w_o[i]

        t = rmsnorm(h, norm_ffn[i]).reshape(B * S, D)
        moe = hierarchical_moe(t, w_router_group[i], b_router_group[i],
                               w_router_expert[i], b_router_expert[i], w1[i], w3[i], w2[i])
        h = h + moe.reshape(B, S, D)

        ple_gate = jax.nn.sigmoid(rmsnorm(h, norm_ple[i]) @ w_ple_gate[i] + b_ple_gate[i])
        h = h + ple_gate * (p[i] @ w_ple_proj[i])
    return h
```

```python
from contextlib import ExitStack

import numpy as np
import concourse.bass as bass
import concourse.mybir as mybir
from concourse.bass_utils import run_bass_kernel_spmd

F32 = mybir.dt.float32
BF16 = mybir.dt.bfloat16
I32 = mybir.dt.int32
AF = mybir.ActivationFunctionType
ALU = mybir.AluOpType
AX = mybir.AxisListType

ENGS = ("pe", "act", "dve", "pool", "sp")
NDMASEM = 8
EPS = 1e-6
STOP = [99]
LAMBDA_INIT = 0.2


class Op:
    __slots__ = ("eng", "fn", "deps", "isdma", "tok", "marked", "cum")

    def __init__(self, eng, fn, isdma):
        self.eng = eng
        self.fn = fn
        self.deps = []
        self.isdma = isdma
        self.tok = None
        self.marked = False
        self.cum = None


class Prog:
    SEM_LIMIT = 12000

    def __init__(self, nc, stack):
        self.nc = nc
        self.stack = stack
        self.nsem = 0
        self.sems = {e: self._newsem(e) for e in ENGS}
        self.finals = []
        self.dsems = {e: [stack.enter_context(nc.semaphore("d_%s%d" % (e, i)))
                          for i in range(NDMASEM)] for e in ("sp", "pool")}
        self.dcount = {e: 0 for e in self.dsems}
        self.base = {e: 0 for e in ENGS}
        self._reset()

    def _newsem(self, e):
        self.nsem += 1
        return self.stack.enter_context(self.nc.semaphore("s_%s_%d" % (e, self.nsem)))

    def _reset(self):
        self.ops = {e: [] for e in ENGS}
        self.lastw = {}
        self.readers = {}

    def _add(self, eng, fn, reads, writes, isdma):
        op = Op(eng, fn, isdma)
        deps = []
        for r in reads:
            w = self.lastw.get(r)
            if w is not None:
                deps.append(w)
        for w_ in writes:
            w = self.lastw.get(w_)
            if w is not None:
                deps.append(w)
            deps.extend(self.readers.get(w_, ()))
        op.deps = list(dict.fromkeys(deps))
        self.ops[eng].append(op)
        for r in reads:
            self.readers.setdefault(r, []).append(op)
        for w_ in writes:
            self.lastw[w_] = op
            self.readers[w_] = []
        if isdma:
            n = self.dcount[eng]
            self.dcount[eng] = n + 1
            sem = self.dsems[eng][n % NDMASEM]
            op.tok = (sem, 16 * (n // NDMASEM + 1))
            op.cum = (sem, 16 * (n // NDMASEM))
        return op

    def op(self, eng, fn, reads=(), writes=()):
        return self._add(eng, fn, tuple(reads), tuple(writes), False)

    def dma(self, eng, fn, reads=(), writes=()):
        return self._add(eng, fn, tuple(reads), tuple(writes), True)

    def flush(self):
        nc = self.nc
        ops = self.ops
        for e in ENGS:
            for op in ops[e]:
                for d in op.deps:
                    if not d.isdma and not (d.eng == "pe" and e == "pe"):
                        d.marked = True
            last = [o for o in ops[e] if not o.isdma]
            if last:
                last[-1].marked = True
        for e in ENGS:
            c = self.base[e]
            for op in ops[e]:
                if not op.isdma and op.marked:
                    if c >= self.SEM_LIMIT:
                        self.finals.append((self.sems[e], c))
                        self.sems[e] = self._newsem(e)
                        c = 0
                    c += 1
                    op.tok = (self.sems[e], c)
            self.base[e] = c
        prog = self

        def emit_engine(e, eng):
            waited = {}

            def wait(sem, val):
                k = id(sem)
                if val > 0 and waited.get(k, 0) < val:
                    waited[k] = val
                    eng.wait_ge(sem, val)

            for op in ops[e]:
                if op.isdma:
                    wait(*op.cum)
                for d in op.deps:
                    if d.eng == "pe" and e == "pe" and not d.isdma:
                        continue
                    wait(*d.tok)
                ins = op.fn(eng)
                if op.isdma:
                    ins.then_inc(op.tok[0], 16)
                elif op.marked:
                    ins.then_inc(op.tok[0], 1)
            for (fs, fv) in prog.finals:
                wait(fs, fv)
            for e2 in ENGS:
                wait(prog.sems[e2], prog.base[e2])
            for q in prog.dsems:
                n = prog.dcount[q]
                for i in range(NDMASEM):
                    k = (n - i + NDMASEM - 1) // NDMASEM if n > i else 0
                    wait(prog.dsems[q][i], 16 * k)

        with nc.Block() as block:
            @block.tensor
            def _(eng):
                emit_engine("pe", eng)

            @block.scalar
            def _(eng):
                emit_engine("act", eng)

            @block.vector
            def _(eng):
                emit_engine("dve", eng)

            @block.gpsimd
            def _(eng):
                emit_engine("pool", eng)

            @block.sync
            def _(eng):
                emit_engine("sp", eng)
        self._reset()


def bcast_rows(ap, nrows, ncols, off=0):
    return bass.AP(ap.tensor, off, [[0, nrows], [1, ncols]])


def build_program(S=4096, NB=2, CAP=1024, debug=(), phases=(1, 2, 3, 4)):
    nc = bass.Bass("TRN2", target_bir_lowering=False)
    NT = NB * S
    NTILE = NT // 128
    NG = NT // 512
    GPS = S // 512
    TPS = S // 128
    NE = 32
    NBLK = CAP // 128

    def din(name, shape, dt=F32):
        return nc.dram_tensor(name, shape, dt, kind="ExternalInput").ap()

    def dscr(name, shape, dt):
        kind = "ExternalOutput" if name in debug else "Internal"
        return nc.dram_tensor(name, shape, dt, kind=kind).ap()

    x = din("x", [NB, S, 1024])
    pin = din("p", [NB, S, 256])
    w_in = din("w_in", [1024, 4608])
    w_ao = din("w_attn_out", [512, 1024])
    w_co = din("w_conv_out", [512, 1024])
    w_o = din("w_o", [1024, 1024])
    w_rt = din("w_rt", [1024, 36])
    w1 = din("w1", [NE, 1024, 512])
    w3 = din("w3", [NE, 1024, 512])
    w2 = din("w2", [NE, 512, 1024])
    w_pg = din("w_ple_gate", [1024, 1024])
    w_pp = din("w_ple_proj", [256, 1024])
    consts = din("consts", [128, 4, 128])
    capoff = din("capoff", [1, 32])
    vec = {n: din(n, [1, l]) for n, l in [
        ("norm_mix", 1024), ("norm_ffn", 1024), ("norm_ple", 1024), ("b_ple_gate", 1024),
        ("b_rt", 36), ("lambda_q1", 64), ("lambda_k1", 64), ("lambda_q2", 64), ("lambda_k2", 64),
        ("q_norm", 128), ("k_norm", 128), ("subln", 128), ("b_conv_in", 1024), ("b_gate", 2048),
        ("conv_b", 512), ("conv_ln_g", 512), ("conv_ln_b", 512), ("b_conv_out", 1024)]}
    conv_w = din("conv_w", [512, 31])
    out = nc.dram_tensor("out", [NB, S, 1024], F32, kind="ExternalOutput").ap()

    uT_d = dscr("uT_d", [1024, NT], BF16)
    attnT_d = dscr("attnT_d", [512, NT], BF16)
    h1_d = dscr("h1_d", [NT, 1024], F32)
    xs_d = dscr("xs_d", [NE * CAP, 1024], BF16)
    ys_d = dscr("ys_d", [NE * CAP, 1024], BF16)
    rt_d = dscr("rt_d", [NT, 4], F32)

    def col_ap(v, n, off=0):
        return bass.AP(v.tensor, off, [[1, n], [1, 1]])

    with ExitStack() as top:
        P = Prog(nc, top)

        if 1 in phases:
          with ExitStack() as st:
            def sb(n, s, d):
                return st.enter_context(nc.sbuf_tensor(n, s, d))

            def ps(n, s, d):
                return st.enter_context(nc.psum_tensor(n, s, d))

            cst_f = sb("cst_f", [128, 4, 128], F32)
            cst = sb("cst", [128, 4, 128], BF16)
            wq = sb("wq", [128, 8, 1536], BF16)
            stage = sb("stage", [128, 2, 1536], F32)
            kT = sb("kT", [128, 4, S], BF16)
            Va = sb("Va", [128, TPS, 4, 129], BF16)
            xt = sb("xt", [128, 2, 1024], F32)
            gmix = sb("gmix", [128, 1024], F32)
            junk = sb("junk", [128, 1024], BF16)
            ub = sb("ub", [128, 2, 1024], BF16)
            uT = sb("uT", [128, 1, 8, 512], BF16)
            qT = sb("qT", [128, 2, 4, 512], BF16)
            sq = sb("sq", [128, 2, 512], BF16)
            lnb = sb("lnb", [128, 2, 512], F32)
            rsb = sb("rsb", [128, 2, 512], F32)
            pT = sb("pT", [128, 4, 512], BF16)
            atm = sb("atm", [128, 4, 512], BF16)
            attnT = sb("attnT", [128, 1, 4, 512], BF16)
            o0n = sb("o0n", [128, 2, 128], F32)
            Ocp = sb("Ocp", [128, 2, 4, 258], F32)
            dd = sb("dd", [128, 2, 128], F32)
            junk2 = sb("junk2", [128, 128], BF16)
            st1 = sb("st1", [128, 64], F32)
            gqk = sb("gqk", [128, 2], F32)
            lamv = sb("lamv", [128, 4, 64], F32)
            lams = sb("lams", [128, 8], F32)

            O_ps = ps("O_ps", [128, 4, 512], F32)
            T_ps = ps("T_ps", [128, 1024], BF16)
            R_ps = ps("R_ps", [128, 3, 512], F32)
            rrot = [0]

            def rbank():
                i = rrot[0] % 3
                rrot[0] += 1
                return i

            ident = cst[:, 0, :]
            tri = cst[:, 1, :]
            bones = cst[:, 3, :]

            P.dma("sp", lambda e: e.dma_start(out=cst_f[:], in_=consts[:, :, :]), writes=["cst_f"])
            P.op("dve", lambda e: e.tensor_copy(out=cst[:], in_=cst_f[:]), reads=["cst_f"], writes=["cst"])
            P.dma("sp", lambda e: e.dma_start(out=gmix[:], in_=bcast_rows(vec["norm_mix"], 128, 1024)), writes=["gmix"])
            P.dma("sp", lambda e: e.dma_start(out=gqk[:, 0:1], in_=col_ap(vec["q_norm"], 128)), writes=["gqk0"])
            P.dma("sp", lambda e: e.dma_start(out=gqk[:, 1:2], in_=col_ap(vec["k_norm"], 128)), writes=["gqk1"])
            P.op("dve", lambda e: e.tensor_scalar(out=gqk[:, 0:1], in0=gqk[:, 0:1], scalar1=0.125, scalar2=None, op0=ALU.mult),
                 reads=["gqk0"], writes=["gqk0"])
            for i, n in enumerate(["lambda_q1", "lambda_k1", "lambda_q2", "lambda_k2"]):
                P.dma("sp", lambda e, i=i, n=n: e.dma_start(out=lamv[:, i, :], in_=bcast_rows(vec[n], 128, 64)), writes=[("lamv", i)])
            for i in range(2):
                P.op("dve", lambda e, i=i: e.tensor_tensor(
                    out=lamv[:, 2 * i, :], in0=lamv[:, 2 * i, :], in1=lamv[:, 2 * i + 1, :], op=ALU.mult),
                    reads=[("lamv", 2 * i), ("lamv", 2 * i + 1)], writes=[("lamv", 2 * i)])
                P.op("dve", lambda e, i=i: e.tensor_reduce(out=lams[:, i:i + 1], in_=lamv[:, 2 * i, :], axis=AX.X, op=ALU.add),
                     reads=[("lamv", 2 * i)], writes=[("lams", i)])
                P.op("act", lambda e, i=i: e.activation(out=lams[:, 2 + i:3 + i], in_=lams[:, i:i + 1], func=AF.Exp),
                     reads=[("lams", i)], writes=[("lams", 2 + i)])
            P.op("dve", lambda e: e.tensor_tensor(out=lams[:, 4:5], in0=lams[:, 3:4], in1=lams[:, 2:3], op=ALU.subtract),
                 reads=[("lams", 2), ("lams", 3)], writes=[("lams", 4)])
            P.op("dve", lambda e: e.tensor_scalar(out=lams[:, 5:6], in0=lams[:, 4:5], scalar1=-LAMBDA_INIT, scalar2=None, op0=ALU.add),
                 reads=[("lams", 4)], writes=["nlam"])
            nlam = lams[:, 5:6]
            P.op("pool", lambda e: e.memset(Va[:, :, :, 128:129], 1.0), writes=["Va_ones"])
            P.op("pool", lambda e: e.memset(qT[:], 0.0), writes=[("qT", c, m) for c in range(4) for m in range(2)])
            zt = sb("zt", [128, 2, 1024], BF16)
            P.op("pool", lambda e: e.memset(zt[:], 0.0), writes=["zt"])
            xs_v = xs_d.rearrange("(n p) c -> p n c", p=128)
            for i in range(NE * CAP // 256):
                P.dma("pool", lambda e, i=i: e.dma_start(out=xs_v[:, i * 2:(i + 1) * 2, :], in_=zt[:]), reads=["zt"], writes=[("xs_z", i)])
            for k in range(8):
                sl = k % 2
                P.dma("sp", lambda e, k=k, sl=sl: e.dma_start(out=stage[:, sl, :], in_=w_in[k * 128:(k + 1) * 128, 0:1536]),
                      writes=[("stage", sl)])
                P.op("pool", lambda e, k=k, sl=sl: e.tensor_copy(out=wq[:, k, :], in_=stage[:, sl, :]),
                     reads=[("stage", sl)], writes=[("wq", k)])
            WQ = [("wq", k) for k in range(8)]

            stc = [0]

            def stat():
                i = stc[0] % 64
                stc[0] += 1
                return st1[:, i:i + 1], ("st1", i)

            tilec = [0]
            deferred = []
            for gi in range(NG):
                b = gi // GPS
                G = gi % GPS
                s0 = G * 512
                us = 0
                for j in range(4):
                    tc_ = tilec[0]
                    tilec[0] += 1
                    xs = tc_ % 2
                    P.dma("sp", lambda e, xs=xs, b=b, r0=s0 + j * 128: e.dma_start(out=xt[:, xs, :], in_=x[b, r0:r0 + 128, :]),
                          writes=[("xt", xs)])
                    ssq, kssq = stat()
                    P.op("act", lambda e, xs=xs, ssq=ssq: e.activation(
                        out=junk[:], in_=xt[:, xs, :], func=AF.Square, accum_out=ssq), reads=[("xt", xs)], writes=["junk", kssq])
                    lnv, klnv = stat()
                    P.op("act", lambda e, ssq=ssq, lnv=lnv: e.activation(out=lnv, in_=ssq, func=AF.Ln, scale=1.0 / 1024, bias=EPS),
                         reads=[kssq], writes=[klnv])
                    rstd, krstd = stat()
                    P.op("act", lambda e, lnv=lnv, rstd=rstd: e.activation(out=rstd, in_=lnv, func=AF.Exp, scale=-0.5),
                         reads=[klnv], writes=[krstd])
                    P.op("dve", lambda e, xs=xs, rstd=rstd: e.scalar_tensor_tensor(
                        out=ub[:, xs, :], in0=xt[:, xs, :], scalar=rstd, in1=gmix[:], op0=ALU.mult, op1=ALU.mult),
                        reads=[("xt", xs), krstd, "gmix"], writes=[("ub", xs)])
                    for k in range(8):
                        P.op("pe", lambda e, xs=xs, k=k: e.transpose(out=T_ps[:, k * 128:(k + 1) * 128], in_=ub[:, xs, k * 128:(k + 1) * 128], identity=ident),
                             reads=[("ub", xs), "cst"], writes=["T_ps"])
                    P.op("dve", lambda e, us=us, j=j: e.tensor_copy(
                        out=uT[:, us, :, j * 128:(j + 1) * 128], in_=T_ps[:].rearrange("p (k t) -> p k t", k=8)),
                        reads=["T_ps"], writes=[("uT", us, j)])
                UT = [("uT", us, j) for j in range(4)]
                P.dma("sp", lambda e, us=us, c0=gi * 512: e.dma_start(
                    out=uT_d.rearrange("(k p) t -> p k t", p=128)[:, :, c0:c0 + 512], in_=uT[:, us, :, :]), reads=UT)

                if STOP[0] <= 1:
                    continue
                def qk_proj(c):
                    rb = c % 2
                    for k in range(8):
                        P.op("pe", lambda e, c=c, k=k, rb=rb: e.matmul(
                            R_ps[:, rb, :], lhsT=wq[:, k, c * 128:(c + 1) * 128], rhs=uT[:, us, k, :], start=(k == 0), stop=(k == 7)),
                            reads=UT + [("wq", k)], writes=[("R", rb)])
                qk_proj(0)
                for c in range(8):
                    rb = c % 2
                    s2 = c % 2
                    P.op("act", lambda e, rb=rb, s2=s2: e.activation(out=sq[:, s2, :], in_=R_ps[:, rb, :], func=AF.Square),
                         reads=[("R", rb)], writes=[("sq", s2)])
                    rb2 = 2
                    P.op("pe", lambda e, rb2=rb2, s2=s2: e.matmul(R_ps[:, rb2, :], lhsT=bones, rhs=sq[:, s2, :], start=True, stop=True),
                         reads=[("sq", s2), "cst"], writes=[("R", rb2)])
                    P.op("act", lambda e, rb2=rb2, s2=s2: e.activation(out=lnb[:, s2, :], in_=R_ps[:, rb2, :], func=AF.Ln, bias=EPS),
                         reads=[("R", rb2)], writes=[("lnb", s2)])
                    P.op("act", lambda e, s2=s2: e.activation(out=rsb[:, s2, :], in_=lnb[:, s2, :], func=AF.Exp, scale=-0.5),
                         reads=[("lnb", s2)], writes=[("rsb", s2)])
                    if c < 4:
                        for m in range(2):
                            pr = slice(m * 64, (m + 1) * 64)
                            P.op("dve", lambda e, rb=rb, s2=s2, m=m, pr=pr, c=c: e.scalar_tensor_tensor(
                                out=qT[pr, m, c, :], in0=R_ps[pr, rb, :], scalar=gqk[pr, 0:1], in1=rsb[pr, s2, :], op0=ALU.mult, op1=ALU.mult),
                                reads=[("R", rb), ("rsb", s2), "gqk0"], writes=[("qT", c, m)])
                    else:
                        P.op("dve", lambda e, rb=rb, s2=s2, c=c, s0=s0: e.scalar_tensor_tensor(
                            out=kT[:, c - 4, s0:s0 + 512], in0=R_ps[:, rb, :], scalar=gqk[:, 1:2], in1=rsb[:, s2, :], op0=ALU.mult, op1=ALU.mult),
                            reads=[("R", rb), ("rsb", s2), "gqk1"], writes=[("kT", c - 4, G)])
                    if c + 1 < 8:
                        qk_proj(c + 1)
                rrot[0] = 0
                if STOP[0] <= 2:
                    continue
                for j in range(4):
                    rb = rbank()
                    for k in range(8):
                        P.op("pe", lambda e, j=j, k=k, rb=rb: e.matmul(
                            R_ps[:, rb, :], lhsT=uT[:, us, k, j * 128:(j + 1) * 128], rhs=wq[:, k, 1024:1536], start=(k == 0), stop=(k == 7)),
                            reads=UT + [("wq", k)], writes=[("R", rb)])
                    P.op("dve", lambda e, j=j, rb=rb, tl=G * 4 + j: e.tensor_copy(
                        out=Va[:, tl, :, 0:128], in_=R_ps[:, rb, :].rearrange("p (h v) -> p h v", h=4)),
                        reads=[("R", rb)], writes=[("Va", G * 4 + j)])

                if STOP[0] <= 3:
                    continue
                nkt = 4 * G + 4
                for h in range(4):
                    steps = [(m, j) for m in range(2) for j in range(nkt)]

                    def emit_qk(si, h=h):
                        m, j = steps[si]
                        r = j - 4 * G
                        q0 = max(r, 0) * 128
                        ncol = 512 - q0
                        rb = rbank()
                        P.op("pe", lambda e, m=m, j=j, q0=q0, ncol=ncol, rb=rb: e.matmul(
                            R_ps[:, rb, 0:ncol], lhsT=kT[:, h, j * 128:(j + 1) * 128],
                            rhs=qT[:, m, h, q0:512], start=True, stop=True),
                            reads=[("kT", h, j // 4), ("qT", h, m)], writes=[("R", rb)])
                        pslot = si % 4
                        P.op("act", lambda e, rb=rb, ncol=ncol, pslot=pslot: e.activation(
                            out=pT[:, pslot, 0:ncol], in_=R_ps[:, rb, 0:ncol], func=AF.Exp),
                            reads=[("R", rb)], writes=[("pT", pslot)])
                        if r >= 0:
                            P.op("pool", lambda e, pslot=pslot: e.tensor_tensor(
                                out=pT[:, pslot, 0:128], in0=pT[:, pslot, 0:128], in1=tri, op=ALU.mult),
                                reads=[("pT", pslot), "cst"], writes=[("pT", pslot)])
                        return (m, j, r, q0, pslot)

                    def emit_pv(info, h=h):
                        m, j, r, q0, pslot = info
                        for c in range(max(r, 0), 4):
                            bank = m * 2 + c // 2
                            off = (c % 2) * 129
                            first = (j == 0 and c % 2 == 0)
                            lastj = (j == 4 * G + c)
                            P.op("pe", lambda e, pslot=pslot, c=c, q0=q0, bank=bank, off=off, first=first, lastj=lastj, j=j: e.matmul(
                                O_ps[:, bank, off:off + 129], lhsT=pT[:, pslot, c * 128 - q0:c * 128 - q0 + 128],
                                rhs=Va[:, j, h, :], start=first, stop=lastj, skip_group_check=True),
                                reads=[("pT", pslot), ("Va", j), "Va_ones"], writes=[("O", m, c // 2)])

                    info = emit_qk(0)
                    for si in range(len(steps)):
                        nxt = emit_qk(si + 1) if si + 1 < len(steps) else None
                        emit_pv(info)
                        info = nxt
                        if deferred and si % 2 == 1:
                            deferred.pop(0)()
                    hp = h % 2
                    P.op("act", lambda e, hp=hp: e.activation(out=Ocp[:, hp, 0:2, :], in_=O_ps[:, 0:2, 0:258], func=AF.Copy),
                         reads=[("O", 0, 0), ("O", 0, 1)], writes=[("Ocp", hp, 0)])
                    P.op("dve", lambda e, hp=hp: e.tensor_copy(out=Ocp[:, hp, 2:4, :], in_=O_ps[:, 2:4, 0:258]),
                         reads=[("O", 1, 0), ("O", 1, 1)], writes=[("Ocp", hp, 1)])
                    def finish_c(c, h=h, hp=hp):
                        o0 = Ocp[:, hp, c // 2, (c % 2) * 129:(c % 2) * 129 + 129]
                        o1 = Ocp[:, hp, 2 + c // 2, (c % 2) * 129:(c % 2) * 129 + 129]
                        k0 = ("Ocp", hp, 0)
                        k1 = ("Ocp", hp, 1)
                        r0, kr0 = stat()
                        r1, kr1 = stat()
                        P.op("dve", lambda e, o0=o0, r0=r0: e.reciprocal(out=r0, in_=o0[:, 128:129]), reads=[k0], writes=[kr0])
                        P.op("dve", lambda e, o1=o1, r1=r1: e.reciprocal(out=r1, in_=o1[:, 128:129]), reads=[k1], writes=[kr1])
                        r1n, kr1n = stat()
                        P.op("dve", lambda e, r1=r1, r1n=r1n: e.tensor_tensor(out=r1n, in0=r1, in1=nlam, op=ALU.mult),
                             reads=[kr1, "nlam"], writes=[kr1n])
                        ds = c % 2
                        P.op("dve", lambda e, o0=o0, r0=r0, ds=ds: e.tensor_scalar(out=o0n[:, ds, :], in0=o0[:, 0:128], scalar1=r0, scalar2=None, op0=ALU.mult),
                             reads=[k0, kr0], writes=[("o0n", ds)])
                        P.op("dve", lambda e, o1=o1, r1n=r1n, ds=ds: e.scalar_tensor_tensor(
                            out=dd[:, ds, :], in0=o1[:, 0:128], scalar=r1n, in1=o0n[:, ds, :], op0=ALU.mult, op1=ALU.add),
                            reads=[k1, kr1n, ("o0n", ds)], writes=[("dd", ds)])
                        ss, kss = stat()
                        P.op("act", lambda e, ds=ds, ss=ss: e.activation(out=junk2[:], in_=dd[:, ds, :], func=AF.Square, accum_out=ss),
                             reads=[("dd", ds)], writes=["junk2", kss])
                        ln2, kln2 = stat()
                        P.op("act", lambda e, ss=ss, ln2=ln2: e.activation(out=ln2, in_=ss, func=AF.Ln, scale=1.0 / 128, bias=EPS),
                             reads=[kss], writes=[kln2])
                        rs2, krs2 = stat()
                        P.op("act", lambda e, ln2=ln2, rs2=rs2: e.activation(out=rs2, in_=ln2, func=AF.Exp, scale=-0.5),
                             reads=[kln2], writes=[krs2])
                        P.op("dve", lambda e, ds=ds, rs2=rs2, c=c, h=h: e.tensor_scalar(
                            out=atm[:, c, h * 128:(h + 1) * 128], in0=dd[:, ds, :], scalar1=rs2, scalar2=None, op0=ALU.mult),
                            reads=[("dd", ds), krs2], writes=[("atm", c, h)])
                    for c in range(4):
                        deferred.append(lambda c=c, f=finish_c: f(c))
                while deferred:
                    deferred.pop(0)()
                if STOP[0] <= 4:
                    continue
                for c in range(4):
                    for a in range(4):
                        P.op("pe", lambda e, c=c, a=a: e.transpose(out=T_ps[:, a * 128:(a + 1) * 128], in_=atm[:, c, a * 128:(a + 1) * 128], identity=ident),
                             reads=[("atm", c, a), "cst"], writes=["T_ps"])
                    P.op("dve", lambda e, us=us, c=c: e.tensor_copy(
                        out=attnT[:, us, :, c * 128:(c + 1) * 128], in_=T_ps[:, 0:512].rearrange("p (a t) -> p a t", a=4)),
                        reads=["T_ps"], writes=[("attnT", us, c)])
                P.dma("sp", lambda e, us=us, c0=gi * 512: e.dma_start(
                    out=attnT_d.rearrange("(a p) t -> p a t", p=128)[:, :, c0:c0 + 512], in_=attnT[:, us, :, :]),
                    reads=[("attnT", us, c) for c in range(4)])
            P.flush()


        if 2 in phases:
          with ExitStack() as st:
            def sb(n, s_, d):
                return st.enter_context(nc.sbuf_tensor(n, s_, d))

            def ps(n, s_, d):
                return st.enter_context(nc.psum_tensor(n, s_, d))

            cst_f = sb("cst_f2", [128, 4, 128], F32)
            cst = sb("cst2", [128, 4, 128], BF16)
            onesC = sb("onesC", [128, 128], F32)
            wcg = sb("wcg", [128, 8, 3072], BF16)
            wao = sb("wao", [128, 4, 1024], BF16)
            wco = sb("wco", [128, 4, 1024], BF16)
            wo = sb("wo", [128, 8, 1024], BF16)
            wrt = sb("wrt", [128, 8, 36], F32)
            uT = sb("uT2", [128, 8, 512], BF16)
            attnT = sb("attnT2", [128, 4, 512], BF16)
            xt = sb("xt2", [128, 4, 1024], F32)
            glu = sb("glu", [128, 4, 542], BF16)
            dgr = sb("dgr", [128, 16, 128], BF16)
            dgc = [0]
            cacc = sb("cacc", [128, 4, 512], F32)
            sqf = sb("sqf", [128, 2, 512], F32)
            mean_sb = sb("mean_sb", [128, 512], F32)
            tmp2 = sqf
            cT = sb("cT", [128, 4, 512], BF16)
            sg = sb("sg", [128, 2, 512], F32)
            sga = sb("sga", [128, 512], F32)
            sgbs = sb("sgbs", [128, 8, 512], BF16)
            t1s = sb("t1s", [128, 8, 512], BF16)
            t2 = sb("t2", [128, 512], F32)
            mT = sb("mT", [128, 8, 512], BF16)
            h1t = sb("h1t", [128, 2, 1024], F32)
            tn = sb("tn", [128, 1024], F32)
            tb = sb("tb", [128, 4, 1024], BF16)
            L4 = sb("L4", [128, 4, 36], F32)
            gmax4 = sb("gmax4", [128, 4], F32)
            ohg4 = sb("ohg4", [128, 4, 4], F32)
            ge4 = sb("ge4", [128, 4, 4], F32)
            gsum4 = sb("gsum4", [128, 4], F32)
            prod4 = sb("prod4", [128, 4, 4, 8], F32)
            els4 = sb("els4", [128, 4, 8], F32)
            top84 = sb("top84", [128, 4, 8], F32)
            m124 = sb("m124", [128, 2, 4, 8], F32)
            d214 = sb("d214", [128, 4], F32)
            den4 = sb("den4", [128, 4], F32)
            s124 = sb("s124", [128, 2, 4, 32], F32)
            sel4 = sb("sel4", [128, 4, 32], BF16)
            dest4 = sb("dest4", [128, 4, 32], F32)
            tmp324 = sb("tmp324", [128, 4, 32], F32)
            rinfo4 = sb("rinfo42", [128, 4, 4], F32)
            idx4 = sb("idx42", [128, 4, 2], I32)
            tT = sb("tT", [128, 8, 128], F32)
            gffn = sb("gffn", [128, 1024], F32)
            bci = sb("bci", [128, 8], F32)
            bg = sb("bg", [128, 16], F32)
            cvb = sb("cvb", [128, 4], F32)
            lng = sb("lng", [128, 4], F32)
            lnbb = sb("lnbb", [128, 4], F32)
            bco = sb("bco", [128, 8], F32)
            cw = sb("cw", [128, 4, 31], F32)
            sln = sb("sln", [128, 1], F32)
            brt = sb("brt", [128, 36], F32)
            base_bc = sb("base_bc", [128, 32], F32)
            ones_bf = sb("ones_bf", [128, 128], BF16)
            rs_ = sb("rs_", [128, 64], F32)

            R_ps = ps("R_ps2", [128, 6, 512], F32)
            TF_ps = ps("TF_ps", [128, 1024], F32)
            rrot = [0]

            def rbank():
                i = rrot[0] % 6
                rrot[0] += 1
                return i

            stc = [0]

            def stat():
                i = stc[0] % 56
                stc[0] += 1
                return rs_[:, i:i + 1], ("rs_", i)

            P.dma("sp", lambda e: e.dma_start(out=cst_f[:], in_=consts[:, :, :]), writes=["cst_f"])
            P.op("dve", lambda e: e.tensor_copy(out=cst[:], in_=cst_f[:]), reads=["cst_f"], writes=["cst"])
            P.op("pool", lambda e: e.memset(onesC[:], 1.0 / 512), writes=["onesC"])
            P.op("pool", lambda e: e.memset(ones_bf[:], 1.0), writes=["ones_bf"])
            P.dma("sp", lambda e: e.dma_start(out=gffn[:], in_=bcast_rows(vec["norm_ffn"], 128, 1024)), writes=["gffn"])
            P.dma("sp", lambda e: e.dma_start(out=brt[:], in_=bcast_rows(vec["b_rt"], 128, 36)), writes=["brt"])
            P.dma("sp", lambda e: e.dma_start(out=base_bc[:], in_=bcast_rows(capoff, 128, 32)), writes=["base_bc"])
            P.dma("sp", lambda e: e.dma_start(out=sln[:], in_=col_ap(vec["subln"], 128)), writes=["sln"])
            P.op("dve", lambda e: e.tensor_scalar(out=sln[:], in0=sln[:], scalar1=1.0 - LAMBDA_INIT, scalar2=None, op0=ALU.mult),
                 reads=["sln"], writes=["sln"])

            def colvec(dst, name, nchunk):
                P.dma("sp", lambda e: e.dma_start(out=dst[:, 0:nchunk], in_=bass.AP(vec[name].tensor, 0, [[1, 128], [128, nchunk]])),
                      writes=[name])
            with nc.allow_non_contiguous_dma(reason="tiny per-channel bias columns"):
                pass
            for dst, name, nch in [(bci, "b_conv_in", 8), (bg, "b_gate", 16), (cvb, "conv_b", 4), (lng, "conv_ln_g", 4),
                                   (lnbb, "conv_ln_b", 4), (bco, "b_conv_out", 8)]:
                for c in range(nch):
                    P.dma("sp", lambda e, dst=dst, name=name, c=c: e.dma_start(out=dst[:, c:c + 1], in_=col_ap(vec[name], 128, c * 128)),
                          writes=[(name, c)])
            BIAS = [(n, c) for _, n, nch in [(0, "b_conv_in", 8), (0, "b_gate", 16), (0, "conv_b", 4), (0, "conv_ln_g", 4),
                                             (0, "conv_ln_b", 4), (0, "b_conv_out", 8)] for c in range(nch)]
            for cc in range(4):
                for kk in range(31):
                    pass
            for cc in range(4):
                P.dma("sp", lambda e, cc=cc: e.dma_start(out=cw[:, cc, :], in_=conv_w[cc * 128:(cc + 1) * 128, :]),
                      writes=[("cw", cc)])
            P.dma("sp", lambda e: e.dma_start(out=wrt[:], in_=w_rt.rearrange("(k p) n -> p k n", p=128)), writes=["wrt"])
            wl = [0]

            def load_cast(dst_fn, src_fn, key, eng="pool", scale_col=None):
                sl = wl[0] % 4
                wl[0] += 1
                P.dma("sp", lambda e, sl=sl: e.dma_start(out=xt[:, sl, :], in_=src_fn()), writes=[("xt", sl)])
                if scale_col is None:
                    P.op(eng, lambda e, sl=sl: e.tensor_copy(out=dst_fn(), in_=xt[:, sl, :]), reads=[("xt", sl)], writes=[key])
                else:
                    P.op("dve", lambda e, sl=sl: e.tensor_scalar(out=dst_fn(), in0=xt[:, sl, :], scalar1=scale_col, scalar2=None, op0=ALU.mult),
                         reads=[("xt", sl), "sln"], writes=[key])
            for k in range(8):
                for c3 in range(3):
                    load_cast(lambda k=k, c3=c3: wcg[:, k, c3 * 1024:(c3 + 1) * 1024],
                              lambda k=k, c3=c3: w_in[k * 128:(k + 1) * 128, 1536 + c3 * 1024:1536 + (c3 + 1) * 1024],
                              ("wcg", k, c3), eng="pool" if (k + c3) % 2 else "dve")
                load_cast(lambda k=k: wo[:, k, :], lambda k=k: w_o[k * 128:(k + 1) * 128, :], ("wo", k), eng="pool")
            for a in range(4):
                load_cast(lambda a=a: wao[:, a, :], lambda a=a: w_ao[a * 128:(a + 1) * 128, :], ("wao", a), scale_col=sln[:, 0:1])
                load_cast(lambda a=a: wco[:, a, :], lambda a=a: w_co[a * 128:(a + 1) * 128, :], ("wco", a), eng="pool")
            WCG = [("wcg", k, c3) for k in range(8) for c3 in range(3)]

            pa_ = [0]
            pb_ = [0]

            def rbankA():
                i = pa_[0] % 3
                pa_[0] += 1
                return i

            def rbankB():
                i = 3 + pb_[0] % 3
                pb_[0] += 1
                return i

            def zip_run2(ga, gb, ratio=1):
                a_done = ga is None
                b_done = gb is None
                while not (a_done and b_done):
                    if not a_done:
                        try:
                            next(ga)
                        except StopIteration:
                            a_done = True
                    for _ in range(ratio):
                        if not b_done:
                            try:
                                next(gb)
                            except StopIteration:
                                b_done = True

            def p2_front_a(gi):
                b = gi // GPS
                G = gi % GPS
                s0 = G * 512
                c0 = gi * 512
                for cc in range(4):
                    ra = rbankA()
                    for k in range(8):
                        yield P.op("pe", lambda e, cc=cc, k=k, ra=ra: e.matmul(R_ps[:, ra, :], lhsT=wcg[:, k, cc * 128:(cc + 1) * 128], rhs=uT[:, k, :],
                                                                       start=(k == 0), stop=(k == 7)), reads=["uT"] + WCG, writes=[("R", ra)])
                    rg = rbankA()
                    for k in range(8):
                        yield P.op("pe", lambda e, cc=cc, k=k, rg=rg: e.matmul(R_ps[:, rg, :], lhsT=wcg[:, k, 512 + cc * 128:512 + (cc + 1) * 128], rhs=uT[:, k, :],
                                                                       start=(k == 0), stop=(k == 7)), reads=["uT"] + WCG, writes=[("R", rg)])
                    s2 = cc % 2
                    yield P.op("act", lambda e, rg=rg, s2=s2, cc=cc: e.activation(out=sg[:, s2, :], in_=R_ps[:, rg, :], func=AF.Sigmoid, bias=bci[:, 4 + cc:5 + cc]),
                         reads=[("R", rg), ("b_conv_in", 4 + cc)], writes=[("sg", s2)])
                    if G == 0:
                        yield P.op("pool", lambda e, cc=cc: e.memset(glu[:, cc, 0:30], 0.0), writes=[("glu", cc)])
                    yield P.op("dve", lambda e, ra=ra, s2=s2, cc=cc: e.scalar_tensor_tensor(
                        out=glu[:, cc, 30:542], in0=R_ps[:, ra, :], scalar=bci[:, cc:cc + 1], in1=sg[:, s2, :], op0=ALU.add, op1=ALU.mult),
                        reads=[("R", ra), ("sg", s2), ("b_conv_in", cc), ("glu", cc)], writes=[("glu", cc)])

            def p2_front_b(gi):
                b = gi // GPS
                G = gi % GPS
                s0 = G * 512
                c0 = gi * 512
                def gate_front(oc):
                    rya = rbankA()
                    for a in range(4):
                        yield P.op("pe", lambda e, a=a, oc=oc, rya=rya: e.matmul(R_ps[:, rya, :], lhsT=wao[:, a, oc * 128:(oc + 1) * 128], rhs=attnT[:, a, :],
                                                                         start=(a == 0), stop=(a == 3)), reads=["attnT", ("wao", a)], writes=[("R", rya)])
                    rga = rbankA()
                    for k in range(8):
                        yield P.op("pe", lambda e, k=k, oc=oc, rga=rga: e.matmul(R_ps[:, rga, :], lhsT=wcg[:, k, 1024 + oc * 128:1024 + (oc + 1) * 128], rhs=uT[:, k, :],
                                                                         start=(k == 0), stop=(k == 7)), reads=["uT"] + WCG, writes=[("R", rga)])
                    rgb = rbankA()
                    for k in range(8):
                        yield P.op("pe", lambda e, k=k, oc=oc, rgb=rgb: e.matmul(R_ps[:, rgb, :], lhsT=wcg[:, k, 2048 + oc * 128:2048 + (oc + 1) * 128], rhs=uT[:, k, :],
                                                                         start=(k == 0), stop=(k == 7)), reads=["uT"] + WCG, writes=[("R", rgb)])
                    yield P.op("act", lambda e, oc=oc, rga=rga: e.activation(out=sga[:], in_=R_ps[:, rga, :], func=AF.Sigmoid, bias=bg[:, oc:oc + 1]),
                         reads=[("R", rga), ("b_gate", oc)], writes=["sga"])
                    yield P.op("act", lambda e, oc=oc, rgb=rgb: e.activation(out=sgbs[:, oc, :], in_=R_ps[:, rgb, :], func=AF.Sigmoid, bias=bg[:, 8 + oc:9 + oc]),
                         reads=[("R", rgb), ("b_gate", 8 + oc)], writes=[("sgbs", oc)])
                    yield P.op("dve", lambda e, rya=rya, oc=oc: e.tensor_tensor(out=t1s[:, oc, :], in0=R_ps[:, rya, :], in1=sga[:], op=ALU.mult),
                         reads=[("R", rya), "sga"], writes=[("t1s", oc)])

                nfront = 0
                for cc in range(4):
                    rcv = rbankA()
                    for kk in range(31):
                        ds_ = dgc[0] % 16
                        dgc[0] += 1
                        yield P.op("act", lambda e, ds_=ds_, cc=cc, kk=kk: e.activation(out=dgr[:, ds_, :], in_=cst[:, 0, :], func=AF.Copy, scale=cw[:, cc, kk:kk + 1]),
                             reads=["cst", ("cw", cc)], writes=[("dgr", ds_)])
                        yield P.op("pe", lambda e, ds_=ds_, cc=cc, kk=kk, rcv=rcv: e.matmul(R_ps[:, rcv, :], lhsT=dgr[:, ds_, :], rhs=glu[:, cc, kk:kk + 512],
                                                                                   start=(kk == 0), stop=(kk == 30)),
                             reads=[("dgr", ds_), ("glu", cc)], writes=[("R", rcv)])
                    yield P.op("dve", lambda e, cc=cc, rcv=rcv: e.tensor_scalar(out=cacc[:, cc, :], in0=R_ps[:, rcv, :], scalar1=cvb[:, cc:cc + 1], scalar2=None, op0=ALU.add),
                         reads=[("R", rcv), ("conv_b", cc)], writes=[("cacc", cc)])
                    for _ in range(2):
                        yield from gate_front(nfront)
                        nfront += 1
                if gi + 1 < NG:
                    c1 = (gi + 1) * 512
                    yield P.dma("sp", lambda e, c1=c1: e.dma_start(out=uT[:], in_=uT_d.rearrange("(k p) t -> p k t", p=128)[:, :, c1:c1 + 512]),
                          writes=["uT"])
                    yield P.dma("sp", lambda e, c1=c1: e.dma_start(out=attnT[:], in_=attnT_d.rearrange("(a p) t -> p a t", p=128)[:, :, c1:c1 + 512]),
                          writes=["attnT"])
                for cc in range(4):
                    yield P.op("pool", lambda e, cc=cc: e.tensor_copy(out=glu[:, cc, 0:30], in_=glu[:, cc, 512:542]),
                         reads=[("glu", cc)], writes=[("glu", cc)])

            def p2_mid(gi):
                b = gi // GPS
                G = gi % GPS
                s0 = G * 512
                c0 = gi * 512
                rm = rbankB()
                rv = rbankB()
                for cc in range(4):
                    yield P.op("pe", lambda e, cc=cc, rm=rm: e.matmul(R_ps[:, rm, :], lhsT=onesC[:], rhs=cacc[:, cc, :], start=(cc == 0), stop=(cc == 3)),
                         reads=[("cacc", cc), "onesC"], writes=[("R", rm)])
                for cc in range(4):
                    s2 = cc % 2
                    yield P.op("act", lambda e, cc=cc, s2=s2: e.activation(out=sqf[:, s2, :], in_=cacc[:, cc, :], func=AF.Square),
                         reads=[("cacc", cc)], writes=[("sqf", s2)])
                    yield P.op("pe", lambda e, cc=cc, rv=rv, s2=s2: e.matmul(R_ps[:, rv, :], lhsT=onesC[:], rhs=sqf[:, s2, :], start=(cc == 0), stop=(cc == 3)),
                         reads=[("sqf", s2), "onesC"], writes=[("R", rv)])
                yield P.op("act", lambda e, rm=rm: e.activation(out=mean_sb[:], in_=R_ps[:, rm, :], func=AF.Copy), reads=[("R", rm)], writes=["mean_sb"])
                yield P.op("dve", lambda e: e.tensor_tensor(out=t2[:], in0=mean_sb[:], in1=mean_sb[:], op=ALU.mult), reads=["mean_sb"], writes=["t2"])
                yield P.op("dve", lambda e, rv=rv: e.tensor_tensor(out=t2[:], in0=R_ps[:, rv, :], in1=t2[:], op=ALU.subtract),
                     reads=[("R", rv), "t2"], writes=["t2"])
                yield P.op("act", lambda e: e.activation(out=t2[:], in_=t2[:], func=AF.Ln, bias=EPS), reads=["t2"], writes=["t2"])
                yield P.op("act", lambda e: e.activation(out=sga[:], in_=t2[:], func=AF.Exp, scale=-0.5), reads=["t2"], writes=["sga"])
                for cc in range(4):
                    s2 = cc % 2
                    yield P.op("dve", lambda e, cc=cc, s2=s2: e.tensor_tensor(out=tmp2[:, s2, :], in0=cacc[:, cc, :], in1=mean_sb[:], op=ALU.subtract),
                         reads=[("cacc", cc), "mean_sb"], writes=[("sqf", s2)])
                    yield P.op("dve", lambda e, s2=s2: e.tensor_tensor(out=tmp2[:, s2, :], in0=tmp2[:, s2, :], in1=sga[:], op=ALU.mult),
                         reads=[("sqf", s2), "sga"], writes=[("sqf", s2)])
                    yield P.op("act", lambda e, cc=cc, s2=s2: e.activation(out=cT[:, cc, :], in_=tmp2[:, s2, :], func=AF.Silu,
                                                                     scale=lng[:, cc:cc + 1], bias=lnbb[:, cc:cc + 1]),
                         reads=[("sqf", s2), ("conv_ln_g", cc), ("conv_ln_b", cc)], writes=[("cT", cc)])
                for oc in range(8):
                    ryb = rbankB()
                    for a in range(4):
                        yield P.op("pe", lambda e, a=a, oc=oc, ryb=ryb: e.matmul(R_ps[:, ryb, :], lhsT=wco[:, a, oc * 128:(oc + 1) * 128], rhs=cT[:, a, :],
                                                                         start=(a == 0), stop=(a == 3)), reads=[("cT", a), ("wco", a)], writes=[("R", ryb)])
                    yield P.op("dve", lambda e, ryb=ryb, oc=oc: e.scalar_tensor_tensor(out=t2[:], in0=R_ps[:, ryb, :], scalar=bco[:, oc:oc + 1], in1=sgbs[:, oc, :],
                                                                                op0=ALU.add, op1=ALU.mult),
                         reads=[("R", ryb), ("sgbs", oc), ("b_conv_out", oc)], writes=["t2"])
                    yield P.op("dve", lambda e, oc=oc: e.tensor_tensor(out=mT[:, oc, :], in0=t1s[:, oc, :], in1=t2[:], op=ALU.add),
                         reads=[("t1s", oc), "t2"], writes=[("mT", oc)])
                MT = [("mT", oc) for oc in range(8)]

            def p2_tail(gi):
                b = gi // GPS
                G = gi % GPS
                s0 = G * 512
                c0 = gi * 512
                MT = [("mT", oc) for oc in range(8)]
                for j in range(4):
                    tile_i = gi * 4 + j
                    for n in range(2):
                        rh = rbankB()
                        for oc in range(8):
                            yield P.op("pe", lambda e, j=j, n=n, oc=oc, rh=rh: e.matmul(R_ps[:, rh, :], lhsT=mT[:, oc, j * 128:(j + 1) * 128], rhs=wo[:, oc, n * 512:(n + 1) * 512],
                                                                                 start=(oc == 0), stop=(oc == 7)), reads=MT + [("wo", oc)], writes=[("R", rh)])
                        yield P.op("dve", lambda e, j=j, n=n, rh=rh: e.tensor_tensor(out=h1t[:, j % 2, n * 512:(n + 1) * 512], in0=R_ps[:, rh, :], in1=xt[:, j, n * 512:(n + 1) * 512], op=ALU.add),
                             reads=[("R", rh), ("xt", j)], writes=[("h1t", j % 2, n)])
                    yield P.dma("sp", lambda e, r0=tile_i * 128, j=j: e.dma_start(out=h1_d[r0:r0 + 128, :], in_=h1t[:, j % 2, :]), reads=[("h1t", j % 2, 0), ("h1t", j % 2, 1)])
                    ssq, kssq = stat()
                    yield P.op("act", lambda e, ssq=ssq, j=j: e.activation(out=tb[:, j, :], in_=h1t[:, j % 2, :], func=AF.Square, accum_out=ssq),
                         reads=[("h1t", j % 2, 0), ("h1t", j % 2, 1)], writes=[("tb", j), kssq])
                    lnv, klnv = stat()
                    yield P.op("act", lambda e, ssq=ssq, lnv=lnv: e.activation(out=lnv, in_=ssq, func=AF.Ln, scale=1.0 / 1024, bias=EPS), reads=[kssq], writes=[klnv])
                    rstd, krstd = stat()
                    yield P.op("act", lambda e, lnv=lnv, rstd=rstd: e.activation(out=rstd, in_=lnv, func=AF.Exp, scale=-0.5), reads=[klnv], writes=[krstd])
                    yield P.op("dve", lambda e, rstd=rstd, j=j: e.scalar_tensor_tensor(out=tn[:], in0=h1t[:, j % 2, :], scalar=rstd, in1=gffn[:], op0=ALU.mult, op1=ALU.mult),
                         reads=[("h1t", j % 2, 0), ("h1t", j % 2, 1), krstd, "gffn"], writes=["tn"])
                    yield P.op("act", lambda e, j=j: e.activation(out=tb[:, j, :], in_=tn[:], func=AF.Copy), reads=["tn"], writes=[("tb", j)])
                    for k in range(8):
                        yield P.op("pe", lambda e, k=k: e.transpose(out=TF_ps[:, k * 128:(k + 1) * 128], in_=tn[:, k * 128:(k + 1) * 128], identity=cst_f[:, 0, :]),
                             reads=["tn", "cst_f"], writes=["TF_ps"])
                    yield P.op("act", lambda e: e.activation(out=tT[:], in_=TF_ps[:].rearrange("p (k t) -> p k t", k=8), func=AF.Copy), reads=["TF_ps"], writes=["tT"])
                    rl = rbankB()
                    for k in range(8):
                        yield P.op("pe", lambda e, k=k, rl=rl: e.matmul(R_ps[:, rl, 0:36], lhsT=tT[:, k, :], rhs=wrt[:, k, :], start=(k == 0), stop=(k == 7)),
                             reads=["tT", "wrt"], writes=[("R", rl)])
                    yield P.op("dve", lambda e, rl=rl, j=j: e.tensor_tensor(out=L4[:, j, :], in0=R_ps[:, rl, 0:36], in1=brt[:], op=ALU.add), reads=[("R", rl), "brt"], writes=[("L4", j)])

            def p2_route(gi):
                b = gi // GPS
                G = gi % GPS
                s0 = G * 512
                c0 = gi * 512
                KL = [("L4", j) for j in range(4)]
                Lg4 = L4[:, :, 0:4]
                Lx = L4[:, :, 4:36].rearrange("p t (g j) -> p t g j", g=4)

                def bc_last(ap2, n):
                    return bass.AP(ap2.tensor, ap2.offset, [list(ap2.ap[0]), list(ap2.ap[1]), [0, n]])

                def col(buf3, c):
                    a = buf3[:, :, c:c + 1]
                    return bass.AP(a.tensor, a.offset, [list(a.ap[0]), list(a.ap[1])])
                P.op("dve", lambda e: e.tensor_reduce(out=gmax4[:], in_=Lg4, axis=AX.X, op=ALU.max), reads=KL, writes=["gmax4"])
                P.op("dve", lambda e: e.tensor_tensor(out=ohg4[:], in0=Lg4, in1=bc_last(gmax4[:], 4), op=ALU.is_equal), reads=KL + ["gmax4"], writes=["ohg4"])
                P.op("dve", lambda e: e.tensor_tensor(out=ge4[:], in0=Lg4, in1=bc_last(gmax4[:], 4), op=ALU.subtract), reads=KL + ["gmax4"], writes=["ge4"])
                P.op("act", lambda e: e.activation(out=ge4[:], in_=ge4[:], func=AF.Exp), reads=["ge4"], writes=["ge4"])
                P.op("dve", lambda e: e.tensor_reduce(out=gsum4[:], in_=ge4[:], axis=AX.X, op=ALU.add), reads=["ge4"], writes=["gsum4"])
                P.op("dve", lambda e: e.reciprocal(out=gsum4[:], in_=gsum4[:]), reads=["gsum4"], writes=["gsum4"])
                ohg_bj = bass.AP(ohg4[:].tensor, ohg4[:].offset, [list(ohg4[:].ap[0]), [4, 4], [1, 4], [0, 8]])
                P.op("dve", lambda e: e.tensor_tensor(out=prod4[:], in0=Lx, in1=ohg_bj, op=ALU.mult), reads=KL + ["ohg4"], writes=["prod4"])
                P.op("dve", lambda e: e.tensor_reduce(out=els4[:], in_=prod4[:].rearrange("p t g j -> p t j g"), axis=AX.X, op=ALU.add), reads=["prod4"], writes=["els4"])
                for t in range(4):
                    P.op("dve", lambda e, t=t: e.max(out=top84[:, t, :], in_=els4[:, t, :]), reads=["els4"], writes=[("top84", t)])
                T8 = [("top84", t) for t in range(4)]
                for i in range(2):
                    P.op("dve", lambda e, i=i: e.tensor_tensor(out=m124[:, i, :, :], in0=els4[:], in1=bc_last(col(top84, i), 8), op=ALU.is_equal),
                         reads=["els4"] + T8, writes=[("m124", i)])
                P.op("dve", lambda e: e.tensor_tensor(out=d214[:], in0=col(top84, 1), in1=col(top84, 0), op=ALU.subtract), reads=T8, writes=["d214"])
                P.op("act", lambda e: e.activation(out=d214[:], in_=d214[:], func=AF.Exp), reads=["d214"], writes=["d214"])
                P.op("dve", lambda e: e.tensor_scalar(out=den4[:], in0=d214[:], scalar1=1.0, scalar2=None, op0=ALU.add), reads=["d214"], writes=["den4"])
                P.op("dve", lambda e: e.reciprocal(out=den4[:], in_=den4[:]), reads=["den4"], writes=["den4"])
                P.op("dve", lambda e: e.tensor_tensor(out=col(rinfo4, 2), in0=gsum4[:], in1=den4[:], op=ALU.mult), reads=["gsum4", "den4", "rinfo4"], writes=["rinfo4"])
                P.op("dve", lambda e: e.tensor_tensor(out=col(rinfo4, 3), in0=col(rinfo4, 2), in1=d214[:], op=ALU.mult), reads=["rinfo4", "d214"], writes=["rinfo4"])
                for i in range(2):
                    mi = m124[:, i, :, :]
                    m_bg = bass.AP(mi.tensor, mi.offset, [list(mi.ap[0]), [8, 4], [0, 4], [1, 8]])
                    P.op("dve", lambda e, i=i, m_bg=m_bg: e.tensor_tensor(out=s124[:, i, :, :].rearrange("p t (g j) -> p t g j", g=4), in0=m_bg, in1=ohg_bj, op=ALU.mult),
                         reads=[("m124", i), "ohg4"], writes=[("s124", i)])
                P.op("dve", lambda e: e.tensor_tensor(out=sel4[:], in0=s124[:, 0, :, :], in1=s124[:, 1, :, :], op=ALU.add), reads=[("s124", 0), ("s124", 1)], writes=["sel4"])
                rc = rbankB()
                for t in range(4):
                    P.op("pe", lambda e, rc=rc, t=t: e.matmul(R_ps[:, rc, t * 32:(t + 1) * 32], lhsT=cst[:, 2, :], rhs=sel4[:, t, :], start=True, stop=(t == 0), skip_group_check=True),
                         reads=["sel4", "cst"], writes=[("R", rc)])
                    for tq in range(t):
                        P.op("pe", lambda e, rc=rc, t=t, tq=tq: e.matmul(R_ps[:, rc, t * 32:(t + 1) * 32], lhsT=ones_bf[:], rhs=sel4[:, tq, :], start=False, stop=(tq == t - 1), skip_group_check=True),
                             reads=["sel4", "ones_bf"], writes=[("R", rc)])
                rt_ = rbankB()
                for t in range(4):
                    P.op("pe", lambda e, rt_=rt_, t=t: e.matmul(R_ps[:, rt_, 0:32], lhsT=ones_bf[:], rhs=sel4[:, t, :], start=(t == 0), stop=(t == 3)),
                         reads=["sel4", "ones_bf"], writes=[("R", rt_)])
                base_b = bass.AP(base_bc[:].tensor, base_bc[:].offset, [list(base_bc[:].ap[0]), [0, 4], [1, 32]])
                P.op("dve", lambda e, rc=rc: e.tensor_tensor(out=dest4[:], in0=R_ps[:, rc, 0:128].rearrange("p (t e) -> p t e", t=4), in1=base_b, op=ALU.add),
                     reads=[("R", rc), "base_bc"], writes=["dest4"])
                P.op("dve", lambda e, rt_=rt_: e.tensor_tensor(out=base_bc[:], in0=R_ps[:, rt_, 0:32], in1=base_bc[:], op=ALU.add), reads=[("R", rt_), "base_bc"], writes=["base_bc"])
                for i in range(2):
                    P.op("dve", lambda e, i=i: e.tensor_tensor(out=tmp324[:], in0=s124[:, i, :, :], in1=dest4[:], op=ALU.mult), reads=[("s124", i), "dest4"], writes=["tmp324"])
                    P.op("dve", lambda e, i=i: e.tensor_reduce(out=col(rinfo4, i), in_=tmp324[:], axis=AX.X, op=ALU.add), reads=["tmp324", "rinfo4"], writes=["rinfo4"])
                P.op("dve", lambda e: e.tensor_copy(out=idx4[:], in_=rinfo4[:, :, 0:2]), reads=["rinfo4"], writes=["idx4"])
                for t in range(4):
                    for i in range(2):
                        P.dma("pool", lambda e, t=t, i=i: e.indirect_dma_start(
                            out=xs_d[:, :], out_offset=bass.IndirectOffsetOnAxis(ap=idx4[:, t, i:i + 1], axis=0), in_=tb[:, t, :], in_offset=None),
                            reads=["idx4", ("tb", t)], writes=["xs_d"])
                P.dma("sp", lambda e, r0=gi * 512: e.dma_start(out=rt_d[r0:r0 + 512, :].rearrange("(t p) c -> p t c", p=128), in_=rinfo4[:]), reads=["rinfo4"])

            P.dma("sp", lambda e: e.dma_start(out=uT[:], in_=uT_d.rearrange("(k p) t -> p k t", p=128)[:, :, 0:512]), writes=["uT"])
            P.dma("sp", lambda e: e.dma_start(out=attnT[:], in_=attnT_d.rearrange("(a p) t -> p a t", p=128)[:, :, 0:512]), writes=["attnT"])
            zip_run2(p2_front_a(0), None)
            zip_run2(p2_front_b(0), None)
            for gi in range(NG):
                b = gi // GPS
                s0 = (gi % GPS) * 512
                for j in range(4):
                    P.dma("sp", lambda e, j=j, b=b, r0=s0 + j * 128: e.dma_start(out=xt[:, j, :], in_=x[b, r0:r0 + 128, :]),
                          writes=[("xt", j)])
                zip_run2(p2_front_a(gi + 1) if gi + 1 < NG else None, p2_mid(gi), 1)
                zip_run2(p2_tail(gi), p2_front_b(gi + 1) if gi + 1 < NG else None, 3)
                p2_route(gi)
            P.flush()


        if 3 in phases:
          with ExitStack() as st:
            def sb(n, s_, d):
                return st.enter_context(nc.sbuf_tensor(n, s_, d))

            def ps(n, s_, d):
                return st.enter_context(nc.psum_tensor(n, s_, d))

            cst_f = sb("cst_f3", [128, 4, 128], F32)
            cst = sb("cst3", [128, 4, 128], BF16)
            wb1 = sb("wb1", [128, 2, 8, 512], BF16)
            wb3 = sb("wb3", [128, 2, 8, 512], BF16)
            wb2 = sb("wb2", [128, 2, 4, 1024], BF16)
            stage = sb("stage3", [128, 4, 1024], F32)
            stage2 = sb("stage3b", [128, 4, 1024], F32)
            xsb = sb("xsb", [128, 2, NBLK, 1024], BF16)
            XT = sb("XT", [128, 2, 8, CAP], BF16)
            hT = sb("hT", [128, 4, CAP], BF16)
            sil = sb("sil", [128, 2, 512], F32)
            ysb = sb("ysb", [128, NBLK, 1024], BF16)
            R_ps = ps("R_ps3", [128, 6, 512], F32)
            T_ps = ps("T_ps3", [128, 2, 1024], BF16)
            rrot = [0]

            def rbank():
                i = rrot[0] % 6
                rrot[0] += 1
                return i

            P.dma("sp", lambda e: e.dma_start(out=cst_f[:], in_=consts[:, :, :]), writes=["cst_f"])
            P.op("dve", lambda e: e.tensor_copy(out=cst[:], in_=cst_f[:]), reads=["cst_f"], writes=["cst"])
            wl = [0]
            blkc = [0]

            def xs_dma(ex):
                eb = ex % 2
                P.dma("sp", lambda e, eb=eb, ex=ex: e.dma_start(
                    out=xsb[:, eb, :, :], in_=xs_d[ex * CAP:(ex + 1) * CAP, :].rearrange("(n p) c -> p n c", p=128)), writes=[("xsb", eb)])

            def load_w13(ex):
                eb = ex % 2
                for kk in range(4):
                    for (wsrc, wdst, nm) in ((w1, wb1, "wb1"), (w3, wb3, "wb3")):
                        sl = wl[0] % 4
                        wl[0] += 1
                        P.dma("sp", lambda e, sl=sl, wsrc=wsrc, ex=ex, kk=kk: e.dma_start(
                            out=stage[:, sl, :].rearrange("p (k f) -> p k f", k=2),
                            in_=wsrc[ex, kk * 256:(kk + 1) * 256, :].rearrange("(k p) f -> p k f", p=128)), writes=[("stage", sl)])
                        P.op("pool", lambda e, sl=sl, wdst=wdst, eb=eb, kk=kk: e.tensor_copy(
                            out=wdst[:, eb, 2 * kk:2 * kk + 2, :], in_=stage[:, sl, :].rearrange("p (k f) -> p k f", k=2)),
                            reads=[("stage", sl)], writes=[(nm, eb, kk)])

            def load_w2_dma(ex):
                for f in range(4):
                    P.dma("sp", lambda e, ex=ex, f=f: e.dma_start(out=stage2[:, f, :], in_=w2[ex, f * 128:(f + 1) * 128, :]), writes=[("stage2", f)])

            def cast_w2(ex, eng):
                eb = ex % 2
                for f in range(4):
                    if eng == "act":
                        P.op("act", lambda e, eb=eb, f=f: e.activation(out=wb2[:, eb, f, :], in_=stage2[:, f, :], func=AF.Copy),
                             reads=[("stage2", f)], writes=[("wb2", eb, f)])
                    else:
                        P.op("pool", lambda e, eb=eb, f=f: e.tensor_copy(out=wb2[:, eb, f, :], in_=stage2[:, f, :]),
                             reads=[("stage2", f)], writes=[("wb2", eb, f)])

            def zip_run3(ga, gb, ratio):
                a_done = ga is None
                b_done = gb is None
                while not (a_done and b_done):
                    if not a_done:
                        try:
                            next(ga)
                        except StopIteration:
                            a_done = True
                    for _ in range(ratio):
                        if not b_done:
                            try:
                                next(gb)
                            except StopIteration:
                                b_done = True

            def gather_T(ex):
                eb = ex % 2
                for blk in range(NBLK):
                    bs = blkc[0] % 2
                    blkc[0] += 1
                    for k in range(8):
                        yield P.op("pe", lambda e, bs=bs, k=k, blk=blk, eb=eb: e.transpose(out=T_ps[:, bs, k * 128:(k + 1) * 128], in_=xsb[:, eb, blk, k * 128:(k + 1) * 128], identity=cst[:, 0, :]),
                                   reads=[("xsb", eb), "cst"], writes=[("T_ps", bs)])
                    if blk % 2 == 0:
                        yield P.op("dve", lambda e, bs=bs, blk=blk, eb=eb: e.tensor_copy(out=XT[:, eb, :, blk * 128:(blk + 1) * 128], in_=T_ps[:, bs, :].rearrange("p (k t) -> p k t", k=8)),
                                   reads=[("T_ps", bs)], writes=[("XT", eb, blk)])
                    else:
                        yield P.op("act", lambda e, bs=bs, blk=blk, eb=eb: e.activation(out=XT[:, eb, :, blk * 128:(blk + 1) * 128], in_=T_ps[:, bs, :].rearrange("p (k t) -> p k t", k=8), func=AF.Copy),
                                   reads=[("T_ps", bs)], writes=[("XT", eb, blk)])

            def expert_compute(ex):
                eb = ex % 2
                W1 = [("wb1", eb, kk) for kk in range(4)]
                W3 = [("wb3", eb, kk) for kk in range(4)]
                W2 = [("wb2", eb, f) for f in range(4)]
                for nt in range(CAP // 512):
                    XTK = [("XT", eb, nt * 4 + i) for i in range(4)]
                    for f in range(4):
                        r1 = rbank()
                        for k in range(8):
                            yield P.op("pe", lambda e, f=f, k=k, nt=nt, r1=r1, eb=eb: e.matmul(R_ps[:, r1, :], lhsT=wb1[:, eb, k, f * 128:(f + 1) * 128], rhs=XT[:, eb, k, nt * 512:(nt + 1) * 512],
                                                                                        start=(k == 0), stop=(k == 7)), reads=W1 + XTK, writes=[("R", r1)])
                        r3 = rbank()
                        for k in range(8):
                            yield P.op("pe", lambda e, f=f, k=k, nt=nt, r3=r3, eb=eb: e.matmul(R_ps[:, r3, :], lhsT=wb3[:, eb, k, f * 128:(f + 1) * 128], rhs=XT[:, eb, k, nt * 512:(nt + 1) * 512],
                                                                                        start=(k == 0), stop=(k == 7)), reads=W3 + XTK, writes=[("R", r3)])
                        ss = f % 2
                        yield P.op("act", lambda e, r1=r1, ss=ss: e.activation(out=sil[:, ss, :], in_=R_ps[:, r1, :], func=AF.Silu), reads=[("R", r1)], writes=[("sil", ss)])
                        yield P.op("dve", lambda e, r3=r3, ss=ss, f=f, nt=nt: e.tensor_tensor(out=hT[:, f, nt * 512:(nt + 1) * 512], in0=R_ps[:, r3, :], in1=sil[:, ss, :], op=ALU.mult),
                                   reads=[("R", r3), ("sil", ss)], writes=[("hT", f, nt)])
                if ex + 1 < NE:
                    cast_w2(ex + 1, "act")
                for blk in range(NBLK):
                    for n in range(2):
                        ry = rbank()
                        for f in range(4):
                            yield P.op("pe", lambda e, f=f, n=n, blk=blk, ry=ry, eb=eb: e.matmul(R_ps[:, ry, :], lhsT=hT[:, f, blk * 128:(blk + 1) * 128], rhs=wb2[:, eb, f, n * 512:(n + 1) * 512],
                                                                                          start=(f == 0), stop=(f == 3)), reads=W2 + [("hT", f, blk // 4)], writes=[("R", ry)])
                        if n == 0:
                            yield P.op("act", lambda e, ry=ry, blk=blk, n=n: e.activation(out=ysb[:, blk, n * 512:(n + 1) * 512], in_=R_ps[:, ry, :], func=AF.Copy),
                                       reads=[("R", ry)], writes=[("ysb", blk, n)])
                        else:
                            yield P.op("dve", lambda e, ry=ry, blk=blk, n=n: e.tensor_copy(out=ysb[:, blk, n * 512:(n + 1) * 512], in_=R_ps[:, ry, :]),
                                       reads=[("R", ry)], writes=[("ysb", blk, n)])
                yield P.dma("sp", lambda e, ex=ex: e.dma_start(out=ys_d[ex * CAP:(ex + 1) * CAP, :].rearrange("(n p) c -> p n c", p=128), in_=ysb[:]),
                            reads=[("ysb", blk, n) for blk in range(NBLK) for n in range(2)])

            xs_dma(0)
            load_w13(0)
            load_w2_dma(0)
            cast_w2(0, "pool")
            xs_dma(1)
            zip_run3(gather_T(0), None, 1)
            for ex in range(NE):
                if ex + 1 < NE:
                    load_w13(ex + 1)
                    load_w2_dma(ex + 1)
                if ex + 2 < NE:
                    xs_dma(ex + 2)
                zip_run3(gather_T(ex + 1) if ex + 1 < NE else None, expert_compute(ex), 8)
            P.flush()

        if 4 in phases:
          with ExitStack() as st:
            def sb(n, s_, d):
                return st.enter_context(nc.sbuf_tensor(n, s_, d))

            def ps(n, s_, d):
                return st.enter_context(nc.psum_tensor(n, s_, d))

            cst_f = sb("cst_f4", [128, 4, 128], F32)
            cst = sb("cst4", [128, 4, 128], BF16)
            wpg = sb("wpg", [128, 8, 1024], BF16)
            wpp = sb("wpp", [128, 2, 1024], BF16)
            stage = sb("stage4", [128, 2, 1024], F32)
            gple = sb("gple", [128, 1024], F32)
            bple = sb("bple", [128, 1024], F32)
            h1t = sb("h1t4", [128, 3, 1024], F32)
            yA = sb("yA", [128, 3, 1024], BF16)
            yB = sb("yB", [128, 3, 1024], BF16)
            h2 = sb("h2", [128, 2, 1024], F32)
            hn = sb("hn", [128, 2, 1024], BF16)
            hnT = sb("hnT", [128, 2, 8, 128], BF16)
            pt = sb("pt", [128, 3, 256], F32)
            pb = sb("pb", [128, 2, 256], BF16)
            pT = sb("pT4", [128, 2, 2, 128], BF16)
            gl = sb("gl", [128, 2, 1024], F32)
            ot = sb("ot", [128, 2, 1024], F32)
            rinfo = sb("rinfo4", [128, NTILE, 4], F32)
            idx = sb("idx4", [128, NTILE, 2], I32)
            junk = sb("junk4", [128, 1024], BF16)
            rs_ = sb("rs4", [128, 16], F32)
            R_ps = ps("R_ps4", [128, 5, 512], F32)
            T_ps = ps("T_ps4", [128, 3, 1024], BF16)
            rrot = [0]

            def rbank():
                i = rrot[0] % 5
                rrot[0] += 1
                return i
            stc = [0]

            def stat():
                i = stc[0] % 16
                stc[0] += 1
                return rs_[:, i:i + 1], ("rs_", i)

            P.dma("sp", lambda e: e.dma_start(out=cst_f[:], in_=consts[:, :, :]), writes=["cst_f"])
            P.op("dve", lambda e: e.tensor_copy(out=cst[:], in_=cst_f[:]), reads=["cst_f"], writes=["cst"])
            P.dma("sp", lambda e: e.dma_start(out=gple[:], in_=bcast_rows(vec["norm_ple"], 128, 1024)), writes=["gple"])
            P.dma("sp", lambda e: e.dma_start(out=bple[:], in_=bcast_rows(vec["b_ple_gate"], 128, 1024)), writes=["bple"])
            wl = [0]
            for k in range(8):
                sl = wl[0] % 2
                wl[0] += 1
                P.dma("sp", lambda e, sl=sl, k=k: e.dma_start(out=stage[:, sl, :], in_=w_pg[k * 128:(k + 1) * 128, :]), writes=[("stage", sl)])
                P.op("pool", lambda e, sl=sl, k=k: e.tensor_copy(out=wpg[:, k, :], in_=stage[:, sl, :]), reads=[("stage", sl)], writes=[("wpg", k)])
            for k in range(2):
                sl = wl[0] % 2
                wl[0] += 1
                P.dma("sp", lambda e, sl=sl, k=k: e.dma_start(out=stage[:, sl, :], in_=w_pp[k * 128:(k + 1) * 128, :]), writes=[("stage", sl)])
                P.op("pool", lambda e, sl=sl, k=k: e.tensor_copy(out=wpp[:, k, :], in_=stage[:, sl, :]), reads=[("stage", sl)], writes=[("wpp", k)])
            WPG = [("wpg", k) for k in range(8)]
            WPP = [("wpp", k) for k in range(2)]
            def p4_loads(ti):
                b = ti // TPS
                r0 = (ti % TPS) * 128
                s = ti % 3
                P.dma("pool", lambda e, s=s, ti=ti: e.indirect_dma_start(out=yA[:, s, :], out_offset=None, in_=ys_d[:, :],
                                                                 in_offset=bass.IndirectOffsetOnAxis(ap=idx[:, ti, 0:1], axis=0)), reads=["idx"], writes=[("yA", s)])
                P.dma("pool", lambda e, s=s, ti=ti: e.indirect_dma_start(out=yB[:, s, :], out_offset=None, in_=ys_d[:, :],
                                                                 in_offset=bass.IndirectOffsetOnAxis(ap=idx[:, ti, 1:2], axis=0)), reads=["idx"], writes=[("yB", s)])
                P.dma("sp", lambda e, s=s, ti=ti: e.dma_start(out=h1t[:, s, :], in_=h1_d[ti * 128:(ti + 1) * 128, :]), writes=[("h1t", s)])
                P.dma("sp", lambda e, s=s, b=b, r0=r0: e.dma_start(out=pt[:, s, :], in_=pin[b, r0:r0 + 128, :]), writes=[("pt", s)])

            P.dma("sp", lambda e: e.dma_start(out=rinfo[:], in_=rt_d.rearrange("(t p) c -> p t c", p=128)), writes=["rinfo"])
            P.op("pool", lambda e: e.tensor_copy(out=idx[:], in_=rinfo[:, :, 0:2]), reads=["rinfo"], writes=["idx"])
            p4_loads(0)
            p4_loads(1)
            def p4_front(ti):
                b = ti // TPS
                r0 = (ti % TPS) * 128
                s = ti % 2
                sl = ti % 3
                yield P.op("dve", lambda e, s=s, sl=sl, ti=ti: e.scalar_tensor_tensor(out=h2[:, s, :], in0=yA[:, sl, :], scalar=rinfo[:, ti, 2:3], in1=h1t[:, sl, :], op0=ALU.mult, op1=ALU.add),
                     reads=[("yA", sl), "rinfo", ("h1t", sl)], writes=[("h2", s)])
                yield P.op("dve", lambda e, s=s, sl=sl, ti=ti: e.scalar_tensor_tensor(out=h2[:, s, :], in0=yB[:, sl, :], scalar=rinfo[:, ti, 3:4], in1=h2[:, s, :], op0=ALU.mult, op1=ALU.add),
                     reads=[("yB", sl), "rinfo", ("h2", s)], writes=[("h2", s)])
                ssq, kssq = stat()
                yield P.op("act", lambda e, s=s, ssq=ssq: e.activation(out=junk[:], in_=h2[:, s, :], func=AF.Square, accum_out=ssq), reads=[("h2", s)], writes=["junk", kssq])
                lnv, klnv = stat()
                yield P.op("act", lambda e, ssq=ssq, lnv=lnv: e.activation(out=lnv, in_=ssq, func=AF.Ln, scale=1.0 / 1024, bias=EPS), reads=[kssq], writes=[klnv])
                rstd, krstd = stat()
                yield P.op("act", lambda e, lnv=lnv, rstd=rstd: e.activation(out=rstd, in_=lnv, func=AF.Exp, scale=-0.5), reads=[klnv], writes=[krstd])
                yield P.op("dve", lambda e, s=s, rstd=rstd: e.scalar_tensor_tensor(out=hn[:, s, :], in0=h2[:, s, :], scalar=rstd, in1=gple[:], op0=ALU.mult, op1=ALU.mult),
                     reads=[("h2", s), krstd, "gple"], writes=[("hn", s)])
                for k in range(8):
                    yield P.op("pe", lambda e, k=k, s=s: e.transpose(out=T_ps[:, s, k * 128:(k + 1) * 128], in_=hn[:, s, k * 128:(k + 1) * 128], identity=cst[:, 0, :]),
                         reads=[("hn", s), "cst"], writes=[("T_ps", s)])
                yield P.op("act", lambda e, s=s: e.activation(out=hnT[:, s, :, :], in_=T_ps[:, s, :].rearrange("p (k t) -> p k t", k=8), func=AF.Copy), reads=[("T_ps", s)], writes=[("hnT", s)])
                yield P.op("act", lambda e, s=s, sl=sl: e.activation(out=pb[:, s, :], in_=pt[:, sl, :], func=AF.Copy), reads=[("pt", sl)], writes=[("pb", s)])
                for k in range(2):
                    yield P.op("pe", lambda e, k=k, s=s: e.transpose(out=T_ps[:, 2, s * 256 + k * 128:s * 256 + (k + 1) * 128], in_=pb[:, s, k * 128:(k + 1) * 128], identity=cst[:, 0, :]),
                         reads=[("pb", s), "cst"], writes=[("T_psp", s)])
                yield P.op("act", lambda e, s=s: e.activation(out=pT[:, s, :, :], in_=T_ps[:, 2, s * 256:(s + 1) * 256].rearrange("p (k t) -> p k t", k=2), func=AF.Copy), reads=[("T_psp", s)], writes=[("pT", s)])

            def p4_back(ti):
                b = ti // TPS
                r0 = (ti % TPS) * 128
                s = ti % 2
                sl = ti % 3
                for n in range(2):
                    rg = rbank()
                    for k in range(8):
                        yield P.op("pe", lambda e, k=k, n=n, rg=rg, s=s: e.matmul(R_ps[:, rg, :], lhsT=hnT[:, s, k, :], rhs=wpg[:, k, n * 512:(n + 1) * 512], start=(k == 0), stop=(k == 7)),
                             reads=[("hnT", s)] + WPG, writes=[("R", rg)])
                    rp = rbank()
                    for k in range(2):
                        yield P.op("pe", lambda e, k=k, n=n, rp=rp, s=s: e.matmul(R_ps[:, rp, :], lhsT=pT[:, s, k, :], rhs=wpp[:, k, n * 512:(n + 1) * 512], start=(k == 0), stop=(k == 1)),
                             reads=[("pT", s)] + WPP, writes=[("R", rp)])
                    yield P.op("dve", lambda e, n=n, rg=rg, s=s: e.tensor_tensor(out=gl[:, s, n * 512:(n + 1) * 512], in0=R_ps[:, rg, :], in1=bple[:, n * 512:(n + 1) * 512], op=ALU.add),
                         reads=[("R", rg), "bple"], writes=[("gl", s, n)])
                    yield P.op("act", lambda e, n=n, s=s: e.activation(out=gl[:, s, n * 512:(n + 1) * 512], in_=gl[:, s, n * 512:(n + 1) * 512], func=AF.Sigmoid),
                         reads=[("gl", s, n)], writes=[("gl", s, n)])
                    yield P.op("dve", lambda e, n=n, rp=rp, s=s: e.tensor_tensor(out=gl[:, s, n * 512:(n + 1) * 512], in0=R_ps[:, rp, :], in1=gl[:, s, n * 512:(n + 1) * 512], op=ALU.mult),
                         reads=[("R", rp), ("gl", s, n)], writes=[("gl", s, n)])
                    yield P.op("dve", lambda e, n=n, s=s: e.tensor_tensor(out=ot[:, s, n * 512:(n + 1) * 512], in0=gl[:, s, n * 512:(n + 1) * 512], in1=h2[:, s, n * 512:(n + 1) * 512], op=ALU.add),
                         reads=[("gl", s, n), ("h2", s)], writes=[("ot", s, n)])
                yield P.dma("sp", lambda e, s=s, b=b, r0=r0: e.dma_start(out=out[b, r0:r0 + 128, :], in_=ot[:, s, :]), reads=[("ot", s, 0), ("ot", s, 1)])

            def zip_run(ga, gb, ratio=2):
                a_done = ga is None
                b_done = gb is None
                while not (a_done and b_done):
                    if not a_done:
                        try:
                            next(ga)
                        except StopIteration:
                            a_done = True
                    for _ in range(ratio):
                        if not b_done:
                            try:
                                next(gb)
                            except StopIteration:
                                b_done = True

            zip_run(p4_front(0), None)
            for ti in range(NTILE):
                if ti + 2 < NTILE:
                    p4_loads(ti + 2)
                zip_run(p4_front(ti + 1) if ti + 1 < NTILE else None, p4_back(ti))
            P.flush()
    return nc


def make_consts(CAP):
    k = np.arange(128)
    c = np.zeros((128, 4, 128), np.float32)
    c[:, 0, :] = np.eye(128, dtype=np.float32)
    c[:, 1, :] = (k[:, None] <= k[None, :]).astype(np.float32)
    c[:, 2, :] = (k[:, None] < k[None, :]).astype(np.float32)
    blk = (k[:, None] // 64 == k[None, :] // 64).astype(np.float32) / 64.0
    c[:, 3, :] = blk
    capoff = (np.arange(32, dtype=np.float32) * CAP).reshape(1, 32)
    return c, capoff


def core_inputs(inputs, core, NB, CAP):
    f = lambda a: np.ascontiguousarray(a, dtype=np.float32)
    L = 0
    consts, capoff = make_consts(CAP)
    m = {
        "x": f(inputs["x"][core * NB:(core + 1) * NB]),
        "p": f(inputs["p"][L, core * NB:(core + 1) * NB]),
        "w_in": f(inputs["w_in"][L]),
        "w_attn_out": f(inputs["w_attn_out"][L]),
        "w_conv_out": f(inputs["w_conv_out"][L]),
        "w_o": f(inputs["w_o"][L]),
        "w_rt": f(np.concatenate([inputs["w_router_group"][L], inputs["w_router_expert"][L]], axis=1)),
        "b_rt": f(np.concatenate([inputs["b_router_group"][L], inputs["b_router_expert"][L]])[None, :]),
        "w1": f(inputs["w1"][L]), "w3": f(inputs["w3"][L]), "w2": f(inputs["w2"][L]),
        "w_ple_gate": f(inputs["w_ple_gate"][L]), "w_ple_proj": f(inputs["w_ple_proj"][L]),
        "consts": consts, "capoff": capoff,
        "conv_w": f(inputs["conv_w"][L].T),
    }
    for n in ["norm_mix", "norm_ffn", "norm_ple", "b_ple_gate", "lambda_q1", "lambda_k1", "lambda_q2",
              "lambda_k2", "subln", "b_conv_in", "b_gate", "conv_b", "conv_ln_g", "conv_ln_b", "b_conv_out"]:
        m[n] = f(inputs[n][L]).reshape(1, -1)
    m["q_norm"] = f(inputs["q_norm"][L]).reshape(1, 128)
    m["k_norm"] = f(inputs["k_norm"][L]).reshape(1, 128)
    return m


_CACHE = {}


def kernel(**inputs):
    NB, CAP = 2, 1024
    if "nc" not in _CACHE:
        _CACHE["nc"] = build_program(S=4096, NB=NB, CAP=CAP)
    nc = _CACHE["nc"]
    in_maps = [core_inputs(inputs, c, NB, CAP) for c in range(8)]
    res = run_bass_kernel_spmd(nc, in_maps, core_ids=list(range(8)))
    return np.concatenate([np.asarray(r["out"]) for r in res.results], axis=0).astype(np.float32)
```

```python
from contextlib import ExitStack

import numpy as np
import concourse.bass as bass
import concourse.mybir as mybir
from concourse.bass_utils import run_bass_kernel_spmd

F32 = mybir.dt.float32
BF16 = mybir.dt.bfloat16
I32 = mybir.dt.int32
AF = mybir.ActivationFunctionType
ALU = mybir.AluOpType
AX = mybir.AxisListType

ENGS = ("pe", "act", "dve", "pool", "sp")
NDMASEM = 8
EPS = 1e-6
STOP = [99]
LAMBDA_INIT = 0.2


class Op:
    __slots__ = ("eng", "fn", "deps", "isdma", "tok", "marked", "cum")

    def __init__(self, eng, fn, isdma):
        self.eng = eng
        self.fn = fn
        self.deps = []
        self.isdma = isdma
        self.tok = None
        self.marked = False
        self.cum = None


class Prog:
    SEM_LIMIT = 12000

    def __init__(self, nc, stack):
        self.nc = nc
        self.stack = stack
        self.nsem = 0
        self.sems = {e: self._newsem(e) for e in ENGS}
        self.finals = []
        self.dsems = {e: [stack.enter_context(nc.semaphore("d_%s%d" % (e, i)))
                          for i in range(NDMASEM)] for e in ("sp", "pool")}
        self.dcount = {e: 0 for e in self.dsems}
        self.base = {e: 0 for e in ENGS}
        self._reset()

    def _newsem(self, e):
        self.nsem += 1
        return self.stack.enter_context(self.nc.semaphore("s_%s_%d" % (e, self.nsem)))

    def _reset(self):
        self.ops = {e: [] for e in ENGS}
        self.lastw = {}
        self.readers = {}

    def _add(self, eng, fn, reads, writes, isdma):
        op = Op(eng, fn, isdma)
        deps = []
        for r in reads:
            w = self.lastw.get(r)
            if w is not None:
                deps.append(w)
        for w_ in writes:
            w = self.lastw.get(w_)
            if w is not None:
                deps.append(w)
            deps.extend(self.readers.get(w_, ()))
        op.deps = list(dict.fromkeys(deps))
        self.ops[eng].append(op)
        for r in reads:
            self.readers.setdefault(r, []).append(op)
        for w_ in writes:
            self.lastw[w_] = op
            self.readers[w_] = []
        if isdma:
            n = self.dcount[eng]
            self.dcount[eng] = n + 1
            sem = self.dsems[eng][n % NDMASEM]
            op.tok = (sem, 16 * (n // NDMASEM + 1))
            op.cum = (sem, 16 * (n // NDMASEM))
        return op

    def op(self, eng, fn, reads=(), writes=()):
        return self._add(eng, fn, tuple(reads), tuple(writes), False)

    def dma(self, eng, fn, reads=(), writes=()):
        return self._add(eng, fn, tuple(reads), tuple(writes), True)

    def flush(self):
        nc = self.nc
        ops = self.ops
        for e in ENGS:
            for op in ops[e]:
                for d in op.deps:
                    if not d.isdma and not (d.eng == "pe" and e == "pe"):
                        d.marked = True
            last = [o for o in ops[e] if not o.isdma]
            if last:
                last[-1].marked = True
        for e in ENGS:
            c = self.base[e]
            for op in ops[e]:
                if not op.isdma and op.marked:
                    if c >= self.SEM_LIMIT:
                        self.finals.append((self.sems[e], c))
                        self.sems[e] = self._newsem(e)
                        c = 0
                    c += 1
                    op.tok = (self.sems[e], c)
            self.base[e] = c
        prog = self

        def emit_engine(e, eng):
            waited = {}

            def wait(sem, val):
                k = id(sem)
                if val > 0 and waited.get(k, 0) < val:
                    waited[k] = val
                    eng.wait_ge(sem, val)

            for op in ops[e]:
                if op.isdma:
                    wait(*op.cum)
                for d in op.deps:
                    if d.eng == "pe" and e == "pe" and not d.isdma:
                        continue
                    wait(*d.tok)
                ins = op.fn(eng)
                if op.isdma:
                    ins.then_inc(op.tok[0], 16)
                elif op.marked:
                    ins.then_inc(op.tok[0], 1)
            for (fs, fv) in prog.finals:
                wait(fs, fv)
            for e2 in ENGS:
                wait(prog.sems[e2], prog.base[e2])
            for q in prog.dsems:
                n = prog.dcount[q]
                for i in range(NDMASEM):
                    k = (n - i + NDMASEM - 1) // NDMASEM if n > i else 0
                    wait(prog.dsems[q][i], 16 * k)

        with nc.Block() as block:
            @block.tensor
            def _(eng):
                emit_engine("pe", eng)

            @block.scalar
            def _(eng):
                emit_engine("act", eng)

            @block.vector
            def _(eng):
                emit_engine("dve", eng)

            @block.gpsimd
            def _(eng):
                emit_engine("pool", eng)

            @block.sync
            def _(eng):
                emit_engine("sp", eng)
        self._reset()


def bcast_rows(ap, nrows, ncols, off=0):
    return bass.AP(ap.tensor, off, [[0, nrows], [1, ncols]])


def build_program(S=4096, NB=2, CAP=1024, debug=(), phases=(1, 2, 3, 4)):
    nc = bass.Bass("TRN2", target_bir_lowering=False)
    NT = NB * S
    NTILE = NT // 128
    NG = NT // 512
    GPS = S // 512
    TPS = S // 128
    NE = 32
    NBLK = CAP // 128

    def din(name, shape, dt=F32):
        return nc.dram_tensor(name, shape, dt, kind="ExternalInput").ap()

    def dscr(name, shape, dt):
        kind = "ExternalOutput" if name in debug else "Internal"
        return nc.dram_tensor(name, shape, dt, kind=kind).ap()

    x = din("x", [NB, S, 1024])
    pin = din("p", [NB, S, 256])
    w_in = din("w_in", [1024, 4608])
    w_ao = din("w_attn_out", [512, 1024])
    w_co = din("w_conv_out", [512, 1024])
    w_o = din("w_o", [1024, 1024])
    w_rt = din("w_rt", [1024, 36])
    w1 = din("w1", [NE, 1024, 512])
    w3 = din("w3", [NE, 1024, 512])
    w2 = din("w2", [NE, 512, 1024])
    w_pg = din("w_ple_gate", [1024, 1024])
    w_pp = din("w_ple_proj", [256, 1024])
    consts = din("consts", [128, 4, 128])
    capoff = din("capoff", [1, 32])
    vec = {n: din(n, [1, l]) for n, l in [
        ("norm_mix", 1024), ("norm_ffn", 1024), ("norm_ple", 1024), ("b_ple_gate", 1024),
        ("b_rt", 36), ("lambda_q1", 64), ("lambda_k1", 64), ("lambda_q2", 64), ("lambda_k2", 64),
        ("q_norm", 128), ("k_norm", 128), ("subln", 128), ("b_conv_in", 1024), ("b_gate", 2048),
        ("conv_b", 512), ("conv_ln_g", 512), ("conv_ln_b", 512), ("b_conv_out", 1024)]}
    conv_w = din("conv_w", [512, 31])
    out = nc.dram_tensor("out", [NB, S, 1024], F32, kind="ExternalOutput").ap()

    uT_d = dscr("uT_d", [1024, NT], BF16)
    attnT_d = dscr("attnT_d", [512, NT], BF16)
    h1_d = dscr("h1_d", [NT, 1024], F32)
    xs_d = dscr("xs_d", [NE * CAP, 1024], BF16)
    ys_d = dscr("ys_d", [NE * CAP, 1024], BF16)
    rt_d = dscr("rt_d", [NT, 4], F32)

    def col_ap(v, n, off=0):
        return bass.AP(v.tensor, off, [[1, n], [1, 1]])

    with ExitStack() as top:
        P = Prog(nc, top)

        if 1 in phases:
          with ExitStack() as st:
            def sb(n, s, d):
                return st.enter_context(nc.sbuf_tensor(n, s, d))

            def ps(n, s, d):
                return st.enter_context(nc.psum_tensor(n, s, d))

            cst_f = sb("cst_f", [128, 4, 128], F32)
            cst = sb("cst", [128, 4, 128], BF16)
            wq = sb("wq", [128, 8, 1536], BF16)
            stage = sb("stage", [128, 2, 1536], F32)
            kT = sb("kT", [128, 4, S], BF16)
            Va = sb("Va", [128, TPS, 4, 129], BF16)
            xt = sb("xt", [128, 2, 1024], F32)
            gmix = sb("gmix", [128, 1024], F32)
            junk = sb("junk", [128, 1024], BF16)
            ub = sb("ub", [128, 2, 1024], BF16)
            uT = sb("uT", [128, 1, 8, 512], BF16)
            qT = sb("qT", [128, 2, 4, 512], BF16)
            sq = sb("sq", [128, 2, 512], BF16)
            lnb = sb("lnb", [128, 2, 512], F32)
            rsb = sb("rsb", [128, 2, 512], F32)
            pT = sb("pT", [128, 3, 2, 512], BF16)
            atm = sb("atm", [128, 4, 512], BF16)
            attnT = sb("attnT", [128, 1, 4, 512], BF16)
            o0n = sb("o0n", [128, 2, 128], F32)
            Ocp = sb("Ocp", [128, 2, 3, 387], F32)
            dd = sb("dd", [128, 2, 128], F32)
            junk2 = sb("junk2", [128, 128], BF16)
            st1 = sb("st1", [128, 64], F32)
            gqk = sb("gqk", [128, 2], F32)
            lamv = sb("lamv", [128, 4, 64], F32)
            lams = sb("lams", [128, 8], F32)

            O_ps = ps("O_ps", [128, 3, 512], F32)
            T_ps = ps("T_ps", [128, 1024], BF16)
            R_ps = ps("R_ps", [128, 4, 512], F32)
            rrot = [0]

            def rbank():
                i = rrot[0] % 3
                rrot[0] += 1
                return i

            ident = cst[:, 0, :]
            tri = cst[:, 1, :]
            bones = cst[:, 3, :]

            P.dma("sp", lambda e: e.dma_start(out=cst_f[:], in_=consts[:, :, :]), writes=["cst_f"])
            P.op("dve", lambda e: e.tensor_copy(out=cst[:], in_=cst_f[:]), reads=["cst_f"], writes=["cst"])
            P.dma("sp", lambda e: e.dma_start(out=gmix[:], in_=bcast_rows(vec["norm_mix"], 128, 1024)), writes=["gmix"])
            P.dma("sp", lambda e: e.dma_start(out=gqk[:, 0:1], in_=col_ap(vec["q_norm"], 128)), writes=["gqk0"])
            P.dma("sp", lambda e: e.dma_start(out=gqk[:, 1:2], in_=col_ap(vec["k_norm"], 128)), writes=["gqk1"])
            P.op("dve", lambda e: e.tensor_scalar(out=gqk[:, 0:1], in0=gqk[:, 0:1], scalar1=0.125, scalar2=None, op0=ALU.mult),
                 reads=["gqk0"], writes=["gqk0"])
            for i, n in enumerate(["lambda_q1", "lambda_k1", "lambda_q2", "lambda_k2"]):
                P.dma("sp", lambda e, i=i, n=n: e.dma_start(out=lamv[:, i, :], in_=bcast_rows(vec[n], 128, 64)), writes=[("lamv", i)])
            for i in range(2):
                P.op("dve", lambda e, i=i: e.tensor_tensor(
                    out=lamv[:, 2 * i, :], in0=lamv[:, 2 * i, :], in1=lamv[:, 2 * i + 1, :], op=ALU.mult),
                    reads=[("lamv", 2 * i), ("lamv", 2 * i + 1)], writes=[("lamv", 2 * i)])
                P.op("dve", lambda e, i=i: e.tensor_reduce(out=lams[:, i:i + 1], in_=lamv[:, 2 * i, :], axis=AX.X, op=ALU.add),
                     reads=[("lamv", 2 * i)], writes=[("lams", i)])
                P.op("act", lambda e, i=i: e.activation(out=lams[:, 2 + i:3 + i], in_=lams[:, i:i + 1], func=AF.Exp),
                     reads=[("lams", i)], writes=[("lams", 2 + i)])
            P.op("dve", lambda e: e.tensor_tensor(out=lams[:, 4:5], in0=lams[:, 3:4], in1=lams[:, 2:3], op=ALU.subtract),
                 reads=[("lams", 2), ("lams", 3)], writes=[("lams", 4)])
            P.op("dve", lambda e: e.tensor_scalar(out=lams[:, 5:6], in0=lams[:, 4:5], scalar1=-LAMBDA_INIT, scalar2=None, op0=ALU.add),
                 reads=[("lams", 4)], writes=["nlam"])
            nlam = lams[:, 5:6]
            P.op("pool", lambda e: e.memset(Va[:, :, :, 128:129], 1.0), writes=["Va_ones"])
            P.op("pool", lambda e: e.memset(qT[:], 0.0), writes=[("qT", c, m) for c in range(4) for m in range(2)])
            zt = sb("zt", [128, 2, 1024], BF16)
            P.op("pool", lambda e: e.memset(zt[:], 0.0), writes=["zt"])
            xs_v = xs_d.rearrange("(n p) c -> p n c", p=128)
            for i in range(NE * CAP // 256):
                P.dma("pool", lambda e, i=i: e.dma_start(out=xs_v[:, i * 2:(i + 1) * 2, :], in_=zt[:]), reads=["zt"], writes=[("xs_z", i)])
            for k in range(8):
                sl = k % 2
                P.dma("sp", lambda e, k=k, sl=sl: e.dma_start(out=stage[:, sl, :], in_=w_in[k * 128:(k + 1) * 128, 0:1536]),
                      writes=[("stage", sl)])
                P.op("pool", lambda e, k=k, sl=sl: e.tensor_copy(out=wq[:, k, :], in_=stage[:, sl, :]),
                     reads=[("stage", sl)], writes=[("wq", k)])
            WQ = [("wq", k) for k in range(8)]

            stc = [0]

            def stat():
                i = stc[0] % 64
                stc[0] += 1
                return st1[:, i:i + 1], ("st1", i)

            tilec = [0]
            deferred = []
            arot = [0]
            for gi in range(NG):
                b = gi // GPS
                G = gi % GPS
                s0 = G * 512
                us = 0
                for j in range(4):
                    tc_ = tilec[0]
                    tilec[0] += 1
                    xs = tc_ % 2
                    P.dma("sp", lambda e, xs=xs, b=b, r0=s0 + j * 128: e.dma_start(out=xt[:, xs, :], in_=x[b, r0:r0 + 128, :]),
                          writes=[("xt", xs)])
                    ssq, kssq = stat()
                    P.op("act", lambda e, xs=xs, ssq=ssq: e.activation(
                        out=junk[:], in_=xt[:, xs, :], func=AF.Square, accum_out=ssq), reads=[("xt", xs)], writes=["junk", kssq])
                    lnv, klnv = stat()
                    P.op("act", lambda e, ssq=ssq, lnv=lnv: e.activation(out=lnv, in_=ssq, func=AF.Ln, scale=1.0 / 1024, bias=EPS),
                         reads=[kssq], writes=[klnv])
                    rstd, krstd = stat()
                    P.op("act", lambda e, lnv=lnv, rstd=rstd: e.activation(out=rstd, in_=lnv, func=AF.Exp, scale=-0.5),
                         reads=[klnv], writes=[krstd])
                    P.op("dve", lambda e, xs=xs, rstd=rstd: e.scalar_tensor_tensor(
                        out=ub[:, xs, :], in0=xt[:, xs, :], scalar=rstd, in1=gmix[:], op0=ALU.mult, op1=ALU.mult),
                        reads=[("xt", xs), krstd, "gmix"], writes=[("ub", xs)])
                    for k in range(8):
                        P.op("pe", lambda e, xs=xs, k=k: e.transpose(out=T_ps[:, k * 128:(k + 1) * 128], in_=ub[:, xs, k * 128:(k + 1) * 128], identity=ident),
                             reads=[("ub", xs), "cst"], writes=["T_ps"])
                    P.op("dve", lambda e, us=us, j=j: e.tensor_copy(
                        out=uT[:, us, :, j * 128:(j + 1) * 128], in_=T_ps[:].rearrange("p (k t) -> p k t", k=8)),
                        reads=["T_ps"], writes=[("uT", us, j)])
                UT = [("uT", us, j) for j in range(4)]
                P.dma("sp", lambda e, us=us, c0=gi * 512: e.dma_start(
                    out=uT_d.rearrange("(k p) t -> p k t", p=128)[:, :, c0:c0 + 512], in_=uT[:, us, :, :]), reads=UT)

                if STOP[0] <= 1:
                    continue
                def qk_proj(c):
                    rb = c % 2
                    for k in range(8):
                        P.op("pe", lambda e, c=c, k=k, rb=rb: e.matmul(
                            R_ps[:, rb, :], lhsT=wq[:, k, c * 128:(c + 1) * 128], rhs=uT[:, us, k, :], start=(k == 0), stop=(k == 7)),
                            reads=UT + [("wq", k)], writes=[("R", rb)])
                qk_proj(0)
                for c in range(8):
                    rb = c % 2
                    s2 = c % 2
                    P.op("act", lambda e, rb=rb, s2=s2: e.activation(out=sq[:, s2, :], in_=R_ps[:, rb, :], func=AF.Square),
                         reads=[("R", rb)], writes=[("sq", s2)])
                    rb2 = 2
                    P.op("pe", lambda e, rb2=rb2, s2=s2: e.matmul(R_ps[:, rb2, :], lhsT=bones, rhs=sq[:, s2, :], start=True, stop=True),
                         reads=[("sq", s2), "cst"], writes=[("R", rb2)])
                    P.op("act", lambda e, rb2=rb2, s2=s2: e.activation(out=lnb[:, s2, :], in_=R_ps[:, rb2, :], func=AF.Ln, bias=EPS),
                         reads=[("R", rb2)], writes=[("lnb", s2)])
                    P.op("act", lambda e, s2=s2: e.activation(out=rsb[:, s2, :], in_=lnb[:, s2, :], func=AF.Exp, scale=-0.5),
                         reads=[("lnb", s2)], writes=[("rsb", s2)])
                    if c < 4:
                        for m in range(2):
                            pr = slice(m * 64, (m + 1) * 64)
                            P.op("dve", lambda e, rb=rb, s2=s2, m=m, pr=pr, c=c: e.scalar_tensor_tensor(
                                out=qT[pr, m, c, :], in0=R_ps[pr, rb, :], scalar=gqk[pr, 0:1], in1=rsb[pr, s2, :], op0=ALU.mult, op1=ALU.mult),
                                reads=[("R", rb), ("rsb", s2), "gqk0"], writes=[("qT", c, m)])
                    else:
                        P.op("dve", lambda e, rb=rb, s2=s2, c=c, s0=s0: e.scalar_tensor_tensor(
                            out=kT[:, c - 4, s0:s0 + 512], in0=R_ps[:, rb, :], scalar=gqk[:, 1:2], in1=rsb[:, s2, :], op0=ALU.mult, op1=ALU.mult),
                            reads=[("R", rb), ("rsb", s2), "gqk1"], writes=[("kT", c - 4, G)])
                    if c + 1 < 8:
                        qk_proj(c + 1)
                rrot[0] = 0
                if STOP[0] <= 2:
                    continue
                for j in range(4):
                    rb = rbank()
                    for k in range(8):
                        P.op("pe", lambda e, j=j, k=k, rb=rb: e.matmul(
                            R_ps[:, rb, :], lhsT=uT[:, us, k, j * 128:(j + 1) * 128], rhs=wq[:, k, 1024:1536], start=(k == 0), stop=(k == 7)),
                            reads=UT + [("wq", k)], writes=[("R", rb)])
                    P.op("dve", lambda e, j=j, rb=rb, tl=G * 4 + j: e.tensor_copy(
                        out=Va[:, tl, :, 0:128], in_=R_ps[:, rb, :].rearrange("p (h v) -> p h v", h=4)),
                        reads=[("R", rb)], writes=[("Va", G * 4 + j)])

                if STOP[0] <= 3:
                    continue
                nkt = 4 * G + 4
                for h in range(4):
                    steps = [(m, jp) for m in range(2) for jp in range(nkt // 2)]

                    def acc(m, c):
                        a = m * 4 + c
                        return a // 3, (a % 3) * 129

                    def emit_qk(si, h=h, steps=steps):
                        m, jp = steps[si]
                        j0 = 2 * jp
                        r0 = j0 - 4 * G
                        q0 = max(r0, 0) * 128
                        ncol = 512 - q0
                        pb_ = arot[0] % 2
                        arot[0] += 1
                        for t in range(2):
                            P.op("pe", lambda e, m=m, j=j0 + t, q0=q0, ncol=ncol, rb=2 * pb_ + t: e.matmul(
                                R_ps[:, rb, 0:ncol], lhsT=kT[:, h, j * 128:(j + 1) * 128],
                                rhs=qT[:, m, h, q0:512], start=True, stop=True),
                                reads=[("kT", h, (j0 + t) // 4), ("qT", h, m)], writes=[("R", 2 * pb_ + t)])
                        pslot = si % 3
                        P.op("act", lambda e, pb_=pb_, ncol=ncol, pslot=pslot: e.activation(
                            out=pT[:, pslot, :, 0:ncol], in_=R_ps[:, 2 * pb_:2 * pb_ + 2, 0:ncol], func=AF.Exp),
                            reads=[("R", 2 * pb_), ("R", 2 * pb_ + 1)], writes=[("pT", pslot)])
                        for t in range(2):
                            r = r0 + t
                            if r >= 0:
                                off = r * 128 - q0
                                P.op("pool", lambda e, pslot=pslot, t=t, off=off: e.tensor_tensor(
                                    out=pT[:, pslot, t, off:off + 128], in0=pT[:, pslot, t, off:off + 128], in1=tri, op=ALU.mult),
                                    reads=[("pT", pslot), "cst"], writes=[("pT", pslot)])
                        return (m, j0, r0, q0, pslot)

                    started = set()

                    def emit_pv(info, h=h, started=started):
                        m, j0, r0, q0, pslot = info
                        for t in range(2):
                            j = j0 + t
                            r = r0 + t
                            for c in range(max(r, 0), 4):
                                bank, off = acc(m, c)
                                first = bank not in started
                                started.add(bank)
                                lastj = (j == 4 * G + c)
                                P.op("pe", lambda e, pslot=pslot, t=t, c=c, q0=q0, bank=bank, off=off, first=first, lastj=lastj, j=j: e.matmul(
                                    O_ps[:, bank, off:off + 129], lhsT=pT[:, pslot, t, c * 128 - q0:c * 128 - q0 + 128],
                                    rhs=Va[:, j, h, :], start=first, stop=lastj, skip_group_check=True),
                                    reads=[("pT", pslot), ("Va", j), "Va_ones"], writes=[("O", bank)])

                    info = emit_qk(0)
                    for si in range(len(steps)):
                        nxt = emit_qk(si + 1) if si + 1 < len(steps) else None
                        emit_pv(info)
                        info = nxt
                        if deferred:
                            deferred.pop(0)()
                    hp = h % 2
                    P.op("act", lambda e, hp=hp: e.activation(out=Ocp[:, hp, 0:2, :], in_=O_ps[:, 0:2, 0:387], func=AF.Copy),
                         reads=[("O", 0), ("O", 1)], writes=[("Ocp", hp, 0)])
                    P.op("dve", lambda e, hp=hp: e.tensor_copy(out=Ocp[:, hp, 2:3, 0:258], in_=O_ps[:, 2:3, 0:258]),
                         reads=[("O", 2)], writes=[("Ocp", hp, 1)])
                    def finish_c(c, h=h, hp=hp):
                        o0 = Ocp[:, hp, c // 3, (c % 3) * 129:(c % 3) * 129 + 129]
                        o1 = Ocp[:, hp, (4 + c) // 3, ((4 + c) % 3) * 129:((4 + c) % 3) * 129 + 129]
                        k0 = ("Ocp", hp, 0)
                        k1 = ("Ocp", hp, 0) if (4 + c) // 3 < 2 else ("Ocp", hp, 1)
                        r0, kr0 = stat()
                        r1, kr1 = stat()
                        P.op("dve", lambda e, o0=o0, r0=r0: e.reciprocal(out=r0, in_=o0[:, 128:129]), reads=[k0], writes=[kr0])
                        P.op("dve", lambda e, o1=o1, r1=r1: e.reciprocal(out=r1, in_=o1[:, 128:129]), reads=[k1], writes=[kr1])
                        r1n, kr1n = stat()
                        P.op("dve", lambda e, r1=r1, r1n=r1n: e.tensor_tensor(out=r1n, in0=r1, in1=nlam, op=ALU.mult),
                             reads=[kr1, "nlam"], writes=[kr1n])
                        ds = c % 2
                        P.op("dve", lambda e, o0=o0, r0=r0, ds=ds: e.tensor_scalar(out=o0n[:, ds, :], in0=o0[:, 0:128], scalar1=r0, scalar2=None, op0=ALU.mult),
                             reads=[k0, kr0], writes=[("o0n", ds)])
                        P.op("dve", lambda e, o1=o1, r1n=r1n, ds=ds: e.scalar_tensor_tensor(
                            out=dd[:, ds, :], in0=o1[:, 0:128], scalar=r1n, in1=o0n[:, ds, :], op0=ALU.mult, op1=ALU.add),
                            reads=[k1, kr1n, ("o0n", ds)], writes=[("dd", ds)])
                        ss, kss = stat()
                        P.op("act", lambda e, ds=ds, ss=ss: e.activation(out=junk2[:], in_=dd[:, ds, :], func=AF.Square, accum_out=ss),
                             reads=[("dd", ds)], writes=["junk2", kss])
                        ln2, kln2 = stat()
                        P.op("act", lambda e, ss=ss, ln2=ln2: e.activation(out=ln2, in_=ss, func=AF.Ln, scale=1.0 / 128, bias=EPS),
                             reads=[kss], writes=[kln2])
                        rs2, krs2 = stat()
                        P.op("act", lambda e, ln2=ln2, rs2=rs2: e.activation(out=rs2, in_=ln2, func=AF.Exp, scale=-0.5),
                             reads=[kln2], writes=[krs2])
                        P.op("dve", lambda e, ds=ds, rs2=rs2, c=c, h=h: e.tensor_scalar(
                            out=atm[:, c, h * 128:(h + 1) * 128], in0=dd[:, ds, :], scalar1=rs2, scalar2=None, op0=ALU.mult),
                            reads=[("dd", ds), krs2], writes=[("atm", c, h)])
                    for c in range(4):
                        deferred.append(lambda c=c, f=finish_c: f(c))
                while deferred:
                    deferred.pop(0)()
                if STOP[0] <= 4:
                    continue
                for c in range(4):
                    for a in range(4):
                        P.op("pe", lambda e, c=c, a=a: e.transpose(out=T_ps[:, a * 128:(a + 1) * 128], in_=atm[:, c, a * 128:(a + 1) * 128], identity=ident),
                             reads=[("atm", c, a), "cst"], writes=["T_ps"])
                    P.op("dve", lambda e, us=us, c=c: e.tensor_copy(
                        out=attnT[:, us, :, c * 128:(c + 1) * 128], in_=T_ps[:, 0:512].rearrange("p (a t) -> p a t", a=4)),
                        reads=["T_ps"], writes=[("attnT", us, c)])
                P.dma("sp", lambda e, us=us, c0=gi * 512: e.dma_start(
                    out=attnT_d.rearrange("(a p) t -> p a t", p=128)[:, :, c0:c0 + 512], in_=attnT[:, us, :, :]),
                    reads=[("attnT", us, c) for c in range(4)])
            P.flush()


        if 2 in phases:
          with ExitStack() as st:
            def sb(n, s_, d):
                return st.enter_context(nc.sbuf_tensor(n, s_, d))

            def ps(n, s_, d):
                return st.enter_context(nc.psum_tensor(n, s_, d))

            cst_f = sb("cst_f2", [128, 4, 128], F32)
            cst = sb("cst2", [128, 4, 128], BF16)
            onesC = sb("onesC", [128, 128], F32)
            wcg = sb("wcg", [128, 8, 3072], BF16)
            wao = sb("wao", [128, 4, 1024], BF16)
            wco = sb("wco", [128, 4, 1024], BF16)
            wo = sb("wo", [128, 8, 1024], BF16)
            wrt = sb("wrt", [128, 8, 36], F32)
            uT = sb("uT2", [128, 8, 512], BF16)
            attnT = sb("attnT2", [128, 4, 512], BF16)
            xt = sb("xt2", [128, 4, 1024], F32)
            glu = sb("glu", [128, 4, 542], BF16)
            dgr = sb("dgr", [128, 16, 128], BF16)
            dgc = [0]
            cacc = sb("cacc", [128, 4, 512], F32)
            sqf = sb("sqf", [128, 2, 512], F32)
            mean_sb = sb("mean_sb", [128, 512], F32)
            tmp2 = sqf
            cT = sb("cT", [128, 4, 512], BF16)
            sg = sb("sg", [128, 2, 512], F32)
            sga = sb("sga", [128, 512], F32)
            sgbs = sb("sgbs", [128, 8, 512], BF16)
            t1s = sb("t1s", [128, 8, 512], BF16)
            t2 = sb("t2", [128, 512], F32)
            mT = sb("mT", [128, 8, 512], BF16)
            h1t = sb("h1t", [128, 2, 1024], F32)
            tn = sb("tn", [128, 1024], F32)
            tb = sb("tb", [128, 4, 1024], BF16)
            L4 = sb("L4", [128, 4, 36], F32)
            gmax4 = sb("gmax4", [128, 4], F32)
            ohg4 = sb("ohg4", [128, 4, 4], F32)
            ge4 = sb("ge4", [128, 4, 4], F32)
            gsum4 = sb("gsum4", [128, 4], F32)
            prod4 = sb("prod4", [128, 4, 4, 8], F32)
            els4 = sb("els4", [128, 4, 8], F32)
            top84 = sb("top84", [128, 4, 8], F32)
            m124 = sb("m124", [128, 2, 4, 8], F32)
            d214 = sb("d214", [128, 4], F32)
            den4 = sb("den4", [128, 4], F32)
            s124 = sb("s124", [128, 2, 4, 32], F32)
            sel4 = sb("sel4", [128, 4, 32], BF16)
            dest4 = sb("dest4", [128, 4, 32], F32)
            tmp324 = sb("tmp324", [128, 4, 32], F32)
            rinfo4 = sb("rinfo42", [128, 4, 4], F32)
            idx4 = sb("idx42", [128, 4, 2], I32)
            tT = sb("tT", [128, 8, 128], F32)
            gffn = sb("gffn", [128, 1024], F32)
            bci = sb("bci", [128, 8], F32)
            bg = sb("bg", [128, 16], F32)
            cvb = sb("cvb", [128, 4], F32)
            lng = sb("lng", [128, 4], F32)
            lnbb = sb("lnbb", [128, 4], F32)
            bco = sb("bco", [128, 8], F32)
            cw = sb("cw", [128, 4, 31], F32)
            sln = sb("sln", [128, 1], F32)
            brt = sb("brt", [128, 36], F32)
            base_bc = sb("base_bc", [128, 32], F32)
            ones_bf = sb("ones_bf", [128, 128], BF16)
            rs_ = sb("rs_", [128, 64], F32)

            R_ps = ps("R_ps2", [128, 6, 512], F32)
            TF_ps = ps("TF_ps", [128, 1024], F32)
            rrot = [0]

            def rbank():
                i = rrot[0] % 6
                rrot[0] += 1
                return i

            stc = [0]

            def stat():
                i = stc[0] % 56
                stc[0] += 1
                return rs_[:, i:i + 1], ("rs_", i)

            P.dma("sp", lambda e: e.dma_start(out=cst_f[:], in_=consts[:, :, :]), writes=["cst_f"])
            P.op("dve", lambda e: e.tensor_copy(out=cst[:], in_=cst_f[:]), reads=["cst_f"], writes=["cst"])
            P.op("pool", lambda e: e.memset(onesC[:], 1.0 / 512), writes=["onesC"])
            P.op("pool", lambda e: e.memset(ones_bf[:], 1.0), writes=["ones_bf"])
            P.dma("sp", lambda e: e.dma_start(out=gffn[:], in_=bcast_rows(vec["norm_ffn"], 128, 1024)), writes=["gffn"])
            P.dma("sp", lambda e: e.dma_start(out=brt[:], in_=bcast_rows(vec["b_rt"], 128, 36)), writes=["brt"])
            P.dma("sp", lambda e: e.dma_start(out=base_bc[:], in_=bcast_rows(capoff, 128, 32)), writes=["base_bc"])
            P.dma("sp", lambda e: e.dma_start(out=sln[:], in_=col_ap(vec["subln"], 128)), writes=["sln"])
            P.op("dve", lambda e: e.tensor_scalar(out=sln[:], in0=sln[:], scalar1=1.0 - LAMBDA_INIT, scalar2=None, op0=ALU.mult),
                 reads=["sln"], writes=["sln"])

            def colvec(dst, name, nchunk):
                P.dma("sp", lambda e: e.dma_start(out=dst[:, 0:nchunk], in_=bass.AP(vec[name].tensor, 0, [[1, 128], [128, nchunk]])),
                      writes=[name])
            with nc.allow_non_contiguous_dma(reason="tiny per-channel bias columns"):
                pass
            for dst, name, nch in [(bci, "b_conv_in", 8), (bg, "b_gate", 16), (cvb, "conv_b", 4), (lng, "conv_ln_g", 4),
                                   (lnbb, "conv_ln_b", 4), (bco, "b_conv_out", 8)]:
                for c in range(nch):
                    P.dma("sp", lambda e, dst=dst, name=name, c=c: e.dma_start(out=dst[:, c:c + 1], in_=col_ap(vec[name], 128, c * 128)),
                          writes=[(name, c)])
            BIAS = [(n, c) for _, n, nch in [(0, "b_conv_in", 8), (0, "b_gate", 16), (0, "conv_b", 4), (0, "conv_ln_g", 4),
                                             (0, "conv_ln_b", 4), (0, "b_conv_out", 8)] for c in range(nch)]
            for cc in range(4):
                for kk in range(31):
                    pass
            for cc in range(4):
                P.dma("sp", lambda e, cc=cc: e.dma_start(out=cw[:, cc, :], in_=conv_w[cc * 128:(cc + 1) * 128, :]),
                      writes=[("cw", cc)])
            P.dma("sp", lambda e: e.dma_start(out=wrt[:], in_=w_rt.rearrange("(k p) n -> p k n", p=128)), writes=["wrt"])
            wl = [0]

            def load_cast(dst_fn, src_fn, key, eng="pool", scale_col=None):
                sl = wl[0] % 4
                wl[0] += 1
                P.dma("sp", lambda e, sl=sl: e.dma_start(out=xt[:, sl, :], in_=src_fn()), writes=[("xt", sl)])
                if scale_col is None:
                    P.op(eng, lambda e, sl=sl: e.tensor_copy(out=dst_fn(), in_=xt[:, sl, :]), reads=[("xt", sl)], writes=[key])
                else:
                    P.op("dve", lambda e, sl=sl: e.tensor_scalar(out=dst_fn(), in0=xt[:, sl, :], scalar1=scale_col, scalar2=None, op0=ALU.mult),
                         reads=[("xt", sl), "sln"], writes=[key])
            for k in range(8):
                for c3 in range(3):
                    load_cast(lambda k=k, c3=c3: wcg[:, k, c3 * 1024:(c3 + 1) * 1024],
                              lambda k=k, c3=c3: w_in[k * 128:(k + 1) * 128, 1536 + c3 * 1024:1536 + (c3 + 1) * 1024],
                              ("wcg", k, c3), eng="pool" if (k + c3) % 2 else "dve")
                load_cast(lambda k=k: wo[:, k, :], lambda k=k: w_o[k * 128:(k + 1) * 128, :], ("wo", k), eng="pool")
            for a in range(4):
                load_cast(lambda a=a: wao[:, a, :], lambda a=a: w_ao[a * 128:(a + 1) * 128, :], ("wao", a), scale_col=sln[:, 0:1])
                load_cast(lambda a=a: wco[:, a, :], lambda a=a: w_co[a * 128:(a + 1) * 128, :], ("wco", a), eng="pool")
            WCG = [("wcg", k, c3) for k in range(8) for c3 in range(3)]

            pa_ = [0]
            pb_ = [0]

            def rbankA():
                i = pa_[0] % 3
                pa_[0] += 1
                return i

            def rbankB():
                i = 3 + pb_[0] % 3
                pb_[0] += 1
                return i

            def zip_run2(ga, gb, ratio=1):
                a_done = ga is None
                b_done = gb is None
                while not (a_done and b_done):
                    if not a_done:
                        try:
                            next(ga)
                        except StopIteration:
                            a_done = True
                    for _ in range(ratio):
                        if not b_done:
                            try:
                                next(gb)
                            except StopIteration:
                                b_done = True

            def p2_front_a(gi):
                b = gi // GPS
                G = gi % GPS
                s0 = G * 512
                c0 = gi * 512
                for cc in range(4):
                    ra = rbankA()
                    for k in range(8):
                        yield P.op("pe", lambda e, cc=cc, k=k, ra=ra: e.matmul(R_ps[:, ra, :], lhsT=wcg[:, k, cc * 128:(cc + 1) * 128], rhs=uT[:, k, :],
                                                                       start=(k == 0), stop=(k == 7)), reads=["uT"] + WCG, writes=[("R", ra)])
                    rg = rbankA()
                    for k in range(8):
                        yield P.op("pe", lambda e, cc=cc, k=k, rg=rg: e.matmul(R_ps[:, rg, :], lhsT=wcg[:, k, 512 + cc * 128:512 + (cc + 1) * 128], rhs=uT[:, k, :],
                                                                       start=(k == 0), stop=(k == 7)), reads=["uT"] + WCG, writes=[("R", rg)])
                    s2 = cc % 2
                    yield P.op("act", lambda e, rg=rg, s2=s2, cc=cc: e.activation(out=sg[:, s2, :], in_=R_ps[:, rg, :], func=AF.Sigmoid, bias=bci[:, 4 + cc:5 + cc]),
                         reads=[("R", rg), ("b_conv_in", 4 + cc)], writes=[("sg", s2)])
                    if G == 0:
                        yield P.op("pool", lambda e, cc=cc: e.memset(glu[:, cc, 0:30], 0.0), writes=[("glu", cc)])
                    yield P.op("dve", lambda e, ra=ra, s2=s2, cc=cc: e.scalar_tensor_tensor(
                        out=glu[:, cc, 30:542], in0=R_ps[:, ra, :], scalar=bci[:, cc:cc + 1], in1=sg[:, s2, :], op0=ALU.add, op1=ALU.mult),
                        reads=[("R", ra), ("sg", s2), ("b_conv_in", cc), ("glu", cc)], writes=[("glu", cc)])

            def p2_front_b(gi):
                b = gi // GPS
                G = gi % GPS
                s0 = G * 512
                c0 = gi * 512
                def gate_front(oc):
                    rya = rbankA()
                    for a in range(4):
                        yield P.op("pe", lambda e, a=a, oc=oc, rya=rya: e.matmul(R_ps[:, rya, :], lhsT=wao[:, a, oc * 128:(oc + 1) * 128], rhs=attnT[:, a, :],
                                                                         start=(a == 0), stop=(a == 3)), reads=["attnT", ("wao", a)], writes=[("R", rya)])
                    rga = rbankA()
                    for k in range(8):
                        yield P.op("pe", lambda e, k=k, oc=oc, rga=rga: e.matmul(R_ps[:, rga, :], lhsT=wcg[:, k, 1024 + oc * 128:1024 + (oc + 1) * 128], rhs=uT[:, k, :],
                                                                         start=(k == 0), stop=(k == 7)), reads=["uT"] + WCG, writes=[("R", rga)])
                    rgb = rbankA()
                    for k in range(8):
                        yield P.op("pe", lambda e, k=k, oc=oc, rgb=rgb: e.matmul(R_ps[:, rgb, :], lhsT=wcg[:, k, 2048 + oc * 128:2048 + (oc + 1) * 128], rhs=uT[:, k, :],
                                                                         start=(k == 0), stop=(k == 7)), reads=["uT"] + WCG, writes=[("R", rgb)])
                    yield P.op("act", lambda e, oc=oc, rga=rga: e.activation(out=sga[:], in_=R_ps[:, rga, :], func=AF.Sigmoid, bias=bg[:, oc:oc + 1]),
                         reads=[("R", rga), ("b_gate", oc)], writes=["sga"])
                    yield P.op("act", lambda e, oc=oc, rgb=rgb: e.activation(out=sgbs[:, oc, :], in_=R_ps[:, rgb, :], func=AF.Sigmoid, bias=bg[:, 8 + oc:9 + oc]),
                         reads=[("R", rgb), ("b_gate", 8 + oc)], writes=[("sgbs", oc)])
                    yield P.op("dve", lambda e, rya=rya, oc=oc: e.tensor_tensor(out=t1s[:, oc, :], in0=R_ps[:, rya, :], in1=sga[:], op=ALU.mult),
                         reads=[("R", rya), "sga"], writes=[("t1s", oc)])

                nfront = 0
                for cc in range(4):
                    rcv = rbankA()
                    for kk in range(31):
                        ds_ = dgc[0] % 16
                        dgc[0] += 1
                        yield P.op("act", lambda e, ds_=ds_, cc=cc, kk=kk: e.activation(out=dgr[:, ds_, :], in_=cst[:, 0, :], func=AF.Copy, scale=cw[:, cc, kk:kk + 1]),
                             reads=["cst", ("cw", cc)], writes=[("dgr", ds_)])
                        yield P.op("pe", lambda e, ds_=ds_, cc=cc, kk=kk, rcv=rcv: e.matmul(R_ps[:, rcv, :], lhsT=dgr[:, ds_, :], rhs=glu[:, cc, kk:kk + 512],
                                                                                   start=(kk == 0), stop=(kk == 30)),
                             reads=[("dgr", ds_), ("glu", cc)], writes=[("R", rcv)])
                    yield P.op("dve", lambda e, cc=cc, rcv=rcv: e.tensor_scalar(out=cacc[:, cc, :], in0=R_ps[:, rcv, :], scalar1=cvb[:, cc:cc + 1], scalar2=None, op0=ALU.add),
                         reads=[("R", rcv), ("conv_b", cc)], writes=[("cacc", cc)])
                    for _ in range(2):
                        yield from gate_front(nfront)
                        nfront += 1
                if gi + 1 < NG:
                    c1 = (gi + 1) * 512
                    yield P.dma("sp", lambda e, c1=c1: e.dma_start(out=uT[:], in_=uT_d.rearrange("(k p) t -> p k t", p=128)[:, :, c1:c1 + 512]),
                          writes=["uT"])
                    yield P.dma("sp", lambda e, c1=c1: e.dma_start(out=attnT[:], in_=attnT_d.rearrange("(a p) t -> p a t", p=128)[:, :, c1:c1 + 512]),
                          writes=["attnT"])
                for cc in range(4):
                    yield P.op("pool", lambda e, cc=cc: e.tensor_copy(out=glu[:, cc, 0:30], in_=glu[:, cc, 512:542]),
                         reads=[("glu", cc)], writes=[("glu", cc)])

            def p2_mid(gi):
                b = gi // GPS
                G = gi % GPS
                s0 = G * 512
                c0 = gi * 512
                rm = rbankB()
                rv = rbankB()
                for cc in range(4):
                    yield P.op("pe", lambda e, cc=cc, rm=rm: e.matmul(R_ps[:, rm, :], lhsT=onesC[:], rhs=cacc[:, cc, :], start=(cc == 0), stop=(cc == 3)),
                         reads=[("cacc", cc), "onesC"], writes=[("R", rm)])
                for cc in range(4):
                    s2 = cc % 2
                    yield P.op("act", lambda e, cc=cc, s2=s2: e.activation(out=sqf[:, s2, :], in_=cacc[:, cc, :], func=AF.Square),
                         reads=[("cacc", cc)], writes=[("sqf", s2)])
                    yield P.op("pe", lambda e, cc=cc, rv=rv, s2=s2: e.matmul(R_ps[:, rv, :], lhsT=onesC[:], rhs=sqf[:, s2, :], start=(cc == 0), stop=(cc == 3)),
                         reads=[("sqf", s2), "onesC"], writes=[("R", rv)])
                yield P.op("act", lambda e, rm=rm: e.activation(out=mean_sb[:], in_=R_ps[:, rm, :], func=AF.Copy), reads=[("R", rm)], writes=["mean_sb"])
                yield P.op("dve", lambda e: e.tensor_tensor(out=t2[:], in0=mean_sb[:], in1=mean_sb[:], op=ALU.mult), reads=["mean_sb"], writes=["t2"])
                yield P.op("dve", lambda e, rv=rv: e.tensor_tensor(out=t2[:], in0=R_ps[:, rv, :], in1=t2[:], op=ALU.subtract),
                     reads=[("R", rv), "t2"], writes=["t2"])
                yield P.op("act", lambda e: e.activation(out=t2[:], in_=t2[:], func=AF.Ln, bias=EPS), reads=["t2"], writes=["t2"])
                yield P.op("act", lambda e: e.activation(out=sga[:], in_=t2[:], func=AF.Exp, scale=-0.5), reads=["t2"], writes=["sga"])
                for cc in range(4):
                    s2 = cc % 2
                    yield P.op("dve", lambda e, cc=cc, s2=s2: e.tensor_tensor(out=tmp2[:, s2, :], in0=cacc[:, cc, :], in1=mean_sb[:], op=ALU.subtract),
                         reads=[("cacc", cc), "mean_sb"], writes=[("sqf", s2)])
                    yield P.op("dve", lambda e, s2=s2: e.tensor_tensor(out=tmp2[:, s2, :], in0=tmp2[:, s2, :], in1=sga[:], op=ALU.mult),
                         reads=[("sqf", s2), "sga"], writes=[("sqf", s2)])
                    yield P.op("act", lambda e, cc=cc, s2=s2: e.activation(out=cT[:, cc, :], in_=tmp2[:, s2, :], func=AF.Silu,
                                                                     scale=lng[:, cc:cc + 1], bias=lnbb[:, cc:cc + 1]),
                         reads=[("sqf", s2), ("conv_ln_g", cc), ("conv_ln_b", cc)], writes=[("cT", cc)])
                for oc in range(8):
                    ryb = rbankB()
                    for a in range(4):
                        yield P.op("pe", lambda e, a=a, oc=oc, ryb=ryb: e.matmul(R_ps[:, ryb, :], lhsT=wco[:, a, oc * 128:(oc + 1) * 128], rhs=cT[:, a, :],
                                                                         start=(a == 0), stop=(a == 3)), reads=[("cT", a), ("wco", a)], writes=[("R", ryb)])
                    yield P.op("dve", lambda e, ryb=ryb, oc=oc: e.scalar_tensor_tensor(out=t2[:], in0=R_ps[:, ryb, :], scalar=bco[:, oc:oc + 1], in1=sgbs[:, oc, :],
                                                                                op0=ALU.add, op1=ALU.mult),
                         reads=[("R", ryb), ("sgbs", oc), ("b_conv_out", oc)], writes=["t2"])
                    yield P.op("dve", lambda e, oc=oc: e.tensor_tensor(out=mT[:, oc, :], in0=t1s[:, oc, :], in1=t2[:], op=ALU.add),
                         reads=[("t1s", oc), "t2"], writes=[("mT", oc)])
                MT = [("mT", oc) for oc in range(8)]

            def p2_tail(gi):
                b = gi // GPS
                G = gi % GPS
                s0 = G * 512
                c0 = gi * 512
                MT = [("mT", oc) for oc in range(8)]
                for j in range(4):
                    tile_i = gi * 4 + j
                    for n in range(2):
                        rh = rbankB()
                        for oc in range(8):
                            yield P.op("pe", lambda e, j=j, n=n, oc=oc, rh=rh: e.matmul(R_ps[:, rh, :], lhsT=mT[:, oc, j * 128:(j + 1) * 128], rhs=wo[:, oc, n * 512:(n + 1) * 512],
                                                                                 start=(oc == 0), stop=(oc == 7)), reads=MT + [("wo", oc)], writes=[("R", rh)])
                        yield P.op("dve", lambda e, j=j, n=n, rh=rh: e.tensor_tensor(out=h1t[:, j % 2, n * 512:(n + 1) * 512], in0=R_ps[:, rh, :], in1=xt[:, j, n * 512:(n + 1) * 512], op=ALU.add),
                             reads=[("R", rh), ("xt", j)], writes=[("h1t", j % 2, n)])
                    yield P.dma("sp", lambda e, r0=tile_i * 128, j=j: e.dma_start(out=h1_d[r0:r0 + 128, :], in_=h1t[:, j % 2, :]), reads=[("h1t", j % 2, 0), ("h1t", j % 2, 1)])
                    ssq, kssq = stat()
                    yield P.op("act", lambda e, ssq=ssq, j=j: e.activation(out=tb[:, j, :], in_=h1t[:, j % 2, :], func=AF.Square, accum_out=ssq),
                         reads=[("h1t", j % 2, 0), ("h1t", j % 2, 1)], writes=[("tb", j), kssq])
                    lnv, klnv = stat()
                    yield P.op("act", lambda e, ssq=ssq, lnv=lnv: e.activation(out=lnv, in_=ssq, func=AF.Ln, scale=1.0 / 1024, bias=EPS), reads=[kssq], writes=[klnv])
                    rstd, krstd = stat()
                    yield P.op("act", lambda e, lnv=lnv, rstd=rstd: e.activation(out=rstd, in_=lnv, func=AF.Exp, scale=-0.5), reads=[klnv], writes=[krstd])
                    yield P.op("dve", lambda e, rstd=rstd, j=j: e.scalar_tensor_tensor(out=tn[:], in0=h1t[:, j % 2, :], scalar=rstd, in1=gffn[:], op0=ALU.mult, op1=ALU.mult),
                         reads=[("h1t", j % 2, 0), ("h1t", j % 2, 1), krstd, "gffn"], writes=["tn"])
                    yield P.op("act", lambda e, j=j: e.activation(out=tb[:, j, :], in_=tn[:], func=AF.Copy), reads=["tn"], writes=[("tb", j)])
                    for k in range(8):
                        yield P.op("pe", lambda e, k=k: e.transpose(out=TF_ps[:, k * 128:(k + 1) * 128], in_=tn[:, k * 128:(k + 1) * 128], identity=cst_f[:, 0, :]),
                             reads=["tn", "cst_f"], writes=["TF_ps"])
                    yield P.op("act", lambda e: e.activation(out=tT[:], in_=TF_ps[:].rearrange("p (k t) -> p k t", k=8), func=AF.Copy), reads=["TF_ps"], writes=["tT"])
                    rl = rbankB()
                    for k in range(8):
                        yield P.op("pe", lambda e, k=k, rl=rl: e.matmul(R_ps[:, rl, 0:36], lhsT=tT[:, k, :], rhs=wrt[:, k, :], start=(k == 0), stop=(k == 7)),
                             reads=["tT", "wrt"], writes=[("R", rl)])
                    yield P.op("dve", lambda e, rl=rl, j=j: e.tensor_tensor(out=L4[:, j, :], in0=R_ps[:, rl, 0:36], in1=brt[:], op=ALU.add), reads=[("R", rl), "brt"], writes=[("L4", j)])

            def p2_route(gi):
                b = gi // GPS
                G = gi % GPS
                s0 = G * 512
                c0 = gi * 512
                KL = [("L4", j) for j in range(4)]
                Lg4 = L4[:, :, 0:4]
                Lx = L4[:, :, 4:36].rearrange("p t (g j) -> p t g j", g=4)

                def bc_last(ap2, n):
                    return bass.AP(ap2.tensor, ap2.offset, [list(ap2.ap[0]), list(ap2.ap[1]), [0, n]])

                def col(buf3, c):
                    a = buf3[:, :, c:c + 1]
                    return bass.AP(a.tensor, a.offset, [list(a.ap[0]), list(a.ap[1])])
                P.op("dve", lambda e: e.tensor_reduce(out=gmax4[:], in_=Lg4, axis=AX.X, op=ALU.max), reads=KL, writes=["gmax4"])
                P.op("dve", lambda e: e.tensor_tensor(out=ohg4[:], in0=Lg4, in1=bc_last(gmax4[:], 4), op=ALU.is_equal), reads=KL + ["gmax4"], writes=["ohg4"])
                P.op("dve", lambda e: e.tensor_tensor(out=ge4[:], in0=Lg4, in1=bc_last(gmax4[:], 4), op=ALU.subtract), reads=KL + ["gmax4"], writes=["ge4"])
                P.op("act", lambda e: e.activation(out=ge4[:], in_=ge4[:], func=AF.Exp), reads=["ge4"], writes=["ge4"])
                P.op("dve", lambda e: e.tensor_reduce(out=gsum4[:], in_=ge4[:], axis=AX.X, op=ALU.add), reads=["ge4"], writes=["gsum4"])
                P.op("dve", lambda e: e.reciprocal(out=gsum4[:], in_=gsum4[:]), reads=["gsum4"], writes=["gsum4"])
                ohg_bj = bass.AP(ohg4[:].tensor, ohg4[:].offset, [list(ohg4[:].ap[0]), [4, 4], [1, 4], [0, 8]])
                P.op("dve", lambda e: e.tensor_tensor(out=prod4[:], in0=Lx, in1=ohg_bj, op=ALU.mult), reads=KL + ["ohg4"], writes=["prod4"])
                P.op("dve", lambda e: e.tensor_reduce(out=els4[:], in_=prod4[:].rearrange("p t g j -> p t j g"), axis=AX.X, op=ALU.add), reads=["prod4"], writes=["els4"])
                for t in range(4):
                    P.op("dve", lambda e, t=t: e.max(out=top84[:, t, :], in_=els4[:, t, :]), reads=["els4"], writes=[("top84", t)])
                T8 = [("top84", t) for t in range(4)]
                for i in range(2):
                    P.op("dve", lambda e, i=i: e.tensor_tensor(out=m124[:, i, :, :], in0=els4[:], in1=bc_last(col(top84, i), 8), op=ALU.is_equal),
                         reads=["els4"] + T8, writes=[("m124", i)])
                P.op("dve", lambda e: e.tensor_tensor(out=d214[:], in0=col(top84, 1), in1=col(top84, 0), op=ALU.subtract), reads=T8, writes=["d214"])
                P.op("act", lambda e: e.activation(out=d214[:], in_=d214[:], func=AF.Exp), reads=["d214"], writes=["d214"])
                P.op("dve", lambda e: e.tensor_scalar(out=den4[:], in0=d214[:], scalar1=1.0, scalar2=None, op0=ALU.add), reads=["d214"], writes=["den4"])
                P.op("dve", lambda e: e.reciprocal(out=den4[:], in_=den4[:]), reads=["den4"], writes=["den4"])
                P.op("dve", lambda e: e.tensor_tensor(out=col(rinfo4, 2), in0=gsum4[:], in1=den4[:], op=ALU.mult), reads=["gsum4", "den4", "rinfo4"], writes=["rinfo4"])
                P.op("dve", lambda e: e.tensor_tensor(out=col(rinfo4, 3), in0=col(rinfo4, 2), in1=d214[:], op=ALU.mult), reads=["rinfo4", "d214"], writes=["rinfo4"])
                for i in range(2):
                    mi = m124[:, i, :, :]
                    m_bg = bass.AP(mi.tensor, mi.offset, [list(mi.ap[0]), [8, 4], [0, 4], [1, 8]])
                    P.op("dve", lambda e, i=i, m_bg=m_bg: e.tensor_tensor(out=s124[:, i, :, :].rearrange("p t (g j) -> p t g j", g=4), in0=m_bg, in1=ohg_bj, op=ALU.mult),
                         reads=[("m124", i), "ohg4"], writes=[("s124", i)])
                P.op("dve", lambda e: e.tensor_tensor(out=sel4[:], in0=s124[:, 0, :, :], in1=s124[:, 1, :, :], op=ALU.add), reads=[("s124", 0), ("s124", 1)], writes=["sel4"])
                rc = rbankB()
                for t in range(4):
                    P.op("pe", lambda e, rc=rc, t=t: e.matmul(R_ps[:, rc, t * 32:(t + 1) * 32], lhsT=cst[:, 2, :], rhs=sel4[:, t, :], start=True, stop=(t == 0), skip_group_check=True),
                         reads=["sel4", "cst"], writes=[("R", rc)])
                    for tq in range(t):
                        P.op("pe", lambda e, rc=rc, t=t, tq=tq: e.matmul(R_ps[:, rc, t * 32:(t + 1) * 32], lhsT=ones_bf[:], rhs=sel4[:, tq, :], start=False, stop=(tq == t - 1), skip_group_check=True),
                             reads=["sel4", "ones_bf"], writes=[("R", rc)])
                rt_ = rbankB()
                for t in range(4):
                    P.op("pe", lambda e, rt_=rt_, t=t: e.matmul(R_ps[:, rt_, 0:32], lhsT=ones_bf[:], rhs=sel4[:, t, :], start=(t == 0), stop=(t == 3)),
                         reads=["sel4", "ones_bf"], writes=[("R", rt_)])
                base_b = bass.AP(base_bc[:].tensor, base_bc[:].offset, [list(base_bc[:].ap[0]), [0, 4], [1, 32]])
                P.op("dve", lambda e, rc=rc: e.tensor_tensor(out=dest4[:], in0=R_ps[:, rc, 0:128].rearrange("p (t e) -> p t e", t=4), in1=base_b, op=ALU.add),
                     reads=[("R", rc), "base_bc"], writes=["dest4"])
                P.op("dve", lambda e, rt_=rt_: e.tensor_tensor(out=base_bc[:], in0=R_ps[:, rt_, 0:32], in1=base_bc[:], op=ALU.add), reads=[("R", rt_), "base_bc"], writes=["base_bc"])
                for i in range(2):
                    P.op("dve", lambda e, i=i: e.tensor_tensor(out=tmp324[:], in0=s124[:, i, :, :], in1=dest4[:], op=ALU.mult), reads=[("s124", i), "dest4"], writes=["tmp324"])
                    P.op("dve", lambda e, i=i: e.tensor_reduce(out=col(rinfo4, i), in_=tmp324[:], axis=AX.X, op=ALU.add), reads=["tmp324", "rinfo4"], writes=["rinfo4"])
                P.op("dve", lambda e: e.tensor_copy(out=idx4[:], in_=rinfo4[:, :, 0:2]), reads=["rinfo4"], writes=["idx4"])
                for t in range(4):
                    for i in range(2):
                        P.dma("pool", lambda e, t=t, i=i: e.indirect_dma_start(
                            out=xs_d[:, :], out_offset=bass.IndirectOffsetOnAxis(ap=idx4[:, t, i:i + 1], axis=0), in_=tb[:, t, :], in_offset=None),
                            reads=["idx4", ("tb", t)], writes=["xs_d"])
                P.dma("sp", lambda e, r0=gi * 512: e.dma_start(out=rt_d[r0:r0 + 512, :].rearrange("(t p) c -> p t c", p=128), in_=rinfo4[:]), reads=["rinfo4"])

            P.dma("sp", lambda e: e.dma_start(out=uT[:], in_=uT_d.rearrange("(k p) t -> p k t", p=128)[:, :, 0:512]), writes=["uT"])
            P.dma("sp", lambda e: e.dma_start(out=attnT[:], in_=attnT_d.rearrange("(a p) t -> p a t", p=128)[:, :, 0:512]), writes=["attnT"])
            zip_run2(p2_front_a(0), None)
            zip_run2(p2_front_b(0), None)
            for gi in range(NG):
                b = gi // GPS
                s0 = (gi % GPS) * 512
                for j in range(4):
                    P.dma("sp", lambda e, j=j, b=b, r0=s0 + j * 128: e.dma_start(out=xt[:, j, :], in_=x[b, r0:r0 + 128, :]),
                          writes=[("xt", j)])
                zip_run2(p2_front_a(gi + 1) if gi + 1 < NG else None, p2_mid(gi), 1)
                zip_run2(p2_tail(gi), p2_front_b(gi + 1) if gi + 1 < NG else None, 3)
                p2_route(gi)
            P.flush()


        if 3 in phases:
          with ExitStack() as st:
            def sb(n, s_, d):
                return st.enter_context(nc.sbuf_tensor(n, s_, d))

            def ps(n, s_, d):
                return st.enter_context(nc.psum_tensor(n, s_, d))

            cst_f = sb("cst_f3", [128, 4, 128], F32)
            cst = sb("cst3", [128, 4, 128], BF16)
            wb1 = sb("wb1", [128, 2, 8, 512], BF16)
            wb3 = sb("wb3", [128, 2, 8, 512], BF16)
            wb2 = sb("wb2", [128, 2, 4, 1024], BF16)
            stage = sb("stage3", [128, 4, 1024], F32)
            stage2 = sb("stage3b", [128, 4, 1024], F32)
            xsb = sb("xsb", [128, 2, NBLK, 1024], BF16)
            XT = sb("XT", [128, 2, 8, CAP], BF16)
            hT = sb("hT", [128, 4, CAP], BF16)
            sil = sb("sil", [128, 2, 512], F32)
            ysb = sb("ysb", [128, NBLK, 1024], BF16)
            R_ps = ps("R_ps3", [128, 6, 512], F32)
            T_ps = ps("T_ps3", [128, 2, 1024], BF16)
            rrot = [0]

            def rbank():
                i = rrot[0] % 6
                rrot[0] += 1
                return i

            P.dma("sp", lambda e: e.dma_start(out=cst_f[:], in_=consts[:, :, :]), writes=["cst_f"])
            P.op("dve", lambda e: e.tensor_copy(out=cst[:], in_=cst_f[:]), reads=["cst_f"], writes=["cst"])
            wl = [0]
            blkc = [0]

            def xs_dma(ex):
                eb = ex % 2
                P.dma("sp", lambda e, eb=eb, ex=ex: e.dma_start(
                    out=xsb[:, eb, :, :], in_=xs_d[ex * CAP:(ex + 1) * CAP, :].rearrange("(n p) c -> p n c", p=128)), writes=[("xsb", eb)])

            def load_w13(ex):
                eb = ex % 2
                for kk in range(4):
                    for (wsrc, wdst, nm) in ((w1, wb1, "wb1"), (w3, wb3, "wb3")):
                        sl = wl[0] % 4
                        wl[0] += 1
                        P.dma("sp", lambda e, sl=sl, wsrc=wsrc, ex=ex, kk=kk: e.dma_start(
                            out=stage[:, sl, :].rearrange("p (k f) -> p k f", k=2),
                            in_=wsrc[ex, kk * 256:(kk + 1) * 256, :].rearrange("(k p) f -> p k f", p=128)), writes=[("stage", sl)])
                        P.op("pool", lambda e, sl=sl, wdst=wdst, eb=eb, kk=kk: e.tensor_copy(
                            out=wdst[:, eb, 2 * kk:2 * kk + 2, :], in_=stage[:, sl, :].rearrange("p (k f) -> p k f", k=2)),
                            reads=[("stage", sl)], writes=[(nm, eb, kk)])

            def load_w2_dma(ex):
                for f in range(4):
                    P.dma("sp", lambda e, ex=ex, f=f: e.dma_start(out=stage2[:, f, :], in_=w2[ex, f * 128:(f + 1) * 128, :]), writes=[("stage2", f)])

            def cast_w2(ex, eng):
                eb = ex % 2
                for f in range(4):
                    if eng == "act":
                        P.op("act", lambda e, eb=eb, f=f: e.activation(out=wb2[:, eb, f, :], in_=stage2[:, f, :], func=AF.Copy),
                             reads=[("stage2", f)], writes=[("wb2", eb, f)])
                    else:
                        P.op("pool", lambda e, eb=eb, f=f: e.tensor_copy(out=wb2[:, eb, f, :], in_=stage2[:, f, :]),
                             reads=[("stage2", f)], writes=[("wb2", eb, f)])

            def zip_run3(ga, gb, ratio):
                a_done = ga is None
                b_done = gb is None
                while not (a_done and b_done):
                    if not a_done:
                        try:
                            next(ga)
                        except StopIteration:
                            a_done = True
                    for _ in range(ratio):
                        if not b_done:
                            try:
                                next(gb)
                            except StopIteration:
                                b_done = True

            def gather_T(ex):
                eb = ex % 2
                for blk in range(NBLK):
                    bs = blkc[0] % 2
                    blkc[0] += 1
                    for k in range(8):
                        yield P.op("pe", lambda e, bs=bs, k=k, blk=blk, eb=eb: e.transpose(out=T_ps[:, bs, k * 128:(k + 1) * 128], in_=xsb[:, eb, blk, k * 128:(k + 1) * 128], identity=cst[:, 0, :]),
                                   reads=[("xsb", eb), "cst"], writes=[("T_ps", bs)])
                    if blk % 2 == 0:
                        yield P.op("dve", lambda e, bs=bs, blk=blk, eb=eb: e.tensor_copy(out=XT[:, eb, :, blk * 128:(blk + 1) * 128], in_=T_ps[:, bs, :].rearrange("p (k t) -> p k t", k=8)),
                                   reads=[("T_ps", bs)], writes=[("XT", eb, blk)])
                    else:
                        yield P.op("act", lambda e, bs=bs, blk=blk, eb=eb: e.activation(out=XT[:, eb, :, blk * 128:(blk + 1) * 128], in_=T_ps[:, bs, :].rearrange("p (k t) -> p k t", k=8), func=AF.Copy),
                                   reads=[("T_ps", bs)], writes=[("XT", eb, blk)])

            def expert_compute(ex):
                eb = ex % 2
                W1 = [("wb1", eb, kk) for kk in range(4)]
                W3 = [("wb3", eb, kk) for kk in range(4)]
                W2 = [("wb2", eb, f) for f in range(4)]
                for nt in range(CAP // 512):
                    XTK = [("XT", eb, nt * 4 + i) for i in range(4)]
                    for f in range(4):
                        r1 = rbank()
                        for k in range(8):
                            yield P.op("pe", lambda e, f=f, k=k, nt=nt, r1=r1, eb=eb: e.matmul(R_ps[:, r1, :], lhsT=wb1[:, eb, k, f * 128:(f + 1) * 128], rhs=XT[:, eb, k, nt * 512:(nt + 1) * 512],
                                                                                        start=(k == 0), stop=(k == 7)), reads=W1 + XTK, writes=[("R", r1)])
                        r3 = rbank()
                        for k in range(8):
                            yield P.op("pe", lambda e, f=f, k=k, nt=nt, r3=r3, eb=eb: e.matmul(R_ps[:, r3, :], lhsT=wb3[:, eb, k, f * 128:(f + 1) * 128], rhs=XT[:, eb, k, nt * 512:(nt + 1) * 512],
                                                                                        start=(k == 0), stop=(k == 7)), reads=W3 + XTK, writes=[("R", r3)])
                        ss = f % 2
                        yield P.op("act", lambda e, r1=r1, ss=ss: e.activation(out=sil[:, ss, :], in_=R_ps[:, r1, :], func=AF.Silu), reads=[("R", r1)], writes=[("sil", ss)])
                        yield P.op("dve", lambda e, r3=r3, ss=ss, f=f, nt=nt: e.tensor_tensor(out=hT[:, f, nt * 512:(nt + 1) * 512], in0=R_ps[:, r3, :], in1=sil[:, ss, :], op=ALU.mult),
                                   reads=[("R", r3), ("sil", ss)], writes=[("hT", f, nt)])
                if ex + 1 < NE:
                    cast_w2(ex + 1, "act")
                for blk in range(NBLK):
                    for n in range(2):
                        ry = rbank()
                        for f in range(4):
                            yield P.op("pe", lambda e, f=f, n=n, blk=blk, ry=ry, eb=eb: e.matmul(R_ps[:, ry, :], lhsT=hT[:, f, blk * 128:(blk + 1) * 128], rhs=wb2[:, eb, f, n * 512:(n + 1) * 512],
                                                                                          start=(f == 0), stop=(f == 3)), reads=W2 + [("hT", f, blk // 4)], writes=[("R", ry)])
                        if n == 0:
                            yield P.op("act", lambda e, ry=ry, blk=blk, n=n: e.activation(out=ysb[:, blk, n * 512:(n + 1) * 512], in_=R_ps[:, ry, :], func=AF.Copy),
                                       reads=[("R", ry)], writes=[("ysb", blk, n)])
                        else:
                            yield P.op("dve", lambda e, ry=ry, blk=blk, n=n: e.tensor_copy(out=ysb[:, blk, n * 512:(n + 1) * 512], in_=R_ps[:, ry, :]),
                                       reads=[("R", ry)], writes=[("ysb", blk, n)])
                yield P.dma("sp", lambda e, ex=ex: e.dma_start(out=ys_d[ex * CAP:(ex + 1) * CAP, :].rearrange("(n p) c -> p n c", p=128), in_=ysb[:]),
                            reads=[("ysb", blk, n) for blk in range(NBLK) for n in range(2)])

            xs_dma(0)
            load_w13(0)
            load_w2_dma(0)
            cast_w2(0, "pool")
            xs_dma(1)
            zip_run3(gather_T(0), None, 1)
            for ex in range(NE):
                if ex + 1 < NE:
                    load_w13(ex + 1)
                    load_w2_dma(ex + 1)
                if ex + 2 < NE:
                    xs_dma(ex + 2)
                zip_run3(gather_T(ex + 1) if ex + 1 < NE else None, expert_compute(ex), 4)
            P.flush()

        if 4 in phases:
          with ExitStack() as st:
            def sb(n, s_, d):
                return st.enter_context(nc.sbuf_tensor(n, s_, d))

            def ps(n, s_, d):
                return st.enter_context(nc.psum_tensor(n, s_, d))

            cst_f = sb("cst_f4", [128, 4, 128], F32)
            cst = sb("cst4", [128, 4, 128], BF16)
            wpg = sb("wpg", [128, 8, 1024], BF16)
            wpp = sb("wpp", [128, 2, 1024], BF16)
            stage = sb("stage4", [128, 2, 1024], F32)
            gple = sb("gple", [128, 1024], F32)
            bple = sb("bple", [128, 1024], F32)
            h1t = sb("h1t4", [128, 3, 1024], F32)
            yA = sb("yA", [128, 3, 1024], BF16)
            yB = sb("yB", [128, 3, 1024], BF16)
            h2 = sb("h2", [128, 2, 1024], F32)
            hn = sb("hn", [128, 2, 1024], BF16)
            hnT = sb("hnT", [128, 2, 8, 128], BF16)
            pt = sb("pt", [128, 3, 256], F32)
            pb = sb("pb", [128, 2, 256], BF16)
            pT = sb("pT4", [128, 2, 2, 128], BF16)
            gl = sb("gl", [128, 2, 1024], F32)
            ot = sb("ot", [128, 2, 1024], F32)
            rinfo = sb("rinfo4", [128, NTILE, 4], F32)
            idx = sb("idx4", [128, NTILE, 2], I32)
            junk = sb("junk4", [128, 1024], BF16)
            rs_ = sb("rs4", [128, 16], F32)
            R_ps = ps("R_ps4", [128, 5, 512], F32)
            T_ps = ps("T_ps4", [128, 3, 1024], BF16)
            rrot = [0]

            def rbank():
                i = rrot[0] % 5
                rrot[0] += 1
                return i
            stc = [0]

            def stat():
                i = stc[0] % 16
                stc[0] += 1
                return rs_[:, i:i + 1], ("rs_", i)

            P.dma("sp", lambda e: e.dma_start(out=cst_f[:], in_=consts[:, :, :]), writes=["cst_f"])
            P.op("dve", lambda e: e.tensor_copy(out=cst[:], in_=cst_f[:]), reads=["cst_f"], writes=["cst"])
            P.dma("sp", lambda e: e.dma_start(out=gple[:], in_=bcast_rows(vec["norm_ple"], 128, 1024)), writes=["gple"])
            P.dma("sp", lambda e: e.dma_start(out=bple[:], in_=bcast_rows(vec["b_ple_gate"], 128, 1024)), writes=["bple"])
            wl = [0]
            for k in range(8):
                sl = wl[0] % 2
                wl[0] += 1
                P.dma("sp", lambda e, sl=sl, k=k: e.dma_start(out=stage[:, sl, :], in_=w_pg[k * 128:(k + 1) * 128, :]), writes=[("stage", sl)])
                P.op("pool", lambda e, sl=sl, k=k: e.tensor_copy(out=wpg[:, k, :], in_=stage[:, sl, :]), reads=[("stage", sl)], writes=[("wpg", k)])
            for k in range(2):
                sl = wl[0] % 2
                wl[0] += 1
                P.dma("sp", lambda e, sl=sl, k=k: e.dma_start(out=stage[:, sl, :], in_=w_pp[k * 128:(k + 1) * 128, :]), writes=[("stage", sl)])
                P.op("pool", lambda e, sl=sl, k=k: e.tensor_copy(out=wpp[:, k, :], in_=stage[:, sl, :]), reads=[("stage", sl)], writes=[("wpp", k)])
            WPG = [("wpg", k) for k in range(8)]
            WPP = [("wpp", k) for k in range(2)]
            def p4_loads(ti):
                b = ti // TPS
                r0 = (ti % TPS) * 128
                s = ti % 3
                P.dma("pool", lambda e, s=s, ti=ti: e.indirect_dma_start(out=yA[:, s, :], out_offset=None, in_=ys_d[:, :],
                                                                 in_offset=bass.IndirectOffsetOnAxis(ap=idx[:, ti, 0:1], axis=0)), reads=["idx"], writes=[("yA", s)])
                P.dma("pool", lambda e, s=s, ti=ti: e.indirect_dma_start(out=yB[:, s, :], out_offset=None, in_=ys_d[:, :],
                                                                 in_offset=bass.IndirectOffsetOnAxis(ap=idx[:, ti, 1:2], axis=0)), reads=["idx"], writes=[("yB", s)])
                P.dma("sp", lambda e, s=s, ti=ti: e.dma_start(out=h1t[:, s, :], in_=h1_d[ti * 128:(ti + 1) * 128, :]), writes=[("h1t", s)])
                P.dma("sp", lambda e, s=s, b=b, r0=r0: e.dma_start(out=pt[:, s, :], in_=pin[b, r0:r0 + 128, :]), writes=[("pt", s)])

            P.dma("sp", lambda e: e.dma_start(out=rinfo[:], in_=rt_d.rearrange("(t p) c -> p t c", p=128)), writes=["rinfo"])
            P.op("pool", lambda e: e.tensor_copy(out=idx[:], in_=rinfo[:, :, 0:2]), reads=["rinfo"], writes=["idx"])
            p4_loads(0)
            p4_loads(1)
            def p4_front(ti):
                b = ti // TPS
                r0 = (ti % TPS) * 128
                s = ti % 2
                sl = ti % 3
                yield P.op("dve", lambda e, s=s, sl=sl, ti=ti: e.scalar_tensor_tensor(out=h2[:, s, :], in0=yA[:, sl, :], scalar=rinfo[:, ti, 2:3], in1=h1t[:, sl, :], op0=ALU.mult, op1=ALU.add),
                     reads=[("yA", sl), "rinfo", ("h1t", sl)], writes=[("h2", s)])
                yield P.op("dve", lambda e, s=s, sl=sl, ti=ti: e.scalar_tensor_tensor(out=h2[:, s, :], in0=yB[:, sl, :], scalar=rinfo[:, ti, 3:4], in1=h2[:, s, :], op0=ALU.mult, op1=ALU.add),
                     reads=[("yB", sl), "rinfo", ("h2", s)], writes=[("h2", s)])
                ssq, kssq = stat()
                yield P.op("act", lambda e, s=s, ssq=ssq: e.activation(out=junk[:], in_=h2[:, s, :], func=AF.Square, accum_out=ssq), reads=[("h2", s)], writes=["junk", kssq])
                lnv, klnv = stat()
                yield P.op("act", lambda e, ssq=ssq, lnv=lnv: e.activation(out=lnv, in_=ssq, func=AF.Ln, scale=1.0 / 1024, bias=EPS), reads=[kssq], writes=[klnv])
                rstd, krstd = stat()
                yield P.op("act", lambda e, lnv=lnv, rstd=rstd: e.activation(out=rstd, in_=lnv, func=AF.Exp, scale=-0.5), reads=[klnv], writes=[krstd])
                yield P.op("dve", lambda e, s=s, rstd=rstd: e.scalar_tensor_tensor(out=hn[:, s, :], in0=h2[:, s, :], scalar=rstd, in1=gple[:], op0=ALU.mult, op1=ALU.mult),
                     reads=[("h2", s), krstd, "gple"], writes=[("hn", s)])
                for k in range(8):
                    yield P.op("pe", lambda e, k=k, s=s: e.transpose(out=T_ps[:, s, k * 128:(k + 1) * 128], in_=hn[:, s, k * 128:(k + 1) * 128], identity=cst[:, 0, :]),
                         reads=[("hn", s), "cst"], writes=[("T_ps", s)])
                yield P.op("act", lambda e, s=s: e.activation(out=hnT[:, s, :, :], in_=T_ps[:, s, :].rearrange("p (k t) -> p k t", k=8), func=AF.Copy), reads=[("T_ps", s)], writes=[("hnT", s)])
                yield P.op("act", lambda e, s=s, sl=sl: e.activation(out=pb[:, s, :], in_=pt[:, sl, :], func=AF.Copy), reads=[("pt", sl)], writes=[("pb", s)])
                for k in range(2):
                    yield P.op("pe", lambda e, k=k, s=s: e.transpose(out=T_ps[:, 2, s * 256 + k * 128:s * 256 + (k + 1) * 128], in_=pb[:, s, k * 128:(k + 1) * 128], identity=cst[:, 0, :]),
                         reads=[("pb", s), "cst"], writes=[("T_psp", s)])
                yield P.op("act", lambda e, s=s: e.activation(out=pT[:, s, :, :], in_=T_ps[:, 2, s * 256:(s + 1) * 256].rearrange("p (k t) -> p k t", k=2), func=AF.Copy), reads=[("T_psp", s)], writes=[("pT", s)])

            def p4_back(ti):
                b = ti // TPS
                r0 = (ti % TPS) * 128
                s = ti % 2
                sl = ti % 3
                for n in range(2):
                    rg = rbank()
                    for k in range(8):
                        yield P.op("pe", lambda e, k=k, n=n, rg=rg, s=s: e.matmul(R_ps[:, rg, :], lhsT=hnT[:, s, k, :], rhs=wpg[:, k, n * 512:(n + 1) * 512], start=(k == 0), stop=(k == 7)),
                             reads=[("hnT", s)] + WPG, writes=[("R", rg)])
                    rp = rbank()
                    for k in range(2):
                        yield P.op("pe", lambda e, k=k, n=n, rp=rp, s=s: e.matmul(R_ps[:, rp, :], lhsT=pT[:, s, k, :], rhs=wpp[:, k, n * 512:(n + 1) * 512], start=(k == 0), stop=(k == 1)),
                             reads=[("pT", s)] + WPP, writes=[("R", rp)])
                    yield P.op("dve", lambda e, n=n, rg=rg, s=s: e.tensor_tensor(out=gl[:, s, n * 512:(n + 1) * 512], in0=R_ps[:, rg, :], in1=bple[:, n * 512:(n + 1) * 512], op=ALU.add),
                         reads=[("R", rg), "bple"], writes=[("gl", s, n)])
                    yield P.op("act", lambda e, n=n, s=s: e.activation(out=gl[:, s, n * 512:(n + 1) * 512], in_=gl[:, s, n * 512:(n + 1) * 512], func=AF.Sigmoid),
                         reads=[("gl", s, n)], writes=[("gl", s, n)])
                    yield P.op("dve", lambda e, n=n, rp=rp, s=s: e.tensor_tensor(out=gl[:, s, n * 512:(n + 1) * 512], in0=R_ps[:, rp, :], in1=gl[:, s, n * 512:(n + 1) * 512], op=ALU.mult),
                         reads=[("R", rp), ("gl", s, n)], writes=[("gl", s, n)])
                    yield P.op("dve", lambda e, n=n, s=s: e.tensor_tensor(out=ot[:, s, n * 512:(n + 1) * 512], in0=gl[:, s, n * 512:(n + 1) * 512], in1=h2[:, s, n * 512:(n + 1) * 512], op=ALU.add),
                         reads=[("gl", s, n), ("h2", s)], writes=[("ot", s, n)])
                yield P.dma("sp", lambda e, s=s, b=b, r0=r0: e.dma_start(out=out[b, r0:r0 + 128, :], in_=ot[:, s, :]), reads=[("ot", s, 0), ("ot", s, 1)])

            def zip_run(ga, gb, ratio=2):
                a_done = ga is None
                b_done = gb is None
                while not (a_done and b_done):
                    if not a_done:
                        try:
                            next(ga)
                        except StopIteration:
                            a_done = True
                    for _ in range(ratio):
                        if not b_done:
                            try:
                                next(gb)
                            except StopIteration:
                                b_done = True

            zip_run(p4_front(0), None)
            for ti in range(NTILE):
                if ti + 2 < NTILE:
                    p4_loads(ti + 2)
                zip_run(p4_front(ti + 1) if ti + 1 < NTILE else None, p4_back(ti))
            P.flush()
    return nc


def make_consts(CAP):
    k = np.arange(128)
    c = np.zeros((128, 4, 128), np.float32)
    c[:, 0, :] = np.eye(128, dtype=np.float32)
    c[:, 1, :] = (k[:, None] <= k[None, :]).astype(np.float32)
    c[:, 2, :] = (k[:, None] < k[None, :]).astype(np.float32)
    blk = (k[:, None] // 64 == k[None, :] // 64).astype(np.float32) / 64.0
    c[:, 3, :] = blk
    capoff = (np.arange(32, dtype=np.float32) * CAP).reshape(1, 32)
    return c, capoff


def core_inputs(inputs, core, NB, CAP):
    f = lambda a: np.ascontiguousarray(a, dtype=np.float32)
    L = 0
    consts, capoff = make_consts(CAP)
    m = {
        "x": f(inputs["x"][core * NB:(core + 1) * NB]),
        "p": f(inputs["p"][L, core * NB:(core + 1) * NB]),
        "w_in": f(inputs["w_in"][L]),
        "w_attn_out": f(inputs["w_attn_out"][L]),
        "w_conv_out": f(inputs["w_conv_out"][L]),
        "w_o": f(inputs["w_o"][L]),
        "w_rt": f(np.concatenate([inputs["w_router_group"][L], inputs["w_router_expert"][L]], axis=1)),
        "b_rt": f(np.concatenate([inputs["b_router_group"][L], inputs["b_router_expert"][L]])[None, :]),
        "w1": f(inputs["w1"][L]), "w3": f(inputs["w3"][L]), "w2": f(inputs["w2"][L]),
        "w_ple_gate": f(inputs["w_ple_gate"][L]), "w_ple_proj": f(inputs["w_ple_proj"][L]),
        "consts": consts, "capoff": capoff,
        "conv_w": f(inputs["conv_w"][L].T),
    }
    for n in ["norm_mix", "norm_ffn", "norm_ple", "b_ple_gate", "lambda_q1", "lambda_k1", "lambda_q2",
              "lambda_k2", "subln", "b_conv_in", "b_gate", "conv_b", "conv_ln_g", "conv_ln_b", "b_conv_out"]:
        m[n] = f(inputs[n][L]).reshape(1, -1)
    m["q_norm"] = f(inputs["q_norm"][L]).reshape(1, 128)
    m["k_norm"] = f(inputs["k_norm"][L]).reshape(1, 128)
    return m


_CACHE = {}


def kernel(**inputs):
    NB, CAP = 2, 1024
    if "nc" not in _CACHE:
        _CACHE["nc"] = build_program(S=4096, NB=NB, CAP=CAP)
    nc = _CACHE["nc"]
    in_maps = [core_inputs(inputs, c, NB, CAP) for c in range(8)]
    res = run_bass_kernel_spmd(nc, in_maps, core_ids=list(range(8)))
    return np.concatenate([np.asarray(r["out"]) for r in res.results], axis=0).astype(np.float32)
```

```python
from contextlib import ExitStack

import numpy as np
import concourse.bass as bass
import concourse.mybir as mybir
from concourse.bass_utils import run_bass_kernel_spmd

F32 = mybir.dt.float32
BF16 = mybir.dt.bfloat16
I32 = mybir.dt.int32
AF = mybir.ActivationFunctionType
ALU = mybir.AluOpType
AX = mybir.AxisListType

ENGS = ("pe", "act", "dve", "pool", "sp")
NDMASEM = 8
EPS = 1e-6
STOP = [99]
LAMBDA_INIT = 0.2


class Op:
    __slots__ = ("eng", "fn", "deps", "isdma", "tok", "marked", "cum")

    def __init__(self, eng, fn, isdma):
        self.eng = eng
        self.fn = fn
        self.deps = []
        self.isdma = isdma
        self.tok = None
        self.marked = False
        self.cum = None


class Prog:
    SEM_LIMIT = 12000

    def __init__(self, nc, stack):
        self.nc = nc
        self.stack = stack
        self.nsem = 0
        self.sems = {e: self._newsem(e) for e in ENGS}
        self.finals = []
        self.dsems = {e: [stack.enter_context(nc.semaphore("d_%s%d" % (e, i)))
                          for i in range(NDMASEM)] for e in ("sp", "pool")}
        self.dcount = {e: 0 for e in self.dsems}
        self.base = {e: 0 for e in ENGS}
        self._reset()

    def _newsem(self, e):
        self.nsem += 1
        return self.stack.enter_context(self.nc.semaphore("s_%s_%d" % (e, self.nsem)))

    def _reset(self):
        self.ops = {e: [] for e in ENGS}
        self.lastw = {}
        self.readers = {}

    def _add(self, eng, fn, reads, writes, isdma):
        op = Op(eng, fn, isdma)
        deps = []
        for r in reads:
            w = self.lastw.get(r)
            if w is not None:
                deps.append(w)
        for w_ in writes:
            w = self.lastw.get(w_)
            if w is not None:
                deps.append(w)
            deps.extend(self.readers.get(w_, ()))
        op.deps = list(dict.fromkeys(deps))
        self.ops[eng].append(op)
        for r in reads:
            self.readers.setdefault(r, []).append(op)
        for w_ in writes:
            self.lastw[w_] = op
            self.readers[w_] = []
        if isdma:
            n = self.dcount[eng]
            self.dcount[eng] = n + 1
            sem = self.dsems[eng][n % NDMASEM]
            op.tok = (sem, 16 * (n // NDMASEM + 1))
            op.cum = (sem, 16 * (n // NDMASEM))
        return op

    def op(self, eng, fn, reads=(), writes=()):
        return self._add(eng, fn, tuple(reads), tuple(writes), False)

    def dma(self, eng, fn, reads=(), writes=()):
        return self._add(eng, fn, tuple(reads), tuple(writes), True)

    def flush(self):
        nc = self.nc
        ops = self.ops
        for e in ENGS:
            for op in ops[e]:
                for d in op.deps:
                    if not d.isdma and not (d.eng == "pe" and e == "pe"):
                        d.marked = True
            last = [o for o in ops[e] if not o.isdma]
            if last:
                last[-1].marked = True
        for e in ENGS:
            c = self.base[e]
            for op in ops[e]:
                if not op.isdma and op.marked:
                    if c >= self.SEM_LIMIT:
                        self.finals.append((self.sems[e], c))
                        self.sems[e] = self._newsem(e)
                        c = 0
                    c += 1
                    op.tok = (self.sems[e], c)
            self.base[e] = c
        prog = self

        def emit_engine(e, eng):
            waited = {}

            def wait(sem, val):
                k = id(sem)
                if val > 0 and waited.get(k, 0) < val:
                    waited[k] = val
                    eng.wait_ge(sem, val)

            for op in ops[e]:
                if op.isdma:
                    wait(*op.cum)
                for d in op.deps:
                    if d.eng == "pe" and e == "pe" and not d.isdma:
                        continue
                    wait(*d.tok)
                ins = op.fn(eng)
                if op.isdma:
                    ins.then_inc(op.tok[0], 16)
                elif op.marked:
                    ins.then_inc(op.tok[0], 1)
            for (fs, fv) in prog.finals:
                wait(fs, fv)
            for e2 in ENGS:
                wait(prog.sems[e2], prog.base[e2])
            for q in prog.dsems:
                n = prog.dcount[q]
                for i in range(NDMASEM):
                    k = (n - i + NDMASEM - 1) // NDMASEM if n > i else 0
                    wait(prog.dsems[q][i], 16 * k)

        with nc.Block() as block:
            @block.tensor
            def _(eng):
                emit_engine("pe", eng)

            @block.scalar
            def _(eng):
                emit_engine("act", eng)

            @block.vector
            def _(eng):
                emit_engine("dve", eng)

            @block.gpsimd
            def _(eng):
                emit_engine("pool", eng)

            @block.sync
            def _(eng):
                emit_engine("sp", eng)
        self._reset()


def bcast_rows(ap, nrows, ncols, off=0):
    return bass.AP(ap.tensor, off, [[0, nrows], [1, ncols]])


def build_program(S=4096, NB=2, CAP=1024, debug=(), phases=(1, 2, 3, 4)):
    nc = bass.Bass("TRN2", target_bir_lowering=False)
    NT = NB * S
    NTILE = NT // 128
    NG = NT // 512
    GPS = S // 512
    TPS = S // 128
    NE = 32
    NBLK = CAP // 128

    def din(name, shape, dt=F32):
        return nc.dram_tensor(name, shape, dt, kind="ExternalInput").ap()

    def dscr(name, shape, dt):
        kind = "ExternalOutput" if name in debug else "Internal"
        return nc.dram_tensor(name, shape, dt, kind=kind).ap()

    x = din("x", [NB, S, 1024])
    pin = din("p", [NB, S, 256])
    w_in = din("w_in", [1024, 4608])
    w_ao = din("w_attn_out", [512, 1024])
    w_co = din("w_conv_out", [512, 1024])
    w_o = din("w_o", [1024, 1024])
    w_rt = din("w_rt", [1024, 36])
    w1 = din("w1", [NE, 1024, 512])
    w3 = din("w3", [NE, 1024, 512])
    w2 = din("w2", [NE, 512, 1024])
    w_pg = din("w_ple_gate", [1024, 1024])
    w_pp = din("w_ple_proj", [256, 1024])
    consts = din("consts", [128, 4, 128])
    capoff = din("capoff", [1, 32])
    vec = {n: din(n, [1, l]) for n, l in [
        ("norm_mix", 1024), ("norm_ffn", 1024), ("norm_ple", 1024), ("b_ple_gate", 1024),
        ("b_rt", 36), ("lambda_q1", 64), ("lambda_k1", 64), ("lambda_q2", 64), ("lambda_k2", 64),
        ("q_norm", 128), ("k_norm", 128), ("subln", 128), ("b_conv_in", 1024), ("b_gate", 2048),
        ("conv_b", 512), ("conv_ln_g", 512), ("conv_ln_b", 512), ("b_conv_out", 1024)]}
    conv_w = din("conv_w", [512, 31])
    out = nc.dram_tensor("out", [NB, S, 1024], F32, kind="ExternalOutput").ap()

    uT_d = dscr("uT_d", [1024, NT], BF16)
    attnT_d = dscr("attnT_d", [512, NT], BF16)
    h1_d = dscr("h1_d", [NT, 1024], F32)
    xs_d = dscr("xs_d", [NE * CAP, 1024], BF16)
    ys_d = dscr("ys_d", [NE * CAP, 1024], BF16)
    rt_d = dscr("rt_d", [NT, 4], F32)

    def col_ap(v, n, off=0):
        return bass.AP(v.tensor, off, [[1, n], [1, 1]])

    with ExitStack() as top:
        P = Prog(nc, top)

        if 1 in phases:
          with ExitStack() as st:
            def sb(n, s, d):
                return st.enter_context(nc.sbuf_tensor(n, s, d))

            def ps(n, s, d):
                return st.enter_context(nc.psum_tensor(n, s, d))

            cst_f = sb("cst_f", [128, 4, 128], F32)
            cst = sb("cst", [128, 4, 128], BF16)
            wq = sb("wq", [128, 8, 1536], BF16)
            stage = sb("stage", [128, 2, 1536], F32)
            kT = sb("kT", [128, 4, S], BF16)
            Va = sb("Va", [128, TPS, 4, 129], BF16)
            xt = sb("xt", [128, 2, 1024], F32)
            gmix = sb("gmix", [128, 1024], F32)
            junk = sb("junk", [128, 1024], BF16)
            ub = sb("ub", [128, 2, 1024], BF16)
            uT = sb("uT", [128, 1, 8, 512], BF16)
            qT = sb("qT", [128, 2, 4, 512], BF16)
            sq = sb("sq", [128, 2, 512], BF16)
            lnb = sb("lnb", [128, 2, 512], F32)
            rsb = sb("rsb", [128, 2, 512], F32)
            pT = sb("pT", [128, 3, 2, 512], BF16)
            atm = sb("atm", [128, 4, 512], BF16)
            attnT = sb("attnT", [128, 1, 4, 512], BF16)
            o0n = sb("o0n", [128, 2, 128], F32)
            Ocp = sb("Ocp", [128, 2, 3, 387], F32)
            dd = sb("dd", [128, 2, 128], F32)
            junk2 = sb("junk2", [128, 128], BF16)
            st1 = sb("st1", [128, 64], F32)
            gqk = sb("gqk", [128, 2], F32)
            lamv = sb("lamv", [128, 4, 64], F32)
            lams = sb("lams", [128, 8], F32)

            O_ps = ps("O_ps", [128, 3, 512], F32)
            T_ps = ps("T_ps", [128, 1024], BF16)
            R_ps = ps("R_ps", [128, 4, 512], F32)
            rrot = [0]

            def rbank():
                i = rrot[0] % 3
                rrot[0] += 1
                return i

            ident = cst[:, 0, :]
            tri = cst[:, 1, :]
            bones = cst[:, 3, :]

            P.dma("sp", lambda e: e.dma_start(out=cst_f[:], in_=consts[:, :, :]), writes=["cst_f"])
            P.op("dve", lambda e: e.tensor_copy(out=cst[:], in_=cst_f[:]), reads=["cst_f"], writes=["cst"])
            P.dma("sp", lambda e: e.dma_start(out=gmix[:], in_=bcast_rows(vec["norm_mix"], 128, 1024)), writes=["gmix"])
            P.dma("sp", lambda e: e.dma_start(out=gqk[:, 0:1], in_=col_ap(vec["q_norm"], 128)), writes=["gqk0"])
            P.dma("sp", lambda e: e.dma_start(out=gqk[:, 1:2], in_=col_ap(vec["k_norm"], 128)), writes=["gqk1"])
            P.op("dve", lambda e: e.tensor_scalar(out=gqk[:, 0:1], in0=gqk[:, 0:1], scalar1=0.125, scalar2=None, op0=ALU.mult),
                 reads=["gqk0"], writes=["gqk0"])
            for i, n in enumerate(["lambda_q1", "lambda_k1", "lambda_q2", "lambda_k2"]):
                P.dma("sp", lambda e, i=i, n=n: e.dma_start(out=lamv[:, i, :], in_=bcast_rows(vec[n], 128, 64)), writes=[("lamv", i)])
            for i in range(2):
                P.op("dve", lambda e, i=i: e.tensor_tensor(
                    out=lamv[:, 2 * i, :], in0=lamv[:, 2 * i, :], in1=lamv[:, 2 * i + 1, :], op=ALU.mult),
                    reads=[("lamv", 2 * i), ("lamv", 2 * i + 1)], writes=[("lamv", 2 * i)])
                P.op("dve", lambda e, i=i: e.tensor_reduce(out=lams[:, i:i + 1], in_=lamv[:, 2 * i, :], axis=AX.X, op=ALU.add),
                     reads=[("lamv", 2 * i)], writes=[("lams", i)])
                P.op("act", lambda e, i=i: e.activation(out=lams[:, 2 + i:3 + i], in_=lams[:, i:i + 1], func=AF.Exp),
                     reads=[("lams", i)], writes=[("lams", 2 + i)])
            P.op("dve", lambda e: e.tensor_tensor(out=lams[:, 4:5], in0=lams[:, 3:4], in1=lams[:, 2:3], op=ALU.subtract),
                 reads=[("lams", 2), ("lams", 3)], writes=[("lams", 4)])
            P.op("dve", lambda e: e.tensor_scalar(out=lams[:, 5:6], in0=lams[:, 4:5], scalar1=-LAMBDA_INIT, scalar2=None, op0=ALU.add),
                 reads=[("lams", 4)], writes=["nlam"])
            nlam = lams[:, 5:6]
            P.op("pool", lambda e: e.memset(Va[:, :, :, 128:129], 1.0), writes=["Va_ones"])
            P.op("pool", lambda e: e.memset(qT[:], 0.0), writes=[("qT", c, m) for c in range(4) for m in range(2)])
            zt = sb("zt", [128, 2, 1024], BF16)
            P.op("pool", lambda e: e.memset(zt[:], 0.0), writes=["zt"])
            xs_v = xs_d.rearrange("(n p) c -> p n c", p=128)
            for i in range(NE * CAP // 256):
                P.dma("pool", lambda e, i=i: e.dma_start(out=xs_v[:, i * 2:(i + 1) * 2, :], in_=zt[:]), reads=["zt"], writes=[("xs_z", i)])
            for k in range(8):
                sl = k % 2
                P.dma("sp", lambda e, k=k, sl=sl: e.dma_start(out=stage[:, sl, :], in_=w_in[k * 128:(k + 1) * 128, 0:1536]),
                      writes=[("stage", sl)])
                P.op("pool", lambda e, k=k, sl=sl: e.tensor_copy(out=wq[:, k, :], in_=stage[:, sl, :]),
                     reads=[("stage", sl)], writes=[("wq", k)])
            WQ = [("wq", k) for k in range(8)]

            stc = [0]

            def stat():
                i = stc[0] % 64
                stc[0] += 1
                return st1[:, i:i + 1], ("st1", i)

            tilec = [0]
            deferred = []
            arot = [0]
            for gi in range(NG):
                b = gi // GPS
                G = gi % GPS
                s0 = G * 512
                us = 0
                for j in range(4):
                    tc_ = tilec[0]
                    tilec[0] += 1
                    xs = tc_ % 2
                    P.dma("sp", lambda e, xs=xs, b=b, r0=s0 + j * 128: e.dma_start(out=xt[:, xs, :], in_=x[b, r0:r0 + 128, :]),
                          writes=[("xt", xs)])
                    ssq, kssq = stat()
                    P.op("act", lambda e, xs=xs, ssq=ssq: e.activation(
                        out=junk[:], in_=xt[:, xs, :], func=AF.Square, accum_out=ssq), reads=[("xt", xs)], writes=["junk", kssq])
                    lnv, klnv = stat()
                    P.op("act", lambda e, ssq=ssq, lnv=lnv: e.activation(out=lnv, in_=ssq, func=AF.Ln, scale=1.0 / 1024, bias=EPS),
                         reads=[kssq], writes=[klnv])
                    rstd, krstd = stat()
                    P.op("act", lambda e, lnv=lnv, rstd=rstd: e.activation(out=rstd, in_=lnv, func=AF.Exp, scale=-0.5),
                         reads=[klnv], writes=[krstd])
                    P.op("dve", lambda e, xs=xs, rstd=rstd: e.scalar_tensor_tensor(
                        out=ub[:, xs, :], in0=xt[:, xs, :], scalar=rstd, in1=gmix[:], op0=ALU.mult, op1=ALU.mult),
                        reads=[("xt", xs), krstd, "gmix"], writes=[("ub", xs)])
                    for k in range(8):
                        P.op("pe", lambda e, xs=xs, k=k: e.transpose(out=T_ps[:, k * 128:(k + 1) * 128], in_=ub[:, xs, k * 128:(k + 1) * 128], identity=ident),
                             reads=[("ub", xs), "cst"], writes=["T_ps"])
                    P.op("dve", lambda e, us=us, j=j: e.tensor_copy(
                        out=uT[:, us, :, j * 128:(j + 1) * 128], in_=T_ps[:].rearrange("p (k t) -> p k t", k=8)),
                        reads=["T_ps"], writes=[("uT", us, j)])
                UT = [("uT", us, j) for j in range(4)]
                P.dma("sp", lambda e, us=us, c0=gi * 512: e.dma_start(
                    out=uT_d.rearrange("(k p) t -> p k t", p=128)[:, :, c0:c0 + 512], in_=uT[:, us, :, :]), reads=UT)

                if STOP[0] <= 1:
                    continue
                def qk_proj(c):
                    rb = c % 2
                    for k in range(8):
                        P.op("pe", lambda e, c=c, k=k, rb=rb: e.matmul(
                            R_ps[:, rb, :], lhsT=wq[:, k, c * 128:(c + 1) * 128], rhs=uT[:, us, k, :], start=(k == 0), stop=(k == 7)),
                            reads=UT + [("wq", k)], writes=[("R", rb)])
                qk_proj(0)
                for c in range(8):
                    rb = c % 2
                    s2 = c % 2
                    P.op("act", lambda e, rb=rb, s2=s2: e.activation(out=sq[:, s2, :], in_=R_ps[:, rb, :], func=AF.Square),
                         reads=[("R", rb)], writes=[("sq", s2)])
                    rb2 = 2
                    P.op("pe", lambda e, rb2=rb2, s2=s2: e.matmul(R_ps[:, rb2, :], lhsT=bones, rhs=sq[:, s2, :], start=True, stop=True),
                         reads=[("sq", s2), "cst"], writes=[("R", rb2)])
                    P.op("act", lambda e, rb2=rb2, s2=s2: e.activation(out=lnb[:, s2, :], in_=R_ps[:, rb2, :], func=AF.Ln, bias=EPS),
                         reads=[("R", rb2)], writes=[("lnb", s2)])
                    P.op("act", lambda e, s2=s2: e.activation(out=rsb[:, s2, :], in_=lnb[:, s2, :], func=AF.Exp, scale=-0.5),
                         reads=[("lnb", s2)], writes=[("rsb", s2)])
                    if c < 4:
                        for m in range(2):
                            pr = slice(m * 64, (m + 1) * 64)
                            P.op("dve", lambda e, rb=rb, s2=s2, m=m, pr=pr, c=c: e.scalar_tensor_tensor(
                                out=qT[pr, m, c, :], in0=R_ps[pr, rb, :], scalar=gqk[pr, 0:1], in1=rsb[pr, s2, :], op0=ALU.mult, op1=ALU.mult),
                                reads=[("R", rb), ("rsb", s2), "gqk0"], writes=[("qT", c, m)])
                    else:
                        P.op("dve", lambda e, rb=rb, s2=s2, c=c, s0=s0: e.scalar_tensor_tensor(
                            out=kT[:, c - 4, s0:s0 + 512], in0=R_ps[:, rb, :], scalar=gqk[:, 1:2], in1=rsb[:, s2, :], op0=ALU.mult, op1=ALU.mult),
                            reads=[("R", rb), ("rsb", s2), "gqk1"], writes=[("kT", c - 4, G)])
                    if c + 1 < 8:
                        qk_proj(c + 1)
                rrot[0] = 0
                if STOP[0] <= 2:
                    continue
                for j in range(4):
                    rb = rbank()
                    for k in range(8):
                        P.op("pe", lambda e, j=j, k=k, rb=rb: e.matmul(
                            R_ps[:, rb, :], lhsT=uT[:, us, k, j * 128:(j + 1) * 128], rhs=wq[:, k, 1024:1536], start=(k == 0), stop=(k == 7)),
                            reads=UT + [("wq", k)], writes=[("R", rb)])
                    P.op("dve", lambda e, j=j, rb=rb, tl=G * 4 + j: e.tensor_copy(
                        out=Va[:, tl, :, 0:128], in_=R_ps[:, rb, :].rearrange("p (h v) -> p h v", h=4)),
                        reads=[("R", rb)], writes=[("Va", G * 4 + j)])

                if STOP[0] <= 3:
                    continue
                nkt = 4 * G + 4
                for h in range(4):
                    steps = [(m, jp) for m in range(2) for jp in range(nkt // 2)]

                    def acc(m, c):
                        a = m * 4 + c
                        return a // 3, (a % 3) * 129

                    def emit_qk(si, h=h, steps=steps):
                        m, jp = steps[si]
                        j0 = 2 * jp
                        r0 = j0 - 4 * G
                        q0 = max(r0, 0) * 128
                        ncol = 512 - q0
                        pb_ = arot[0] % 2
                        arot[0] += 1
                        for t in range(2):
                            P.op("pe", lambda e, m=m, j=j0 + t, q0=q0, ncol=ncol, rb=2 * pb_ + t: e.matmul(
                                R_ps[:, rb, 0:ncol], lhsT=kT[:, h, j * 128:(j + 1) * 128],
                                rhs=qT[:, m, h, q0:512], start=True, stop=True),
                                reads=[("kT", h, (j0 + t) // 4), ("qT", h, m)], writes=[("R", 2 * pb_ + t)])
                        pslot = si % 3
                        P.op("act", lambda e, pb_=pb_, ncol=ncol, pslot=pslot: e.activation(
                            out=pT[:, pslot, :, 0:ncol], in_=R_ps[:, 2 * pb_:2 * pb_ + 2, 0:ncol], func=AF.Exp),
                            reads=[("R", 2 * pb_), ("R", 2 * pb_ + 1)], writes=[("pT", pslot)])
                        for t in range(2):
                            r = r0 + t
                            if r >= 0:
                                off = r * 128 - q0
                                P.op("pool", lambda e, pslot=pslot, t=t, off=off: e.tensor_tensor(
                                    out=pT[:, pslot, t, off:off + 128], in0=pT[:, pslot, t, off:off + 128], in1=tri, op=ALU.mult),
                                    reads=[("pT", pslot), "cst"], writes=[("pT", pslot)])
                        return (m, j0, r0, q0, pslot)

                    started = set()

                    def emit_pv(info, h=h, started=started):
                        m, j0, r0, q0, pslot = info
                        for t in range(2):
                            j = j0 + t
                            r = r0 + t
                            for c in range(max(r, 0), 4):
                                bank, off = acc(m, c)
                                first = bank not in started
                                started.add(bank)
                                lastj = (j == 4 * G + c)
                                P.op("pe", lambda e, pslot=pslot, t=t, c=c, q0=q0, bank=bank, off=off, first=first, lastj=lastj, j=j: e.matmul(
                                    O_ps[:, bank, off:off + 129], lhsT=pT[:, pslot, t, c * 128 - q0:c * 128 - q0 + 128],
                                    rhs=Va[:, j, h, :], start=first, stop=lastj, skip_group_check=True),
                                    reads=[("pT", pslot), ("Va", j), "Va_ones"], writes=[("O", bank)])

                    info = emit_qk(0)
                    for si in range(len(steps)):
                        nxt = emit_qk(si + 1) if si + 1 < len(steps) else None
                        emit_pv(info)
                        info = nxt
                        if deferred:
                            deferred.pop(0)()
                    hp = h % 2
                    P.op("act", lambda e, hp=hp: e.activation(out=Ocp[:, hp, 0:2, :], in_=O_ps[:, 0:2, 0:387], func=AF.Copy),
                         reads=[("O", 0), ("O", 1)], writes=[("Ocp", hp, 0)])
                    P.op("dve", lambda e, hp=hp: e.tensor_copy(out=Ocp[:, hp, 2:3, 0:258], in_=O_ps[:, 2:3, 0:258]),
                         reads=[("O", 2)], writes=[("Ocp", hp, 1)])
                    def finish_c(c, h=h, hp=hp):
                        o0 = Ocp[:, hp, c // 3, (c % 3) * 129:(c % 3) * 129 + 129]
                        o1 = Ocp[:, hp, (4 + c) // 3, ((4 + c) % 3) * 129:((4 + c) % 3) * 129 + 129]
                        k0 = ("Ocp", hp, 0)
                        k1 = ("Ocp", hp, 0) if (4 + c) // 3 < 2 else ("Ocp", hp, 1)
                        r0, kr0 = stat()
                        r1, kr1 = stat()
                        P.op("dve", lambda e, o0=o0, r0=r0: e.reciprocal(out=r0, in_=o0[:, 128:129]), reads=[k0], writes=[kr0])
                        P.op("dve", lambda e, o1=o1, r1=r1: e.reciprocal(out=r1, in_=o1[:, 128:129]), reads=[k1], writes=[kr1])
                        r1n, kr1n = stat()
                        P.op("dve", lambda e, r1=r1, r1n=r1n: e.tensor_tensor(out=r1n, in0=r1, in1=nlam, op=ALU.mult),
                             reads=[kr1, "nlam"], writes=[kr1n])
                        ds = c % 2
                        P.op("dve", lambda e, o0=o0, r0=r0, ds=ds: e.tensor_scalar(out=o0n[:, ds, :], in0=o0[:, 0:128], scalar1=r0, scalar2=None, op0=ALU.mult),
                             reads=[k0, kr0], writes=[("o0n", ds)])
                        P.op("dve", lambda e, o1=o1, r1n=r1n, ds=ds: e.scalar_tensor_tensor(
                            out=dd[:, ds, :], in0=o1[:, 0:128], scalar=r1n, in1=o0n[:, ds, :], op0=ALU.mult, op1=ALU.add),
                            reads=[k1, kr1n, ("o0n", ds)], writes=[("dd", ds)])
                        ss, kss = stat()
                        P.op("act", lambda e, ds=ds, ss=ss: e.activation(out=junk2[:], in_=dd[:, ds, :], func=AF.Square, accum_out=ss),
                             reads=[("dd", ds)], writes=["junk2", kss])
                        ln2, kln2 = stat()
                        P.op("act", lambda e, ss=ss, ln2=ln2: e.activation(out=ln2, in_=ss, func=AF.Ln, scale=1.0 / 128, bias=EPS),
                             reads=[kss], writes=[kln2])
                        rs2, krs2 = stat()
                        P.op("act", lambda e, ln2=ln2, rs2=rs2: e.activation(out=rs2, in_=ln2, func=AF.Exp, scale=-0.5),
                             reads=[kln2], writes=[krs2])
                        P.op("dve", lambda e, ds=ds, rs2=rs2, c=c, h=h: e.tensor_scalar(
                            out=atm[:, c, h * 128:(h + 1) * 128], in0=dd[:, ds, :], scalar1=rs2, scalar2=None, op0=ALU.mult),
                            reads=[("dd", ds), krs2], writes=[("atm", c, h)])
                    for c in range(4):
                        deferred.append(lambda c=c, f=finish_c: f(c))
                while deferred:
                    deferred.pop(0)()
                if STOP[0] <= 4:
                    continue
                for c in range(4):
                    for a in range(4):
                        P.op("pe", lambda e, c=c, a=a: e.transpose(out=T_ps[:, a * 128:(a + 1) * 128], in_=atm[:, c, a * 128:(a + 1) * 128], identity=ident),
                             reads=[("atm", c, a), "cst"], writes=["T_ps"])
                    P.op("dve", lambda e, us=us, c=c: e.tensor_copy(
                        out=attnT[:, us, :, c * 128:(c + 1) * 128], in_=T_ps[:, 0:512].rearrange("p (a t) -> p a t", a=4)),
                        reads=["T_ps"], writes=[("attnT", us, c)])
                P.dma("sp", lambda e, us=us, c0=gi * 512: e.dma_start(
                    out=attnT_d.rearrange("(a p) t -> p a t", p=128)[:, :, c0:c0 + 512], in_=attnT[:, us, :, :]),
                    reads=[("attnT", us, c) for c in range(4)])
            P.flush()


        if 2 in phases:
          with ExitStack() as st:
            def sb(n, s_, d):
                return st.enter_context(nc.sbuf_tensor(n, s_, d))

            def ps(n, s_, d):
                return st.enter_context(nc.psum_tensor(n, s_, d))

            cst_f = sb("cst_f2", [128, 4, 128], F32)
            cst = sb("cst2", [128, 4, 128], BF16)
            onesC = sb("onesC", [128, 128], F32)
            wcg = sb("wcg", [128, 8, 3072], BF16)
            wao = sb("wao", [128, 4, 1024], BF16)
            wco = sb("wco", [128, 4, 1024], BF16)
            wo = sb("wo", [128, 8, 1024], BF16)
            wrt = sb("wrt", [128, 8, 36], F32)
            uT = sb("uT2", [128, 8, 512], BF16)
            attnT = sb("attnT2", [128, 4, 512], BF16)
            xt = sb("xt2", [128, 4, 1024], F32)
            glu = sb("glu", [128, 4, 542], BF16)
            dgr = sb("dgr", [128, 16, 128], BF16)
            dgc = [0]
            cacc = sb("cacc", [128, 4, 512], F32)
            sqf = sb("sqf", [128, 2, 512], F32)
            mean_sb = sb("mean_sb", [128, 512], F32)
            tmp2 = sqf
            cT = sb("cT", [128, 4, 512], BF16)
            sg = sb("sg", [128, 2, 512], F32)
            sga = sb("sga", [128, 512], F32)
            sgbs = sb("sgbs", [128, 8, 512], BF16)
            t1s = sb("t1s", [128, 8, 512], BF16)
            t2 = sb("t2", [128, 512], F32)
            mT = sb("mT", [128, 8, 512], BF16)
            h1t = sb("h1t", [128, 2, 1024], F32)
            tn = sb("tn", [128, 1024], F32)
            tb = sb("tb", [128, 4, 1024], BF16)
            L4 = sb("L4", [128, 4, 36], F32)
            gmax4 = sb("gmax4", [128, 4], F32)
            ohg4 = sb("ohg4", [128, 4, 4], F32)
            ge4 = sb("ge4", [128, 4, 4], F32)
            gsum4 = sb("gsum4", [128, 4], F32)
            prod4 = sb("prod4", [128, 4, 4, 8], F32)
            els4 = sb("els4", [128, 4, 8], F32)
            top84 = sb("top84", [128, 4, 8], F32)
            m124 = sb("m124", [128, 2, 4, 8], F32)
            d214 = sb("d214", [128, 4], F32)
            den4 = sb("den4", [128, 4], F32)
            s124 = sb("s124", [128, 2, 4, 32], F32)
            sel4 = sb("sel4", [128, 4, 32], BF16)
            dest4 = sb("dest4", [128, 4, 32], F32)
            tmp324 = sb("tmp324", [128, 4, 32], F32)
            rinfo4 = sb("rinfo42", [128, 4, 4], F32)
            idx4 = sb("idx42", [128, 4, 2], I32)
            tT = sb("tT", [128, 8, 128], F32)
            gffn = sb("gffn", [128, 1024], F32)
            bci = sb("bci", [128, 8], F32)
            bg = sb("bg", [128, 16], F32)
            cvb = sb("cvb", [128, 4], F32)
            lng = sb("lng", [128, 4], F32)
            lnbb = sb("lnbb", [128, 4], F32)
            bco = sb("bco", [128, 8], F32)
            cw = sb("cw", [128, 4, 31], F32)
            sln = sb("sln", [128, 1], F32)
            brt = sb("brt", [128, 36], F32)
            base_bc = sb("base_bc", [128, 32], F32)
            ones_bf = sb("ones_bf", [128, 128], BF16)
            rs_ = sb("rs_", [128, 64], F32)

            R_ps = ps("R_ps2", [128, 6, 512], F32)
            TF_ps = ps("TF_ps", [128, 1024], F32)
            rrot = [0]

            def rbank():
                i = rrot[0] % 6
                rrot[0] += 1
                return i

            stc = [0]

            def stat():
                i = stc[0] % 56
                stc[0] += 1
                return rs_[:, i:i + 1], ("rs_", i)

            P.dma("sp", lambda e: e.dma_start(out=cst_f[:], in_=consts[:, :, :]), writes=["cst_f"])
            P.op("dve", lambda e: e.tensor_copy(out=cst[:], in_=cst_f[:]), reads=["cst_f"], writes=["cst"])
            P.op("pool", lambda e: e.memset(onesC[:], 1.0 / 512), writes=["onesC"])
            P.op("pool", lambda e: e.memset(ones_bf[:], 1.0), writes=["ones_bf"])
            P.dma("sp", lambda e: e.dma_start(out=gffn[:], in_=bcast_rows(vec["norm_ffn"], 128, 1024)), writes=["gffn"])
            P.dma("sp", lambda e: e.dma_start(out=brt[:], in_=bcast_rows(vec["b_rt"], 128, 36)), writes=["brt"])
            P.dma("sp", lambda e: e.dma_start(out=base_bc[:], in_=bcast_rows(capoff, 128, 32)), writes=["base_bc"])
            P.dma("sp", lambda e: e.dma_start(out=sln[:], in_=col_ap(vec["subln"], 128)), writes=["sln"])
            P.op("dve", lambda e: e.tensor_scalar(out=sln[:], in0=sln[:], scalar1=1.0 - LAMBDA_INIT, scalar2=None, op0=ALU.mult),
                 reads=["sln"], writes=["sln"])

            def colvec(dst, name, nchunk):
                P.dma("sp", lambda e: e.dma_start(out=dst[:, 0:nchunk], in_=bass.AP(vec[name].tensor, 0, [[1, 128], [128, nchunk]])),
                      writes=[name])
            with nc.allow_non_contiguous_dma(reason="tiny per-channel bias columns"):
                pass
            for dst, name, nch in [(bci, "b_conv_in", 8), (bg, "b_gate", 16), (cvb, "conv_b", 4), (lng, "conv_ln_g", 4),
                                   (lnbb, "conv_ln_b", 4), (bco, "b_conv_out", 8)]:
                for c in range(nch):
                    P.dma("sp", lambda e, dst=dst, name=name, c=c: e.dma_start(out=dst[:, c:c + 1], in_=col_ap(vec[name], 128, c * 128)),
                          writes=[(name, c)])
            BIAS = [(n, c) for _, n, nch in [(0, "b_conv_in", 8), (0, "b_gate", 16), (0, "conv_b", 4), (0, "conv_ln_g", 4),
                                             (0, "conv_ln_b", 4), (0, "b_conv_out", 8)] for c in range(nch)]
            for cc in range(4):
                for kk in range(31):
                    pass
            for cc in range(4):
                P.dma("sp", lambda e, cc=cc: e.dma_start(out=cw[:, cc, :], in_=conv_w[cc * 128:(cc + 1) * 128, :]),
                      writes=[("cw", cc)])
            P.dma("sp", lambda e: e.dma_start(out=wrt[:], in_=w_rt.rearrange("(k p) n -> p k n", p=128)), writes=["wrt"])
            wl = [0]

            def load_cast(dst_fn, src_fn, key, eng="pool", scale_col=None):
                sl = wl[0] % 4
                wl[0] += 1
                P.dma("sp", lambda e, sl=sl: e.dma_start(out=xt[:, sl, :], in_=src_fn()), writes=[("xt", sl)])
                if scale_col is None:
                    P.op(eng, lambda e, sl=sl: e.tensor_copy(out=dst_fn(), in_=xt[:, sl, :]), reads=[("xt", sl)], writes=[key])
                else:
                    P.op("dve", lambda e, sl=sl: e.tensor_scalar(out=dst_fn(), in0=xt[:, sl, :], scalar1=scale_col, scalar2=None, op0=ALU.mult),
                         reads=[("xt", sl), "sln"], writes=[key])
            def load_wcg(c3):
                for k in range(8):
                    load_cast(lambda k=k, c3=c3: wcg[:, k, c3 * 1024:(c3 + 1) * 1024],
                              lambda k=k, c3=c3: w_in[k * 128:(k + 1) * 128, 1536 + c3 * 1024:1536 + (c3 + 1) * 1024],
                              ("wcg", k, c3), eng="pool" if (k + c3) % 2 else "dve")
            load_wcg(0)
            for a in range(4):
                load_cast(lambda a=a: wao[:, a, :], lambda a=a: w_ao[a * 128:(a + 1) * 128, :], ("wao", a), scale_col=sln[:, 0:1])
            load_wcg(1)
            load_wcg(2)
            for a in range(4):
                load_cast(lambda a=a: wco[:, a, :], lambda a=a: w_co[a * 128:(a + 1) * 128, :], ("wco", a), eng="pool")
            for k in range(8):
                load_cast(lambda k=k: wo[:, k, :], lambda k=k: w_o[k * 128:(k + 1) * 128, :], ("wo", k), eng="pool" if k % 2 else "dve")
            WCG = [("wcg", k, 0) for k in range(8)]
            WCG12 = [("wcg", k, c3) for k in range(8) for c3 in (1, 2)]

            pa_ = [0]
            pb_ = [0]

            def rbankA():
                i = pa_[0] % 3
                pa_[0] += 1
                return i

            def rbankB():
                i = 3 + pb_[0] % 3
                pb_[0] += 1
                return i

            def zip_run2(ga, gb, ratio=1):
                a_done = ga is None
                b_done = gb is None
                while not (a_done and b_done):
                    if not a_done:
                        try:
                            next(ga)
                        except StopIteration:
                            a_done = True
                    for _ in range(ratio):
                        if not b_done:
                            try:
                                next(gb)
                            except StopIteration:
                                b_done = True

            def p2_front_a(gi):
                b = gi // GPS
                G = gi % GPS
                s0 = G * 512
                c0 = gi * 512
                for cc in range(4):
                    ra = rbankA()
                    for k in range(8):
                        yield P.op("pe", lambda e, cc=cc, k=k, ra=ra: e.matmul(R_ps[:, ra, :], lhsT=wcg[:, k, cc * 128:(cc + 1) * 128], rhs=uT[:, k, :],
                                                                       start=(k == 0), stop=(k == 7)), reads=["uT"] + WCG, writes=[("R", ra)])
                    rg = rbankA()
                    for k in range(8):
                        yield P.op("pe", lambda e, cc=cc, k=k, rg=rg: e.matmul(R_ps[:, rg, :], lhsT=wcg[:, k, 512 + cc * 128:512 + (cc + 1) * 128], rhs=uT[:, k, :],
                                                                       start=(k == 0), stop=(k == 7)), reads=["uT"] + WCG, writes=[("R", rg)])
                    s2 = cc % 2
                    yield P.op("act", lambda e, rg=rg, s2=s2, cc=cc: e.activation(out=sg[:, s2, :], in_=R_ps[:, rg, :], func=AF.Sigmoid, bias=bci[:, 4 + cc:5 + cc]),
                         reads=[("R", rg), ("b_conv_in", 4 + cc)], writes=[("sg", s2)])
                    if G == 0:
                        yield P.op("pool", lambda e, cc=cc: e.memset(glu[:, cc, 0:30], 0.0), writes=[("glu", cc)])
                    yield P.op("dve", lambda e, ra=ra, s2=s2, cc=cc: e.scalar_tensor_tensor(
                        out=glu[:, cc, 30:542], in0=R_ps[:, ra, :], scalar=bci[:, cc:cc + 1], in1=sg[:, s2, :], op0=ALU.add, op1=ALU.mult),
                        reads=[("R", ra), ("sg", s2), ("b_conv_in", cc), ("glu", cc)], writes=[("glu", cc)])

            def p2_front_b(gi):
                b = gi // GPS
                G = gi % GPS
                s0 = G * 512
                c0 = gi * 512
                def gate_front(oc):
                    rya = rbankA()
                    for a in range(4):
                        yield P.op("pe", lambda e, a=a, oc=oc, rya=rya: e.matmul(R_ps[:, rya, :], lhsT=wao[:, a, oc * 128:(oc + 1) * 128], rhs=attnT[:, a, :],
                                                                         start=(a == 0), stop=(a == 3)), reads=["attnT", ("wao", a)], writes=[("R", rya)])
                    rga = rbankA()
                    for k in range(8):
                        yield P.op("pe", lambda e, k=k, oc=oc, rga=rga: e.matmul(R_ps[:, rga, :], lhsT=wcg[:, k, 1024 + oc * 128:1024 + (oc + 1) * 128], rhs=uT[:, k, :],
                                                                         start=(k == 0), stop=(k == 7)), reads=["uT"] + WCG12, writes=[("R", rga)])
                    rgb = rbankA()
                    for k in range(8):
                        yield P.op("pe", lambda e, k=k, oc=oc, rgb=rgb: e.matmul(R_ps[:, rgb, :], lhsT=wcg[:, k, 2048 + oc * 128:2048 + (oc + 1) * 128], rhs=uT[:, k, :],
                                                                         start=(k == 0), stop=(k == 7)), reads=["uT"] + WCG12, writes=[("R", rgb)])
                    yield P.op("act", lambda e, oc=oc, rga=rga: e.activation(out=sga[:], in_=R_ps[:, rga, :], func=AF.Sigmoid, bias=bg[:, oc:oc + 1]),
                         reads=[("R", rga), ("b_gate", oc)], writes=["sga"])
                    yield P.op("act", lambda e, oc=oc, rgb=rgb: e.activation(out=sgbs[:, oc, :], in_=R_ps[:, rgb, :], func=AF.Sigmoid, bias=bg[:, 8 + oc:9 + oc]),
                         reads=[("R", rgb), ("b_gate", 8 + oc)], writes=[("sgbs", oc)])
                    yield P.op("dve", lambda e, rya=rya, oc=oc: e.tensor_tensor(out=t1s[:, oc, :], in0=R_ps[:, rya, :], in1=sga[:], op=ALU.mult),
                         reads=[("R", rya), "sga"], writes=[("t1s", oc)])

                nfront = 0
                for cc in range(4):
                    rcv = rbankA()
                    for kk in range(31):
                        ds_ = dgc[0] % 16
                        dgc[0] += 1
                        yield P.op("act", lambda e, ds_=ds_, cc=cc, kk=kk: e.activation(out=dgr[:, ds_, :], in_=cst[:, 0, :], func=AF.Copy, scale=cw[:, cc, kk:kk + 1]),
                             reads=["cst", ("cw", cc)], writes=[("dgr", ds_)])
                        yield P.op("pe", lambda e, ds_=ds_, cc=cc, kk=kk, rcv=rcv: e.matmul(R_ps[:, rcv, :], lhsT=dgr[:, ds_, :], rhs=glu[:, cc, kk:kk + 512],
                                                                                   start=(kk == 0), stop=(kk == 30)),
                             reads=[("dgr", ds_), ("glu", cc)], writes=[("R", rcv)])
                    yield P.op("dve", lambda e, cc=cc, rcv=rcv: e.tensor_scalar(out=cacc[:, cc, :], in0=R_ps[:, rcv, :], scalar1=cvb[:, cc:cc + 1], scalar2=None, op0=ALU.add),
                         reads=[("R", rcv), ("conv_b", cc)], writes=[("cacc", cc)])
                    for _ in range(2):
                        yield from gate_front(nfront)
                        nfront += 1
                if gi + 1 < NG:
                    c1 = (gi + 1) * 512
                    yield P.dma("sp", lambda e, c1=c1: e.dma_start(out=uT[:], in_=uT_d.rearrange("(k p) t -> p k t", p=128)[:, :, c1:c1 + 512]),
                          writes=["uT"])
                    yield P.dma("sp", lambda e, c1=c1: e.dma_start(out=attnT[:], in_=attnT_d.rearrange("(a p) t -> p a t", p=128)[:, :, c1:c1 + 512]),
                          writes=["attnT"])
                for cc in range(4):
                    yield P.op("pool", lambda e, cc=cc: e.tensor_copy(out=glu[:, cc, 0:30], in_=glu[:, cc, 512:542]),
                         reads=[("glu", cc)], writes=[("glu", cc)])

            def p2_mid(gi):
                b = gi // GPS
                G = gi % GPS
                s0 = G * 512
                c0 = gi * 512
                rm = rbankB()
                rv = rbankB()
                for cc in range(4):
                    yield P.op("pe", lambda e, cc=cc, rm=rm: e.matmul(R_ps[:, rm, :], lhsT=onesC[:], rhs=cacc[:, cc, :], start=(cc == 0), stop=(cc == 3)),
                         reads=[("cacc", cc), "onesC"], writes=[("R", rm)])
                for cc in range(4):
                    s2 = cc % 2
                    yield P.op("act", lambda e, cc=cc, s2=s2: e.activation(out=sqf[:, s2, :], in_=cacc[:, cc, :], func=AF.Square),
                         reads=[("cacc", cc)], writes=[("sqf", s2)])
                    yield P.op("pe", lambda e, cc=cc, rv=rv, s2=s2: e.matmul(R_ps[:, rv, :], lhsT=onesC[:], rhs=sqf[:, s2, :], start=(cc == 0), stop=(cc == 3)),
                         reads=[("sqf", s2), "onesC"], writes=[("R", rv)])
                yield P.op("act", lambda e, rm=rm: e.activation(out=mean_sb[:], in_=R_ps[:, rm, :], func=AF.Copy), reads=[("R", rm)], writes=["mean_sb"])
                yield P.op("dve", lambda e: e.tensor_tensor(out=t2[:], in0=mean_sb[:], in1=mean_sb[:], op=ALU.mult), reads=["mean_sb"], writes=["t2"])
                yield P.op("dve", lambda e, rv=rv: e.tensor_tensor(out=t2[:], in0=R_ps[:, rv, :], in1=t2[:], op=ALU.subtract),
                     reads=[("R", rv), "t2"], writes=["t2"])
                yield P.op("act", lambda e: e.activation(out=t2[:], in_=t2[:], func=AF.Ln, bias=EPS), reads=["t2"], writes=["t2"])
                yield P.op("act", lambda e: e.activation(out=sga[:], in_=t2[:], func=AF.Exp, scale=-0.5), reads=["t2"], writes=["sga"])
                for cc in range(4):
                    s2 = cc % 2
                    yield P.op("dve", lambda e, cc=cc, s2=s2: e.tensor_tensor(out=tmp2[:, s2, :], in0=cacc[:, cc, :], in1=mean_sb[:], op=ALU.subtract),
                         reads=[("cacc", cc), "mean_sb"], writes=[("sqf", s2)])
                    yield P.op("dve", lambda e, s2=s2: e.tensor_tensor(out=tmp2[:, s2, :], in0=tmp2[:, s2, :], in1=sga[:], op=ALU.mult),
                         reads=[("sqf", s2), "sga"], writes=[("sqf", s2)])
                    yield P.op("act", lambda e, cc=cc, s2=s2: e.activation(out=cT[:, cc, :], in_=tmp2[:, s2, :], func=AF.Silu,
                                                                     scale=lng[:, cc:cc + 1], bias=lnbb[:, cc:cc + 1]),
                         reads=[("sqf", s2), ("conv_ln_g", cc), ("conv_ln_b", cc)], writes=[("cT", cc)])
                for oc in range(8):
                    ryb = rbankB()
                    for a in range(4):
                        yield P.op("pe", lambda e, a=a, oc=oc, ryb=ryb: e.matmul(R_ps[:, ryb, :], lhsT=wco[:, a, oc * 128:(oc + 1) * 128], rhs=cT[:, a, :],
                                                                         start=(a == 0), stop=(a == 3)), reads=[("cT", a), ("wco", a)], writes=[("R", ryb)])
                    yield P.op("dve", lambda e, ryb=ryb, oc=oc: e.scalar_tensor_tensor(out=t2[:], in0=R_ps[:, ryb, :], scalar=bco[:, oc:oc + 1], in1=sgbs[:, oc, :],
                                                                                op0=ALU.add, op1=ALU.mult),
                         reads=[("R", ryb), ("sgbs", oc), ("b_conv_out", oc)], writes=["t2"])
                    yield P.op("dve", lambda e, oc=oc: e.tensor_tensor(out=mT[:, oc, :], in0=t1s[:, oc, :], in1=t2[:], op=ALU.add),
                         reads=[("t1s", oc), "t2"], writes=[("mT", oc)])
                MT = [("mT", oc) for oc in range(8)]

            def p2_tail(gi):
                b = gi // GPS
                G = gi % GPS
                s0 = G * 512
                c0 = gi * 512
                MT = [("mT", oc) for oc in range(8)]
                for j in range(4):
                    tile_i = gi * 4 + j
                    for n in range(2):
                        rh = rbankB()
                        for oc in range(8):
                            yield P.op("pe", lambda e, j=j, n=n, oc=oc, rh=rh: e.matmul(R_ps[:, rh, :], lhsT=mT[:, oc, j * 128:(j + 1) * 128], rhs=wo[:, oc, n * 512:(n + 1) * 512],
                                                                                 start=(oc == 0), stop=(oc == 7)), reads=MT + [("wo", oc)], writes=[("R", rh)])
                        yield P.op("dve", lambda e, j=j, n=n, rh=rh: e.tensor_tensor(out=h1t[:, j % 2, n * 512:(n + 1) * 512], in0=R_ps[:, rh, :], in1=xt[:, j, n * 512:(n + 1) * 512], op=ALU.add),
                             reads=[("R", rh), ("xt", j)], writes=[("h1t", j % 2, n)])
                    yield P.dma("sp", lambda e, r0=tile_i * 128, j=j: e.dma_start(out=h1_d[r0:r0 + 128, :], in_=h1t[:, j % 2, :]), reads=[("h1t", j % 2, 0), ("h1t", j % 2, 1)])
                    ssq, kssq = stat()
                    yield P.op("act", lambda e, ssq=ssq, j=j: e.activation(out=tb[:, j, :], in_=h1t[:, j % 2, :], func=AF.Square, accum_out=ssq),
                         reads=[("h1t", j % 2, 0), ("h1t", j % 2, 1)], writes=[("tb", j), kssq])
                    lnv, klnv = stat()
                    yield P.op("act", lambda e, ssq=ssq, lnv=lnv: e.activation(out=lnv, in_=ssq, func=AF.Ln, scale=1.0 / 1024, bias=EPS), reads=[kssq], writes=[klnv])
                    rstd, krstd = stat()
                    yield P.op("act", lambda e, lnv=lnv, rstd=rstd: e.activation(out=rstd, in_=lnv, func=AF.Exp, scale=-0.5), reads=[klnv], writes=[krstd])
                    yield P.op("dve", lambda e, rstd=rstd, j=j: e.scalar_tensor_tensor(out=tn[:], in0=h1t[:, j % 2, :], scalar=rstd, in1=gffn[:], op0=ALU.mult, op1=ALU.mult),
                         reads=[("h1t", j % 2, 0), ("h1t", j % 2, 1), krstd, "gffn"], writes=["tn"])
                    yield P.op("act", lambda e, j=j: e.activation(out=tb[:, j, :], in_=tn[:], func=AF.Copy), reads=["tn"], writes=[("tb", j)])
                    for k in range(8):
                        yield P.op("pe", lambda e, k=k: e.transpose(out=TF_ps[:, k * 128:(k + 1) * 128], in_=tn[:, k * 128:(k + 1) * 128], identity=cst_f[:, 0, :]),
                             reads=["tn", "cst_f"], writes=["TF_ps"])
                    yield P.op("act", lambda e: e.activation(out=tT[:], in_=TF_ps[:].rearrange("p (k t) -> p k t", k=8), func=AF.Copy), reads=["TF_ps"], writes=["tT"])
                    rl = rbankB()
                    for k in range(8):
                        yield P.op("pe", lambda e, k=k, rl=rl: e.matmul(R_ps[:, rl, 0:36], lhsT=tT[:, k, :], rhs=wrt[:, k, :], start=(k == 0), stop=(k == 7)),
                             reads=["tT", "wrt"], writes=[("R", rl)])
                    yield P.op("dve", lambda e, rl=rl, j=j: e.tensor_tensor(out=L4[:, j, :], in0=R_ps[:, rl, 0:36], in1=brt[:], op=ALU.add), reads=[("R", rl), "brt"], writes=[("L4", j)])

            def p2_route(gi):
                b = gi // GPS
                G = gi % GPS
                s0 = G * 512
                c0 = gi * 512
                KL = [("L4", j) for j in range(4)]
                Lg4 = L4[:, :, 0:4]
                Lx = L4[:, :, 4:36].rearrange("p t (g j) -> p t g j", g=4)

                def bc_last(ap2, n):
                    return bass.AP(ap2.tensor, ap2.offset, [list(ap2.ap[0]), list(ap2.ap[1]), [0, n]])

                def col(buf3, c):
                    a = buf3[:, :, c:c + 1]
                    return bass.AP(a.tensor, a.offset, [list(a.ap[0]), list(a.ap[1])])
                P.op("dve", lambda e: e.tensor_reduce(out=gmax4[:], in_=Lg4, axis=AX.X, op=ALU.max), reads=KL, writes=["gmax4"])
                P.op("dve", lambda e: e.tensor_tensor(out=ohg4[:], in0=Lg4, in1=bc_last(gmax4[:], 4), op=ALU.is_equal), reads=KL + ["gmax4"], writes=["ohg4"])
                P.op("dve", lambda e: e.tensor_tensor(out=ge4[:], in0=Lg4, in1=bc_last(gmax4[:], 4), op=ALU.subtract), reads=KL + ["gmax4"], writes=["ge4"])
                P.op("act", lambda e: e.activation(out=ge4[:], in_=ge4[:], func=AF.Exp), reads=["ge4"], writes=["ge4"])
                P.op("dve", lambda e: e.tensor_reduce(out=gsum4[:], in_=ge4[:], axis=AX.X, op=ALU.add), reads=["ge4"], writes=["gsum4"])
                P.op("dve", lambda e: e.reciprocal(out=gsum4[:], in_=gsum4[:]), reads=["gsum4"], writes=["gsum4"])
                ohg_bj = bass.AP(ohg4[:].tensor, ohg4[:].offset, [list(ohg4[:].ap[0]), [4, 4], [1, 4], [0, 8]])
                P.op("dve", lambda e: e.tensor_tensor(out=prod4[:], in0=Lx, in1=ohg_bj, op=ALU.mult), reads=KL + ["ohg4"], writes=["prod4"])
                P.op("dve", lambda e: e.tensor_reduce(out=els4[:], in_=prod4[:].rearrange("p t g j -> p t j g"), axis=AX.X, op=ALU.add), reads=["prod4"], writes=["els4"])
                for t in range(4):
                    P.op("dve", lambda e, t=t: e.max(out=top84[:, t, :], in_=els4[:, t, :]), reads=["els4"], writes=[("top84", t)])
                T8 = [("top84", t) for t in range(4)]
                for i in range(2):
                    P.op("dve", lambda e, i=i: e.tensor_tensor(out=m124[:, i, :, :], in0=els4[:], in1=bc_last(col(top84, i), 8), op=ALU.is_equal),
                         reads=["els4"] + T8, writes=[("m124", i)])
                P.op("dve", lambda e: e.tensor_tensor(out=d214[:], in0=col(top84, 1), in1=col(top84, 0), op=ALU.subtract), reads=T8, writes=["d214"])
                P.op("act", lambda e: e.activation(out=d214[:], in_=d214[:], func=AF.Exp), reads=["d214"], writes=["d214"])
                P.op("dve", lambda e: e.tensor_scalar(out=den4[:], in0=d214[:], scalar1=1.0, scalar2=None, op0=ALU.add), reads=["d214"], writes=["den4"])
                P.op("dve", lambda e: e.reciprocal(out=den4[:], in_=den4[:]), reads=["den4"], writes=["den4"])
                P.op("dve", lambda e: e.tensor_tensor(out=col(rinfo4, 2), in0=gsum4[:], in1=den4[:], op=ALU.mult), reads=["gsum4", "den4", "rinfo4"], writes=["rinfo4"])
                P.op("dve", lambda e: e.tensor_tensor(out=col(rinfo4, 3), in0=col(rinfo4, 2), in1=d214[:], op=ALU.mult), reads=["rinfo4", "d214"], writes=["rinfo4"])
                for i in range(2):
                    mi = m124[:, i, :, :]
                    m_bg = bass.AP(mi.tensor, mi.offset, [list(mi.ap[0]), [8, 4], [0, 4], [1, 8]])
                    P.op("dve", lambda e, i=i, m_bg=m_bg: e.tensor_tensor(out=s124[:, i, :, :].rearrange("p t (g j) -> p t g j", g=4), in0=m_bg, in1=ohg_bj, op=ALU.mult),
                         reads=[("m124", i), "ohg4"], writes=[("s124", i)])
                P.op("dve", lambda e: e.tensor_tensor(out=sel4[:], in0=s124[:, 0, :, :], in1=s124[:, 1, :, :], op=ALU.add), reads=[("s124", 0), ("s124", 1)], writes=["sel4"])
                rc = rbankB()
                for t in range(4):
                    P.op("pe", lambda e, rc=rc, t=t: e.matmul(R_ps[:, rc, t * 32:(t + 1) * 32], lhsT=cst[:, 2, :], rhs=sel4[:, t, :], start=True, stop=(t == 0), skip_group_check=True),
                         reads=["sel4", "cst"], writes=[("R", rc)])
                    for tq in range(t):
                        P.op("pe", lambda e, rc=rc, t=t, tq=tq: e.matmul(R_ps[:, rc, t * 32:(t + 1) * 32], lhsT=ones_bf[:], rhs=sel4[:, tq, :], start=False, stop=(tq == t - 1), skip_group_check=True),
                             reads=["sel4", "ones_bf"], writes=[("R", rc)])
                rt_ = rbankB()
                for t in range(4):
                    P.op("pe", lambda e, rt_=rt_, t=t: e.matmul(R_ps[:, rt_, 0:32], lhsT=ones_bf[:], rhs=sel4[:, t, :], start=(t == 0), stop=(t == 3)),
                         reads=["sel4", "ones_bf"], writes=[("R", rt_)])
                base_b = bass.AP(base_bc[:].tensor, base_bc[:].offset, [list(base_bc[:].ap[0]), [0, 4], [1, 32]])
                P.op("dve", lambda e, rc=rc: e.tensor_tensor(out=dest4[:], in0=R_ps[:, rc, 0:128].rearrange("p (t e) -> p t e", t=4), in1=base_b, op=ALU.add),
                     reads=[("R", rc), "base_bc"], writes=["dest4"])
                P.op("dve", lambda e, rt_=rt_: e.tensor_tensor(out=base_bc[:], in0=R_ps[:, rt_, 0:32], in1=base_bc[:], op=ALU.add), reads=[("R", rt_), "base_bc"], writes=["base_bc"])
                for i in range(2):
                    P.op("dve", lambda e, i=i: e.tensor_tensor(out=tmp324[:], in0=s124[:, i, :, :], in1=dest4[:], op=ALU.mult), reads=[("s124", i), "dest4"], writes=["tmp324"])
                    P.op("dve", lambda e, i=i: e.tensor_reduce(out=col(rinfo4, i), in_=tmp324[:], axis=AX.X, op=ALU.add), reads=["tmp324", "rinfo4"], writes=["rinfo4"])
                P.op("dve", lambda e: e.tensor_copy(out=idx4[:], in_=rinfo4[:, :, 0:2]), reads=["rinfo4"], writes=["idx4"])
                for t in range(4):
                    for i in range(2):
                        P.dma("pool", lambda e, t=t, i=i: e.indirect_dma_start(
                            out=xs_d[:, :], out_offset=bass.IndirectOffsetOnAxis(ap=idx4[:, t, i:i + 1], axis=0), in_=tb[:, t, :], in_offset=None),
                            reads=["idx4", ("tb", t)], writes=["xs_d"])
                P.dma("sp", lambda e, r0=gi * 512: e.dma_start(out=rt_d[r0:r0 + 512, :].rearrange("(t p) c -> p t c", p=128), in_=rinfo4[:]), reads=["rinfo4"])

            P.dma("sp", lambda e: e.dma_start(out=uT[:], in_=uT_d.rearrange("(k p) t -> p k t", p=128)[:, :, 0:512]), writes=["uT"])
            P.dma("sp", lambda e: e.dma_start(out=attnT[:], in_=attnT_d.rearrange("(a p) t -> p a t", p=128)[:, :, 0:512]), writes=["attnT"])
            zip_run2(p2_front_a(0), None)
            zip_run2(p2_front_b(0), None)
            for gi in range(NG):
                b = gi // GPS
                s0 = (gi % GPS) * 512
                for j in range(4):
                    P.dma("sp", lambda e, j=j, b=b, r0=s0 + j * 128: e.dma_start(out=xt[:, j, :], in_=x[b, r0:r0 + 128, :]),
                          writes=[("xt", j)])
                zip_run2(p2_front_a(gi + 1) if gi + 1 < NG else None, p2_mid(gi), 1)
                zip_run2(p2_tail(gi), p2_front_b(gi + 1) if gi + 1 < NG else None, 3)
                p2_route(gi)
            P.flush()


        if 3 in phases:
          with ExitStack() as st:
            def sb(n, s_, d):
                return st.enter_context(nc.sbuf_tensor(n, s_, d))

            def ps(n, s_, d):
                return st.enter_context(nc.psum_tensor(n, s_, d))

            cst_f = sb("cst_f3", [128, 4, 128], F32)
            cst = sb("cst3", [128, 4, 128], BF16)
            wb1 = sb("wb1", [128, 2, 8, 512], BF16)
            wb3 = sb("wb3", [128, 2, 8, 512], BF16)
            wb2 = sb("wb2", [128, 2, 4, 1024], BF16)
            stage = sb("stage3", [128, 4, 1024], F32)
            stage2 = sb("stage3b", [128, 4, 1024], F32)
            xsb = sb("xsb", [128, 2, NBLK, 1024], BF16)
            XT = sb("XT", [128, 2, 8, CAP], BF16)
            hT = sb("hT", [128, 4, CAP], BF16)
            sil = sb("sil", [128, 2, 512], F32)
            ysb = sb("ysb", [128, NBLK, 1024], BF16)
            R_ps = ps("R_ps3", [128, 6, 512], F32)
            T_ps = ps("T_ps3", [128, 2, 1024], BF16)
            rrot = [0]

            def rbank():
                i = rrot[0] % 6
                rrot[0] += 1
                return i

            P.dma("sp", lambda e: e.dma_start(out=cst_f[:], in_=consts[:, :, :]), writes=["cst_f"])
            P.op("dve", lambda e: e.tensor_copy(out=cst[:], in_=cst_f[:]), reads=["cst_f"], writes=["cst"])
            wl = [0]
            blkc = [0]

            def xs_dma(ex):
                eb = ex % 2
                P.dma("sp", lambda e, eb=eb, ex=ex: e.dma_start(
                    out=xsb[:, eb, :, :], in_=xs_d[ex * CAP:(ex + 1) * CAP, :].rearrange("(n p) c -> p n c", p=128)), writes=[("xsb", eb)])

            def load_w13(ex):
                eb = ex % 2
                for kk in range(4):
                    for (wsrc, wdst, nm) in ((w1, wb1, "wb1"), (w3, wb3, "wb3")):
                        sl = wl[0] % 4
                        wl[0] += 1
                        P.dma("sp", lambda e, sl=sl, wsrc=wsrc, ex=ex, kk=kk: e.dma_start(
                            out=stage[:, sl, :].rearrange("p (k f) -> p k f", k=2),
                            in_=wsrc[ex, kk * 256:(kk + 1) * 256, :].rearrange("(k p) f -> p k f", p=128)), writes=[("stage", sl)])
                        P.op("pool", lambda e, sl=sl, wdst=wdst, eb=eb, kk=kk: e.tensor_copy(
                            out=wdst[:, eb, 2 * kk:2 * kk + 2, :], in_=stage[:, sl, :].rearrange("p (k f) -> p k f", k=2)),
                            reads=[("stage", sl)], writes=[(nm, eb, kk)])

            def load_w2_dma(ex):
                for f in range(4):
                    P.dma("sp", lambda e, ex=ex, f=f: e.dma_start(out=stage2[:, f, :], in_=w2[ex, f * 128:(f + 1) * 128, :]), writes=[("stage2", f)])

            def cast_w2(ex, eng):
                eb = ex % 2
                for f in range(4):
                    if eng == "act":
                        P.op("act", lambda e, eb=eb, f=f: e.activation(out=wb2[:, eb, f, :], in_=stage2[:, f, :], func=AF.Copy),
                             reads=[("stage2", f)], writes=[("wb2", eb, f)])
                    else:
                        P.op("pool", lambda e, eb=eb, f=f: e.tensor_copy(out=wb2[:, eb, f, :], in_=stage2[:, f, :]),
                             reads=[("stage2", f)], writes=[("wb2", eb, f)])

            def zip_run3(ga, gb, ratio):
                a_done = ga is None
                b_done = gb is None
                while not (a_done and b_done):
                    if not a_done:
                        try:
                            next(ga)
                        except StopIteration:
                            a_done = True
                    for _ in range(ratio):
                        if not b_done:
                            try:
                                next(gb)
                            except StopIteration:
                                b_done = True

            def gather_T(ex):
                eb = ex % 2
                for blk in range(NBLK):
                    bs = blkc[0] % 2
                    blkc[0] += 1
                    for k in range(8):
                        yield P.op("pe", lambda e, bs=bs, k=k, blk=blk, eb=eb: e.transpose(out=T_ps[:, bs, k * 128:(k + 1) * 128], in_=xsb[:, eb, blk, k * 128:(k + 1) * 128], identity=cst[:, 0, :]),
                                   reads=[("xsb", eb), "cst"], writes=[("T_ps", bs)])
                    if blk % 2 == 0:
                        yield P.op("dve", lambda e, bs=bs, blk=blk, eb=eb: e.tensor_copy(out=XT[:, eb, :, blk * 128:(blk + 1) * 128], in_=T_ps[:, bs, :].rearrange("p (k t) -> p k t", k=8)),
                                   reads=[("T_ps", bs)], writes=[("XT", eb, blk)])
                    else:
                        yield P.op("act", lambda e, bs=bs, blk=blk, eb=eb: e.activation(out=XT[:, eb, :, blk * 128:(blk + 1) * 128], in_=T_ps[:, bs, :].rearrange("p (k t) -> p k t", k=8), func=AF.Copy),
                                   reads=[("T_ps", bs)], writes=[("XT", eb, blk)])

            def expert_compute(ex):
                eb = ex % 2
                W1 = [("wb1", eb, kk) for kk in range(4)]
                W3 = [("wb3", eb, kk) for kk in range(4)]
                W2 = [("wb2", eb, f) for f in range(4)]
                for nt in range(CAP // 512):
                    XTK = [("XT", eb, nt * 4 + i) for i in range(4)]
                    for f in range(4):
                        r1 = rbank()
                        for k in range(8):
                            yield P.op("pe", lambda e, f=f, k=k, nt=nt, r1=r1, eb=eb: e.matmul(R_ps[:, r1, :], lhsT=wb1[:, eb, k, f * 128:(f + 1) * 128], rhs=XT[:, eb, k, nt * 512:(nt + 1) * 512],
                                                                                        start=(k == 0), stop=(k == 7)), reads=W1 + XTK, writes=[("R", r1)])
                        r3 = rbank()
                        for k in range(8):
                            yield P.op("pe", lambda e, f=f, k=k, nt=nt, r3=r3, eb=eb: e.matmul(R_ps[:, r3, :], lhsT=wb3[:, eb, k, f * 128:(f + 1) * 128], rhs=XT[:, eb, k, nt * 512:(nt + 1) * 512],
                                                                                        start=(k == 0), stop=(k == 7)), reads=W3 + XTK, writes=[("R", r3)])
                        ss = f % 2
                        yield P.op("act", lambda e, r1=r1, ss=ss: e.activation(out=sil[:, ss, :], in_=R_ps[:, r1, :], func=AF.Silu), reads=[("R", r1)], writes=[("sil", ss)])
                        yield P.op("dve", lambda e, r3=r3, ss=ss, f=f, nt=nt: e.tensor_tensor(out=hT[:, f, nt * 512:(nt + 1) * 512], in0=R_ps[:, r3, :], in1=sil[:, ss, :], op=ALU.mult),
                                   reads=[("R", r3), ("sil", ss)], writes=[("hT", f, nt)])
                if ex + 1 < NE:
                    cast_w2(ex + 1, "act")
                for blk in range(NBLK):
                    for n in range(2):
                        ry = rbank()
                        for f in range(4):
                            yield P.op("pe", lambda e, f=f, n=n, blk=blk, ry=ry, eb=eb: e.matmul(R_ps[:, ry, :], lhsT=hT[:, f, blk * 128:(blk + 1) * 128], rhs=wb2[:, eb, f, n * 512:(n + 1) * 512],
                                                                                          start=(f == 0), stop=(f == 3)), reads=W2 + [("hT", f, blk // 4)], writes=[("R", ry)])
                        if n == 0:
                            yield P.op("act", lambda e, ry=ry, blk=blk, n=n: e.activation(out=ysb[:, blk, n * 512:(n + 1) * 512], in_=R_ps[:, ry, :], func=AF.Copy),
                                       reads=[("R", ry)], writes=[("ysb", blk, n)])
                        else:
                            yield P.op("dve", lambda e, ry=ry, blk=blk, n=n: e.tensor_copy(out=ysb[:, blk, n * 512:(n + 1) * 512], in_=R_ps[:, ry, :]),
                                       reads=[("R", ry)], writes=[("ysb", blk, n)])
                yield P.dma("sp", lambda e, ex=ex: e.dma_start(out=ys_d[ex * CAP:(ex + 1) * CAP, :].rearrange("(n p) c -> p n c", p=128), in_=ysb[:]),
                            reads=[("ysb", blk, n) for blk in range(NBLK) for n in range(2)])

            xs_dma(0)
            load_w13(0)
            load_w2_dma(0)
            cast_w2(0, "pool")
            xs_dma(1)
            zip_run3(gather_T(0), None, 1)
            for ex in range(NE):
                if ex + 1 < NE:
                    load_w13(ex + 1)
                    load_w2_dma(ex + 1)
                if ex + 2 < NE:
                    xs_dma(ex + 2)
                zip_run3(gather_T(ex + 1) if ex + 1 < NE else None, expert_compute(ex), 4)
            P.flush()

        if 4 in phases:
          with ExitStack() as st:
            def sb(n, s_, d):
                return st.enter_context(nc.sbuf_tensor(n, s_, d))

            def ps(n, s_, d):
                return st.enter_context(nc.psum_tensor(n, s_, d))

            cst_f = sb("cst_f4", [128, 4, 128], F32)
            cst = sb("cst4", [128, 4, 128], BF16)
            wpg = sb("wpg", [128, 8, 1024], BF16)
            wpp = sb("wpp", [128, 2, 1024], BF16)
            stage = sb("stage4", [128, 2, 1024], F32)
            gple = sb("gple", [128, 1024], F32)
            bple = sb("bple", [128, 1024], F32)
            h1t = sb("h1t4", [128, 3, 1024], F32)
            yA = sb("yA", [128, 3, 1024], BF16)
            yB = sb("yB", [128, 3, 1024], BF16)
            h2 = sb("h2", [128, 2, 1024], F32)
            hn = sb("hn", [128, 2, 1024], BF16)
            hnT = sb("hnT", [128, 2, 8, 128], BF16)
            pt = sb("pt", [128, 3, 256], F32)
            pb = sb("pb", [128, 2, 256], BF16)
            pT = sb("pT4", [128, 2, 2, 128], BF16)
            gl = sb("gl", [128, 2, 1024], F32)
            ot = sb("ot", [128, 2, 1024], F32)
            rinfo = sb("rinfo4", [128, NTILE, 4], F32)
            idx = sb("idx4", [128, NTILE, 2], I32)
            junk = sb("junk4", [128, 1024], BF16)
            rs_ = sb("rs4", [128, 16], F32)
            R_ps = ps("R_ps4", [128, 5, 512], F32)
            T_ps = ps("T_ps4", [128, 3, 1024], BF16)
            rrot = [0]

            def rbank():
                i = rrot[0] % 5
                rrot[0] += 1
                return i
            stc = [0]

            def stat():
                i = stc[0] % 16
                stc[0] += 1
                return rs_[:, i:i + 1], ("rs_", i)

            P.dma("sp", lambda e: e.dma_start(out=cst_f[:], in_=consts[:, :, :]), writes=["cst_f"])
            P.op("dve", lambda e: e.tensor_copy(out=cst[:], in_=cst_f[:]), reads=["cst_f"], writes=["cst"])
            P.dma("sp", lambda e: e.dma_start(out=gple[:], in_=bcast_rows(vec["norm_ple"], 128, 1024)), writes=["gple"])
            P.dma("sp", lambda e: e.dma_start(out=bple[:], in_=bcast_rows(vec["b_ple_gate"], 128, 1024)), writes=["bple"])
            wl = [0]
            for k in range(8):
                sl = wl[0] % 2
                wl[0] += 1
                P.dma("sp", lambda e, sl=sl, k=k: e.dma_start(out=stage[:, sl, :], in_=w_pg[k * 128:(k + 1) * 128, :]), writes=[("stage", sl)])
                P.op("pool", lambda e, sl=sl, k=k: e.tensor_copy(out=wpg[:, k, :], in_=stage[:, sl, :]), reads=[("stage", sl)], writes=[("wpg", k)])
            for k in range(2):
                sl = wl[0] % 2
                wl[0] += 1
                P.dma("sp", lambda e, sl=sl, k=k: e.dma_start(out=stage[:, sl, :], in_=w_pp[k * 128:(k + 1) * 128, :]), writes=[("stage", sl)])
                P.op("pool", lambda e, sl=sl, k=k: e.tensor_copy(out=wpp[:, k, :], in_=stage[:, sl, :]), reads=[("stage", sl)], writes=[("wpp", k)])
            WPG = [("wpg", k) for k in range(8)]
            WPP = [("wpp", k) for k in range(2)]
            def p4_loads(ti):
                b = ti // TPS
                r0 = (ti % TPS) * 128
                s = ti % 3
                P.dma("pool", lambda e, s=s, ti=ti: e.indirect_dma_start(out=yA[:, s, :], out_offset=None, in_=ys_d[:, :],
                                                                 in_offset=bass.IndirectOffsetOnAxis(ap=idx[:, ti, 0:1], axis=0)), reads=["idx"], writes=[("yA", s)])
                P.dma("pool", lambda e, s=s, ti=ti: e.indirect_dma_start(out=yB[:, s, :], out_offset=None, in_=ys_d[:, :],
                                                                 in_offset=bass.IndirectOffsetOnAxis(ap=idx[:, ti, 1:2], axis=0)), reads=["idx"], writes=[("yB", s)])
                P.dma("sp", lambda e, s=s, ti=ti: e.dma_start(out=h1t[:, s, :], in_=h1_d[ti * 128:(ti + 1) * 128, :]), writes=[("h1t", s)])
                P.dma("sp", lambda e, s=s, b=b, r0=r0: e.dma_start(out=pt[:, s, :], in_=pin[b, r0:r0 + 128, :]), writes=[("pt", s)])

            P.dma("sp", lambda e: e.dma_start(out=rinfo[:], in_=rt_d.rearrange("(t p) c -> p t c", p=128)), writes=["rinfo"])
            P.op("pool", lambda e: e.tensor_copy(out=idx[:], in_=rinfo[:, :, 0:2]), reads=["rinfo"], writes=["idx"])
            p4_loads(0)
            p4_loads(1)
            def p4_front(ti):
                b = ti // TPS
                r0 = (ti % TPS) * 128
                s = ti % 2
                sl = ti % 3
                yield P.op("dve", lambda e, s=s, sl=sl, ti=ti: e.scalar_tensor_tensor(out=h2[:, s, :], in0=yA[:, sl, :], scalar=rinfo[:, ti, 2:3], in1=h1t[:, sl, :], op0=ALU.mult, op1=ALU.add),
                     reads=[("yA", sl), "rinfo", ("h1t", sl)], writes=[("h2", s)])
                yield P.op("dve", lambda e, s=s, sl=sl, ti=ti: e.scalar_tensor_tensor(out=h2[:, s, :], in0=yB[:, sl, :], scalar=rinfo[:, ti, 3:4], in1=h2[:, s, :], op0=ALU.mult, op1=ALU.add),
                     reads=[("yB", sl), "rinfo", ("h2", s)], writes=[("h2", s)])
                ssq, kssq = stat()
                yield P.op("act", lambda e, s=s, ssq=ssq: e.activation(out=junk[:], in_=h2[:, s, :], func=AF.Square, accum_out=ssq), reads=[("h2", s)], writes=["junk", kssq])
                lnv, klnv = stat()
                yield P.op("act", lambda e, ssq=ssq, lnv=lnv: e.activation(out=lnv, in_=ssq, func=AF.Ln, scale=1.0 / 1024, bias=EPS), reads=[kssq], writes=[klnv])
                rstd, krstd = stat()
                yield P.op("act", lambda e, lnv=lnv, rstd=rstd: e.activation(out=rstd, in_=lnv, func=AF.Exp, scale=-0.5), reads=[klnv], writes=[krstd])
                yield P.op("dve", lambda e, s=s, rstd=rstd: e.scalar_tensor_tensor(out=hn[:, s, :], in0=h2[:, s, :], scalar=rstd, in1=gple[:], op0=ALU.mult, op1=ALU.mult),
                     reads=[("h2", s), krstd, "gple"], writes=[("hn", s)])
                for k in range(8):
                    yield P.op("pe", lambda e, k=k, s=s: e.transpose(out=T_ps[:, s, k * 128:(k + 1) * 128], in_=hn[:, s, k * 128:(k + 1) * 128], identity=cst[:, 0, :]),
                         reads=[("hn", s), "cst"], writes=[("T_ps", s)])
                yield P.op("act", lambda e, s=s: e.activation(out=hnT[:, s, :, :], in_=T_ps[:, s, :].rearrange("p (k t) -> p k t", k=8), func=AF.Copy), reads=[("T_ps", s)], writes=[("hnT", s)])
                yield P.op("act", lambda e, s=s, sl=sl: e.activation(out=pb[:, s, :], in_=pt[:, sl, :], func=AF.Copy), reads=[("pt", sl)], writes=[("pb", s)])
                for k in range(2):
                    yield P.op("pe", lambda e, k=k, s=s: e.transpose(out=T_ps[:, 2, s * 256 + k * 128:s * 256 + (k + 1) * 128], in_=pb[:, s, k * 128:(k + 1) * 128], identity=cst[:, 0, :]),
                         reads=[("pb", s), "cst"], writes=[("T_psp", s)])
                yield P.op("act", lambda e, s=s: e.activation(out=pT[:, s, :, :], in_=T_ps[:, 2, s * 256:(s + 1) * 256].rearrange("p (k t) -> p k t", k=2), func=AF.Copy), reads=[("T_psp", s)], writes=[("pT", s)])

            def p4_back(ti):
                b = ti // TPS
                r0 = (ti % TPS) * 128
                s = ti % 2
                sl = ti % 3
                for n in range(2):
                    rg = rbank()
                    for k in range(8):
                        yield P.op("pe", lambda e, k=k, n=n, rg=rg, s=s: e.matmul(R_ps[:, rg, :], lhsT=hnT[:, s, k, :], rhs=wpg[:, k, n * 512:(n + 1) * 512], start=(k == 0), stop=(k == 7)),
                             reads=[("hnT", s)] + WPG, writes=[("R", rg)])
                    rp = rbank()
                    for k in range(2):
                        yield P.op("pe", lambda e, k=k, n=n, rp=rp, s=s: e.matmul(R_ps[:, rp, :], lhsT=pT[:, s, k, :], rhs=wpp[:, k, n * 512:(n + 1) * 512], start=(k == 0), stop=(k == 1)),
                             reads=[("pT", s)] + WPP, writes=[("R", rp)])
                    yield P.op("dve", lambda e, n=n, rg=rg, s=s: e.tensor_tensor(out=gl[:, s, n * 512:(n + 1) * 512], in0=R_ps[:, rg, :], in1=bple[:, n * 512:(n + 1) * 512], op=ALU.add),
                         reads=[("R", rg), "bple"], writes=[("gl", s, n)])
                    yield P.op("act", lambda e, n=n, s=s: e.activation(out=gl[:, s, n * 512:(n + 1) * 512], in_=gl[:, s, n * 512:(n + 1) * 512], func=AF.Sigmoid),
                         reads=[("gl", s, n)], writes=[("gl", s, n)])
                    yield P.op("dve", lambda e, n=n, rp=rp, s=s: e.tensor_tensor(out=gl[:, s, n * 512:(n + 1) * 512], in0=R_ps[:, rp, :], in1=gl[:, s, n * 512:(n + 1) * 512], op=ALU.mult),
                         reads=[("R", rp), ("gl", s, n)], writes=[("gl", s, n)])
                    yield P.op("dve", lambda e, n=n, s=s: e.tensor_tensor(out=ot[:, s, n * 512:(n + 1) * 512], in0=gl[:, s, n * 512:(n + 1) * 512], in1=h2[:, s, n * 512:(n + 1) * 512], op=ALU.add),
                         reads=[("gl", s, n), ("h2", s)], writes=[("ot", s, n)])
                yield P.dma("sp", lambda e, s=s, b=b, r0=r0: e.dma_start(out=out[b, r0:r0 + 128, :], in_=ot[:, s, :]), reads=[("ot", s, 0), ("ot", s, 1)])

            def zip_run(ga, gb, ratio=2):
                a_done = ga is None
                b_done = gb is None
                while not (a_done and b_done):
                    if not a_done:
                        try:
                            next(ga)
                        except StopIteration:
                            a_done = True
                    for _ in range(ratio):
                        if not b_done:
                            try:
                                next(gb)
                            except StopIteration:
                                b_done = True

            zip_run(p4_front(0), None)
            for ti in range(NTILE):
                if ti + 2 < NTILE:
                    p4_loads(ti + 2)
                zip_run(p4_front(ti + 1) if ti + 1 < NTILE else None, p4_back(ti))
            P.flush()
    return nc


def make_consts(CAP):
    k = np.arange(128)
    c = np.zeros((128, 4, 128), np.float32)
    c[:, 0, :] = np.eye(128, dtype=np.float32)
    c[:, 1, :] = (k[:, None] <= k[None, :]).astype(np.float32)
    c[:, 2, :] = (k[:, None] < k[None, :]).astype(np.float32)
    blk = (k[:, None] // 64 == k[None, :] // 64).astype(np.float32) / 64.0
    c[:, 3, :] = blk
    capoff = (np.arange(32, dtype=np.float32) * CAP).reshape(1, 32)
    return c, capoff


def core_inputs(inputs, core, NB, CAP):
    f = lambda a: np.ascontiguousarray(a, dtype=np.float32)
    L = 0
    consts, capoff = make_consts(CAP)
    m = {
        "x": f(inputs["x"][core * NB:(core + 1) * NB]),
        "p": f(inputs["p"][L, core * NB:(core + 1) * NB]),
        "w_in": f(inputs["w_in"][L]),
        "w_attn_out": f(inputs["w_attn_out"][L]),
        "w_conv_out": f(inputs["w_conv_out"][L]),
        "w_o": f(inputs["w_o"][L]),
        "w_rt": f(np.concatenate([inputs["w_router_group"][L], inputs["w_router_expert"][L]], axis=1)),
        "b_rt": f(np.concatenate([inputs["b_router_group"][L], inputs["b_router_expert"][L]])[None, :]),
        "w1": f(inputs["w1"][L]), "w3": f(inputs["w3"][L]), "w2": f(inputs["w2"][L]),
        "w_ple_gate": f(inputs["w_ple_gate"][L]), "w_ple_proj": f(inputs["w_ple_proj"][L]),
        "consts": consts, "capoff": capoff,
        "conv_w": f(inputs["conv_w"][L].T),
    }
    for n in ["norm_mix", "norm_ffn", "norm_ple", "b_ple_gate", "lambda_q1", "lambda_k1", "lambda_q2",
              "lambda_k2", "subln", "b_conv_in", "b_gate", "conv_b", "conv_ln_g", "conv_ln_b", "b_conv_out"]:
        m[n] = f(inputs[n][L]).reshape(1, -1)
    m["q_norm"] = f(inputs["q_norm"][L]).reshape(1, 128)
    m["k_norm"] = f(inputs["k_norm"][L]).reshape(1, 128)
    return m


_CACHE = {}


def kernel(**inputs):
    NB, CAP = 2, 1024
    if "nc" not in _CACHE:
        _CACHE["nc"] = build_program(S=4096, NB=NB, CAP=CAP)
    nc = _CACHE["nc"]
    in_maps = [core_inputs(inputs, c, NB, CAP) for c in range(8)]
    res = run_bass_kernel_spmd(nc, in_maps, core_ids=list(range(8)))
    return np.concatenate([np.asarray(r["out"]) for r in res.results], axis=0).astype(np.float32)
```

```python
from contextlib import ExitStack

import numpy as np
import concourse.bass as bass
import concourse.mybir as mybir
from concourse.bass_utils import run_bass_kernel_spmd

F32 = mybir.dt.float32
BF16 = mybir.dt.bfloat16
I32 = mybir.dt.int32
AF = mybir.ActivationFunctionType
ALU = mybir.AluOpType
AX = mybir.AxisListType

ENGS = ("pe", "act", "dve", "pool", "sp")
NDMASEM = 8
EPS = 1e-6
STOP = [99]
LAMBDA_INIT = 0.2


class Op:
    __slots__ = ("eng", "fn", "deps", "isdma", "tok", "marked", "cum")

    def __init__(self, eng, fn, isdma):
        self.eng = eng
        self.fn = fn
        self.deps = []
        self.isdma = isdma
        self.tok = None
        self.marked = False
        self.cum = None


class Prog:
    SEM_LIMIT = 12000

    def __init__(self, nc, stack):
        self.nc = nc
        self.stack = stack
        self.nsem = 0
        self.sems = {e: self._newsem(e) for e in ENGS}
        self.finals = []
        self.dsems = {e: [stack.enter_context(nc.semaphore("d_%s%d" % (e, i)))
                          for i in range(NDMASEM)] for e in ("sp", "pool")}
        self.dcount = {e: 0 for e in self.dsems}
        self.base = {e: 0 for e in ENGS}
        self._reset()

    def _newsem(self, e):
        self.nsem += 1
        return self.stack.enter_context(self.nc.semaphore("s_%s_%d" % (e, self.nsem)))

    def _reset(self):
        self.ops = {e: [] for e in ENGS}
        self.lastw = {}
        self.readers = {}

    def _add(self, eng, fn, reads, writes, isdma):
        op = Op(eng, fn, isdma)
        deps = []
        for r in reads:
            w = self.lastw.get(r)
            if w is not None:
                deps.append(w)
        for w_ in writes:
            w = self.lastw.get(w_)
            if w is not None:
                deps.append(w)
            deps.extend(self.readers.get(w_, ()))
        op.deps = list(dict.fromkeys(deps))
        self.ops[eng].append(op)
        for r in reads:
            self.readers.setdefault(r, []).append(op)
        for w_ in writes:
            self.lastw[w_] = op
            self.readers[w_] = []
        if isdma:
            n = self.dcount[eng]
            self.dcount[eng] = n + 1
            sem = self.dsems[eng][n % NDMASEM]
            op.tok = (sem, 16 * (n // NDMASEM + 1))
            op.cum = (sem, 16 * (n // NDMASEM))
        return op

    def op(self, eng, fn, reads=(), writes=()):
        return self._add(eng, fn, tuple(reads), tuple(writes), False)

    def dma(self, eng, fn, reads=(), writes=()):
        return self._add(eng, fn, tuple(reads), tuple(writes), True)

    def flush(self):
        nc = self.nc
        ops = self.ops
        for e in ENGS:
            for op in ops[e]:
                for d in op.deps:
                    if not d.isdma and not (d.eng == "pe" and e == "pe"):
                        d.marked = True
            last = [o for o in ops[e] if not o.isdma]
            if last:
                last[-1].marked = True
        for e in ENGS:
            c = self.base[e]
            for op in ops[e]:
                if not op.isdma and op.marked:
                    if c >= self.SEM_LIMIT:
                        self.finals.append((self.sems[e], c))
                        self.sems[e] = self._newsem(e)
                        c = 0
                    c += 1
                    op.tok = (self.sems[e], c)
            self.base[e] = c
        prog = self

        def emit_engine(e, eng):
            waited = {}

            def wait(sem, val):
                k = id(sem)
                if val > 0 and waited.get(k, 0) < val:
                    waited[k] = val
                    eng.wait_ge(sem, val)

            for op in ops[e]:
                if op.isdma:
                    wait(*op.cum)
                for d in op.deps:
                    if d.eng == "pe" and e == "pe" and not d.isdma:
                        continue
                    wait(*d.tok)
                ins = op.fn(eng)
                if op.isdma:
                    ins.then_inc(op.tok[0], 16)
                elif op.marked:
                    ins.then_inc(op.tok[0], 1)
            for (fs, fv) in prog.finals:
                wait(fs, fv)
            for e2 in ENGS:
                wait(prog.sems[e2], prog.base[e2])
            for q in prog.dsems:
                n = prog.dcount[q]
                for i in range(NDMASEM):
                    k = (n - i + NDMASEM - 1) // NDMASEM if n > i else 0
                    wait(prog.dsems[q][i], 16 * k)

        with nc.Block() as block:
            @block.tensor
            def _(eng):
                emit_engine("pe", eng)

            @block.scalar
            def _(eng):
                emit_engine("act", eng)

            @block.vector
            def _(eng):
                emit_engine("dve", eng)

            @block.gpsimd
            def _(eng):
                emit_engine("pool", eng)

            @block.sync
            def _(eng):
                emit_engine("sp", eng)
        self._reset()


def bcast_rows(ap, nrows, ncols, off=0):
    return bass.AP(ap.tensor, off, [[0, nrows], [1, ncols]])


def build_program(S=4096, NB=2, CAP=1024, debug=(), phases=(1, 2, 3, 4)):
    nc = bass.Bass("TRN2", target_bir_lowering=False)
    NT = NB * S
    NTILE = NT // 128
    NG = NT // 512
    GPS = S // 512
    TPS = S // 128
    NE = 32
    NBLK = CAP // 128

    def din(name, shape, dt=F32):
        return nc.dram_tensor(name, shape, dt, kind="ExternalInput").ap()

    def dscr(name, shape, dt):
        kind = "ExternalOutput" if name in debug else "Internal"
        return nc.dram_tensor(name, shape, dt, kind=kind).ap()

    x = din("x", [NB, S, 1024])
    pin = din("p", [NB, S, 256])
    w_in = din("w_in", [1024, 4608])
    w_ao = din("w_attn_out", [512, 1024])
    w_co = din("w_conv_out", [512, 1024])
    w_o = din("w_o", [1024, 1024])
    w_rt = din("w_rt", [1024, 36])
    w1 = din("w1", [NE, 1024, 512])
    w3 = din("w3", [NE, 1024, 512])
    w2 = din("w2", [NE, 512, 1024])
    w_pg = din("w_ple_gate", [1024, 1024])
    w_pp = din("w_ple_proj", [256, 1024])
    consts = din("consts", [128, 4, 128])
    capoff = din("capoff", [1, 32])
    vec = {n: din(n, [1, l]) for n, l in [
        ("norm_mix", 1024), ("norm_ffn", 1024), ("norm_ple", 1024), ("b_ple_gate", 1024),
        ("b_rt", 36), ("lambda_q1", 64), ("lambda_k1", 64), ("lambda_q2", 64), ("lambda_k2", 64),
        ("q_norm", 128), ("k_norm", 128), ("subln", 128), ("b_conv_in", 1024), ("b_gate", 2048),
        ("conv_b", 512), ("conv_ln_g", 512), ("conv_ln_b", 512), ("b_conv_out", 1024)]}
    conv_w = din("conv_w", [512, 31])
    out = nc.dram_tensor("out", [NB, S, 1024], F32, kind="ExternalOutput").ap()

    uT_d = dscr("uT_d", [1024, NT], BF16)
    attnT_d = dscr("attnT_d", [512, NT], BF16)
    h1_d = dscr("h1_d", [NT, 1024], F32)
    xs_d = dscr("xs_d", [NE * CAP, 1024], BF16)
    ys_d = dscr("ys_d", [NE * CAP, 1024], BF16)
    rt_d = dscr("rt_d", [NT, 4], F32)

    def col_ap(v, n, off=0):
        return bass.AP(v.tensor, off, [[1, n], [1, 1]])

    with ExitStack() as top:
        P = Prog(nc, top)

        if 1 in phases:
          with ExitStack() as st:
            def sb(n, s, d):
                return st.enter_context(nc.sbuf_tensor(n, s, d))

            def ps(n, s, d):
                return st.enter_context(nc.psum_tensor(n, s, d))

            cst_f = sb("cst_f", [128, 4, 128], F32)
            cst = sb("cst", [128, 4, 128], BF16)
            wq = sb("wq", [128, 8, 1536], BF16)
            stage = sb("stage", [128, 2, 1536], F32)
            kT = sb("kT", [128, 4, S], BF16)
            Va = sb("Va", [128, TPS, 4, 129], BF16)
            xt = sb("xt", [128, 2, 1024], F32)
            gmix = sb("gmix", [128, 1024], F32)
            junk = sb("junk", [128, 1024], BF16)
            ub = sb("ub", [128, 2, 1024], BF16)
            uT = sb("uT", [128, 1, 8, 512], BF16)
            qT = sb("qT", [128, 2, 4, 512], BF16)
            sq = sb("sq", [128, 2, 512], BF16)
            lnb = sb("lnb", [128, 2, 512], F32)
            rsb = sb("rsb", [128, 2, 512], F32)
            pT = sb("pT", [128, 3, 2, 512], BF16)
            atm = sb("atm", [128, 4, 512], BF16)
            attnT = sb("attnT", [128, 1, 4, 512], BF16)
            o0n = sb("o0n", [128, 2, 128], F32)
            Ocp = sb("Ocp", [128, 2, 3, 387], F32)
            dd = sb("dd", [128, 2, 128], F32)
            junk2 = sb("junk2", [128, 128], BF16)
            st1 = sb("st1", [128, 64], F32)
            gqk = sb("gqk", [128, 2], F32)
            lamv = sb("lamv", [128, 4, 64], F32)
            lams = sb("lams", [128, 8], F32)

            O_ps = ps("O_ps", [128, 3, 512], F32)
            T_ps = ps("T_ps", [128, 1024], BF16)
            R_ps = ps("R_ps", [128, 4, 512], F32)
            rrot = [0]

            def rbank():
                i = rrot[0] % 3
                rrot[0] += 1
                return i

            ident = cst[:, 0, :]
            tri = cst[:, 1, :]
            bones = cst[:, 3, :]

            P.dma("sp", lambda e: e.dma_start(out=cst_f[:], in_=consts[:, :, :]), writes=["cst_f"])
            P.op("dve", lambda e: e.tensor_copy(out=cst[:], in_=cst_f[:]), reads=["cst_f"], writes=["cst"])
            P.dma("sp", lambda e: e.dma_start(out=gmix[:], in_=bcast_rows(vec["norm_mix"], 128, 1024)), writes=["gmix"])
            P.dma("sp", lambda e: e.dma_start(out=gqk[:, 0:1], in_=col_ap(vec["q_norm"], 128)), writes=["gqk0"])
            P.dma("sp", lambda e: e.dma_start(out=gqk[:, 1:2], in_=col_ap(vec["k_norm"], 128)), writes=["gqk1"])
            P.op("dve", lambda e: e.tensor_scalar(out=gqk[:, 0:1], in0=gqk[:, 0:1], scalar1=0.125, scalar2=None, op0=ALU.mult),
                 reads=["gqk0"], writes=["gqk0"])
            for i, n in enumerate(["lambda_q1", "lambda_k1", "lambda_q2", "lambda_k2"]):
                P.dma("sp", lambda e, i=i, n=n: e.dma_start(out=lamv[:, i, :], in_=bcast_rows(vec[n], 128, 64)), writes=[("lamv", i)])
            for i in range(2):
                P.op("dve", lambda e, i=i: e.tensor_tensor(
                    out=lamv[:, 2 * i, :], in0=lamv[:, 2 * i, :], in1=lamv[:, 2 * i + 1, :], op=ALU.mult),
                    reads=[("lamv", 2 * i), ("lamv", 2 * i + 1)], writes=[("lamv", 2 * i)])
                P.op("dve", lambda e, i=i: e.tensor_reduce(out=lams[:, i:i + 1], in_=lamv[:, 2 * i, :], axis=AX.X, op=ALU.add),
                     reads=[("lamv", 2 * i)], writes=[("lams", i)])
                P.op("act", lambda e, i=i: e.activation(out=lams[:, 2 + i:3 + i], in_=lams[:, i:i + 1], func=AF.Exp),
                     reads=[("lams", i)], writes=[("lams", 2 + i)])
            P.op("dve", lambda e: e.tensor_tensor(out=lams[:, 4:5], in0=lams[:, 3:4], in1=lams[:, 2:3], op=ALU.subtract),
                 reads=[("lams", 2), ("lams", 3)], writes=[("lams", 4)])
            P.op("dve", lambda e: e.tensor_scalar(out=lams[:, 5:6], in0=lams[:, 4:5], scalar1=-LAMBDA_INIT, scalar2=None, op0=ALU.add),
                 reads=[("lams", 4)], writes=["nlam"])
            nlam = lams[:, 5:6]
            P.op("pool", lambda e: e.memset(Va[:, :, :, 128:129], 1.0), writes=["Va_ones"])
            P.op("pool", lambda e: e.memset(qT[:], 0.0), writes=[("qT", c, m) for c in range(4) for m in range(2)])
            zt = sb("zt", [128, 2, 1024], BF16)
            P.op("pool", lambda e: e.memset(zt[:], 0.0), writes=["zt"])
            xs_v = xs_d.rearrange("(n p) c -> p n c", p=128)
            for i in range(NE * CAP // 256):
                P.dma("pool", lambda e, i=i: e.dma_start(out=xs_v[:, i * 2:(i + 1) * 2, :], in_=zt[:]), reads=["zt"], writes=[("xs_z", i)])
            for k in range(8):
                sl = k % 2
                P.dma("sp", lambda e, k=k, sl=sl: e.dma_start(out=stage[:, sl, :], in_=w_in[k * 128:(k + 1) * 128, 0:1536]),
                      writes=[("stage", sl)])
                P.op("pool", lambda e, k=k, sl=sl: e.tensor_copy(out=wq[:, k, :], in_=stage[:, sl, :]),
                     reads=[("stage", sl)], writes=[("wq", k)])
            WQ = [("wq", k) for k in range(8)]

            stc = [0]

            def stat():
                i = stc[0] % 64
                stc[0] += 1
                return st1[:, i:i + 1], ("st1", i)

            tilec = [0]
            deferred = []
            arot = [0]
            for gi in range(NG):
                b = gi // GPS
                G = gi % GPS
                s0 = G * 512
                us = 0
                for j in range(4):
                    tc_ = tilec[0]
                    tilec[0] += 1
                    xs = tc_ % 2
                    P.dma("sp", lambda e, xs=xs, b=b, r0=s0 + j * 128: e.dma_start(out=xt[:, xs, :], in_=x[b, r0:r0 + 128, :]),
                          writes=[("xt", xs)])
                    ssq, kssq = stat()
                    P.op("act", lambda e, xs=xs, ssq=ssq: e.activation(
                        out=junk[:], in_=xt[:, xs, :], func=AF.Square, accum_out=ssq), reads=[("xt", xs)], writes=["junk", kssq])
                    lnv, klnv = stat()
                    P.op("act", lambda e, ssq=ssq, lnv=lnv: e.activation(out=lnv, in_=ssq, func=AF.Ln, scale=1.0 / 1024, bias=EPS),
                         reads=[kssq], writes=[klnv])
                    rstd, krstd = stat()
                    P.op("act", lambda e, lnv=lnv, rstd=rstd: e.activation(out=rstd, in_=lnv, func=AF.Exp, scale=-0.5),
                         reads=[klnv], writes=[krstd])
                    P.op("dve", lambda e, xs=xs, rstd=rstd: e.scalar_tensor_tensor(
                        out=ub[:, xs, :], in0=xt[:, xs, :], scalar=rstd, in1=gmix[:], op0=ALU.mult, op1=ALU.mult),
                        reads=[("xt", xs), krstd, "gmix"], writes=[("ub", xs)])
                    for k in range(8):
                        P.op("pe", lambda e, xs=xs, k=k: e.transpose(out=T_ps[:, k * 128:(k + 1) * 128], in_=ub[:, xs, k * 128:(k + 1) * 128], identity=ident),
                             reads=[("ub", xs), "cst"], writes=["T_ps"])
                    P.op("dve", lambda e, us=us, j=j: e.tensor_copy(
                        out=uT[:, us, :, j * 128:(j + 1) * 128], in_=T_ps[:].rearrange("p (k t) -> p k t", k=8)),
                        reads=["T_ps"], writes=[("uT", us, j)])
                UT = [("uT", us, j) for j in range(4)]
                P.dma("sp", lambda e, us=us, c0=gi * 512: e.dma_start(
                    out=uT_d.rearrange("(k p) t -> p k t", p=128)[:, :, c0:c0 + 512], in_=uT[:, us, :, :]), reads=UT)

                if STOP[0] <= 1:
                    continue
                def qk_proj(c):
                    rb = c % 2
                    for k in range(8):
                        P.op("pe", lambda e, c=c, k=k, rb=rb: e.matmul(
                            R_ps[:, rb, :], lhsT=wq[:, k, c * 128:(c + 1) * 128], rhs=uT[:, us, k, :], start=(k == 0), stop=(k == 7)),
                            reads=UT + [("wq", k)], writes=[("R", rb)])
                qk_proj(0)
                for c in range(8):
                    rb = c % 2
                    s2 = c % 2
                    P.op("act", lambda e, rb=rb, s2=s2: e.activation(out=sq[:, s2, :], in_=R_ps[:, rb, :], func=AF.Square),
                         reads=[("R", rb)], writes=[("sq", s2)])
                    rb2 = 2
                    P.op("pe", lambda e, rb2=rb2, s2=s2: e.matmul(R_ps[:, rb2, :], lhsT=bones, rhs=sq[:, s2, :], start=True, stop=True),
                         reads=[("sq", s2), "cst"], writes=[("R", rb2)])
                    P.op("act", lambda e, rb2=rb2, s2=s2: e.activation(out=lnb[:, s2, :], in_=R_ps[:, rb2, :], func=AF.Ln, bias=EPS),
                         reads=[("R", rb2)], writes=[("lnb", s2)])
                    P.op("act", lambda e, s2=s2: e.activation(out=rsb[:, s2, :], in_=lnb[:, s2, :], func=AF.Exp, scale=-0.5),
                         reads=[("lnb", s2)], writes=[("rsb", s2)])
                    if c < 4:
                        for m in range(2):
                            pr = slice(m * 64, (m + 1) * 64)
                            P.op("dve", lambda e, rb=rb, s2=s2, m=m, pr=pr, c=c: e.scalar_tensor_tensor(
                                out=qT[pr, m, c, :], in0=R_ps[pr, rb, :], scalar=gqk[pr, 0:1], in1=rsb[pr, s2, :], op0=ALU.mult, op1=ALU.mult),
                                reads=[("R", rb), ("rsb", s2), "gqk0"], writes=[("qT", c, m)])
                    else:
                        P.op("dve", lambda e, rb=rb, s2=s2, c=c, s0=s0: e.scalar_tensor_tensor(
                            out=kT[:, c - 4, s0:s0 + 512], in0=R_ps[:, rb, :], scalar=gqk[:, 1:2], in1=rsb[:, s2, :], op0=ALU.mult, op1=ALU.mult),
                            reads=[("R", rb), ("rsb", s2), "gqk1"], writes=[("kT", c - 4, G)])
                    if c + 1 < 8:
                        qk_proj(c + 1)
                rrot[0] = 0
                if STOP[0] <= 2:
                    continue
                for j in range(4):
                    rb = rbank()
                    for k in range(8):
                        P.op("pe", lambda e, j=j, k=k, rb=rb: e.matmul(
                            R_ps[:, rb, :], lhsT=uT[:, us, k, j * 128:(j + 1) * 128], rhs=wq[:, k, 1024:1536], start=(k == 0), stop=(k == 7)),
                            reads=UT + [("wq", k)], writes=[("R", rb)])
                    P.op("dve", lambda e, j=j, rb=rb, tl=G * 4 + j: e.tensor_copy(
                        out=Va[:, tl, :, 0:128], in_=R_ps[:, rb, :].rearrange("p (h v) -> p h v", h=4)),
                        reads=[("R", rb)], writes=[("Va", G * 4 + j)])

                if STOP[0] <= 3:
                    continue
                nkt = 4 * G + 4
                for h in range(4):
                    steps = [(m, jp) for m in range(2) for jp in range(nkt // 2)]

                    def acc(m, c):
                        a = m * 4 + c
                        return a // 3, (a % 3) * 129

                    def emit_qk(si, h=h, steps=steps):
                        m, jp = steps[si]
                        j0 = 2 * jp
                        r0 = j0 - 4 * G
                        q0 = max(r0, 0) * 128
                        ncol = 512 - q0
                        pb_ = arot[0] % 2
                        arot[0] += 1
                        for t in range(2):
                            P.op("pe", lambda e, m=m, j=j0 + t, q0=q0, ncol=ncol, rb=2 * pb_ + t: e.matmul(
                                R_ps[:, rb, 0:ncol], lhsT=kT[:, h, j * 128:(j + 1) * 128],
                                rhs=qT[:, m, h, q0:512], start=True, stop=True),
                                reads=[("kT", h, (j0 + t) // 4), ("qT", h, m)], writes=[("R", 2 * pb_ + t)])
                        pslot = si % 3
                        P.op("act", lambda e, pb_=pb_, ncol=ncol, pslot=pslot: e.activation(
                            out=pT[:, pslot, :, 0:ncol], in_=R_ps[:, 2 * pb_:2 * pb_ + 2, 0:ncol], func=AF.Exp),
                            reads=[("R", 2 * pb_), ("R", 2 * pb_ + 1)], writes=[("pT", pslot)])
                        for t in range(2):
                            r = r0 + t
                            if r >= 0:
                                off = r * 128 - q0
                                P.op("pool", lambda e, pslot=pslot, t=t, off=off: e.tensor_tensor(
                                    out=pT[:, pslot, t, off:off + 128], in0=pT[:, pslot, t, off:off + 128], in1=tri, op=ALU.mult),
                                    reads=[("pT", pslot), "cst"], writes=[("pT", pslot)])
                        return (m, j0, r0, q0, pslot)

                    started = set()

                    def emit_pv(info, h=h, started=started):
                        m, j0, r0, q0, pslot = info
                        for t in range(2):
                            j = j0 + t
                            r = r0 + t
                            for c in range(max(r, 0), 4):
                                bank, off = acc(m, c)
                                first = bank not in started
                                started.add(bank)
                                lastj = (j == 4 * G + c)
                                P.op("pe", lambda e, pslot=pslot, t=t, c=c, q0=q0, bank=bank, off=off, first=first, lastj=lastj, j=j: e.matmul(
                                    O_ps[:, bank, off:off + 129], lhsT=pT[:, pslot, t, c * 128 - q0:c * 128 - q0 + 128],
                                    rhs=Va[:, j, h, :], start=first, stop=lastj, skip_group_check=True),
                                    reads=[("pT", pslot), ("Va", j), "Va_ones"], writes=[("O", bank)])

                    info = emit_qk(0)
                    for si in range(len(steps)):
                        nxt = emit_qk(si + 1) if si + 1 < len(steps) else None
                        emit_pv(info)
                        info = nxt
                        if deferred:
                            deferred.pop(0)()
                    hp = h % 2
                    P.op("act", lambda e, hp=hp: e.activation(out=Ocp[:, hp, 0:2, :], in_=O_ps[:, 0:2, 0:387], func=AF.Copy),
                         reads=[("O", 0), ("O", 1)], writes=[("Ocp", hp, 0)])
                    P.op("dve", lambda e, hp=hp: e.tensor_copy(out=Ocp[:, hp, 2:3, 0:258], in_=O_ps[:, 2:3, 0:258]),
                         reads=[("O", 2)], writes=[("Ocp", hp, 1)])
                    def finish_c(c, h=h, hp=hp):
                        o0 = Ocp[:, hp, c // 3, (c % 3) * 129:(c % 3) * 129 + 129]
                        o1 = Ocp[:, hp, (4 + c) // 3, ((4 + c) % 3) * 129:((4 + c) % 3) * 129 + 129]
                        k0 = ("Ocp", hp, 0)
                        k1 = ("Ocp", hp, 0) if (4 + c) // 3 < 2 else ("Ocp", hp, 1)
                        r0, kr0 = stat()
                        r1, kr1 = stat()
                        P.op("dve", lambda e, o0=o0, r0=r0: e.reciprocal(out=r0, in_=o0[:, 128:129]), reads=[k0], writes=[kr0])
                        P.op("dve", lambda e, o1=o1, r1=r1: e.reciprocal(out=r1, in_=o1[:, 128:129]), reads=[k1], writes=[kr1])
                        r1n, kr1n = stat()
                        P.op("dve", lambda e, r1=r1, r1n=r1n: e.tensor_tensor(out=r1n, in0=r1, in1=nlam, op=ALU.mult),
                             reads=[kr1, "nlam"], writes=[kr1n])
                        ds = c % 2
                        P.op("dve", lambda e, o0=o0, r0=r0, ds=ds: e.tensor_scalar(out=o0n[:, ds, :], in0=o0[:, 0:128], scalar1=r0, scalar2=None, op0=ALU.mult),
                             reads=[k0, kr0], writes=[("o0n", ds)])
                        P.op("dve", lambda e, o1=o1, r1n=r1n, ds=ds: e.scalar_tensor_tensor(
                            out=dd[:, ds, :], in0=o1[:, 0:128], scalar=r1n, in1=o0n[:, ds, :], op0=ALU.mult, op1=ALU.add),
                            reads=[k1, kr1n, ("o0n", ds)], writes=[("dd", ds)])
                        ss, kss = stat()
                        P.op("act", lambda e, ds=ds, ss=ss: e.activation(out=junk2[:], in_=dd[:, ds, :], func=AF.Square, accum_out=ss),
                             reads=[("dd", ds)], writes=["junk2", kss])
                        ln2, kln2 = stat()
                        P.op("act", lambda e, ss=ss, ln2=ln2: e.activation(out=ln2, in_=ss, func=AF.Ln, scale=1.0 / 128, bias=EPS),
                             reads=[kss], writes=[kln2])
                        rs2, krs2 = stat()
                        P.op("act", lambda e, ln2=ln2, rs2=rs2: e.activation(out=rs2, in_=ln2, func=AF.Exp, scale=-0.5),
                             reads=[kln2], writes=[krs2])
                        P.op("dve", lambda e, ds=ds, rs2=rs2, c=c, h=h: e.tensor_scalar(
                            out=atm[:, c, h * 128:(h + 1) * 128], in0=dd[:, ds, :], scalar1=rs2, scalar2=None, op0=ALU.mult),
                            reads=[("dd", ds), krs2], writes=[("atm", c, h)])
                    for c in range(4):
                        deferred.append(lambda c=c, f=finish_c: f(c))
                while deferred:
                    deferred.pop(0)()
                if STOP[0] <= 4:
                    continue
                for c in range(4):
                    for a in range(4):
                        P.op("pe", lambda e, c=c, a=a: e.transpose(out=T_ps[:, a * 128:(a + 1) * 128], in_=atm[:, c, a * 128:(a + 1) * 128], identity=ident),
                             reads=[("atm", c, a), "cst"], writes=["T_ps"])
                    P.op("dve", lambda e, us=us, c=c: e.tensor_copy(
                        out=attnT[:, us, :, c * 128:(c + 1) * 128], in_=T_ps[:, 0:512].rearrange("p (a t) -> p a t", a=4)),
                        reads=["T_ps"], writes=[("attnT", us, c)])
                P.dma("sp", lambda e, us=us, c0=gi * 512: e.dma_start(
                    out=attnT_d.rearrange("(a p) t -> p a t", p=128)[:, :, c0:c0 + 512], in_=attnT[:, us, :, :]),
                    reads=[("attnT", us, c) for c in range(4)])
            P.flush()


        if 2 in phases:
          with ExitStack() as st:
            def sb(n, s_, d):
                return st.enter_context(nc.sbuf_tensor(n, s_, d))

            def ps(n, s_, d):
                return st.enter_context(nc.psum_tensor(n, s_, d))

            cst_f = sb("cst_f2", [128, 4, 128], F32)
            cst = sb("cst2", [128, 4, 128], BF16)
            onesC = sb("onesC", [128, 128], F32)
            wcg = sb("wcg", [128, 8, 3072], BF16)
            wao = sb("wao", [128, 4, 1024], BF16)
            wco = sb("wco", [128, 4, 1024], BF16)
            wo = sb("wo", [128, 8, 1024], BF16)
            wrt = sb("wrt", [128, 8, 36], F32)
            uT = sb("uT2", [128, 8, 512], BF16)
            attnT = sb("attnT2", [128, 4, 512], BF16)
            xt = sb("xt2", [128, 4, 1024], F32)
            glu = sb("glu", [128, 4, 542], BF16)
            dgr = sb("dgr", [128, 16, 128], BF16)
            dgc = [0]
            cacc = sb("cacc", [128, 4, 512], F32)
            sqf = sb("sqf", [128, 2, 512], F32)
            mean_sb = sb("mean_sb", [128, 512], F32)
            tmp2 = sqf
            cT = sb("cT", [128, 4, 512], BF16)
            sg = sb("sg", [128, 2, 512], F32)
            sga = sb("sga", [128, 512], F32)
            sgbs = sb("sgbs", [128, 8, 512], BF16)
            t1s = sb("t1s", [128, 8, 512], BF16)
            t2 = sb("t2", [128, 512], F32)
            mT = sb("mT", [128, 8, 512], BF16)
            h1t = sb("h1t", [128, 2, 1024], F32)
            tn = sb("tn", [128, 1024], F32)
            tb = sb("tb", [128, 4, 1024], BF16)
            L4 = sb("L4", [128, 4, 36], F32)
            gmax4 = sb("gmax4", [128, 4], F32)
            ohg4 = sb("ohg4", [128, 4, 4], F32)
            ge4 = sb("ge4", [128, 4, 4], F32)
            gsum4 = sb("gsum4", [128, 4], F32)
            prod4 = sb("prod4", [128, 4, 4, 8], F32)
            els4 = sb("els4", [128, 4, 8], F32)
            top84 = sb("top84", [128, 4, 8], F32)
            m124 = sb("m124", [128, 2, 4, 8], F32)
            d214 = sb("d214", [128, 4], F32)
            den4 = sb("den4", [128, 4], F32)
            s124 = sb("s124", [128, 2, 4, 32], F32)
            sel4 = sb("sel4", [128, 4, 32], BF16)
            dest4 = sb("dest4", [128, 4, 32], F32)
            tmp324 = sb("tmp324", [128, 4, 32], F32)
            rinfo4 = sb("rinfo42", [128, 4, 4], F32)
            idx4 = sb("idx42", [128, 4, 2], I32)
            tT = sb("tT", [128, 8, 128], F32)
            gffn = sb("gffn", [128, 1024], F32)
            bci = sb("bci", [128, 8], F32)
            bg = sb("bg", [128, 16], F32)
            cvb = sb("cvb", [128, 4], F32)
            lng = sb("lng", [128, 4], F32)
            lnbb = sb("lnbb", [128, 4], F32)
            bco = sb("bco", [128, 8], F32)
            cw = sb("cw", [128, 4, 31], F32)
            sln = sb("sln", [128, 1], F32)
            brt = sb("brt", [128, 36], F32)
            base_bc = sb("base_bc", [128, 32], F32)
            ones_bf = sb("ones_bf", [128, 128], BF16)
            rs_ = sb("rs_", [128, 64], F32)

            R_ps = ps("R_ps2", [128, 6, 512], F32)
            TF_ps = ps("TF_ps", [128, 1024], F32)
            rrot = [0]

            def rbank():
                i = rrot[0] % 6
                rrot[0] += 1
                return i

            stc = [0]

            def stat():
                i = stc[0] % 56
                stc[0] += 1
                return rs_[:, i:i + 1], ("rs_", i)

            P.dma("sp", lambda e: e.dma_start(out=cst_f[:], in_=consts[:, :, :]), writes=["cst_f"])
            P.op("dve", lambda e: e.tensor_copy(out=cst[:], in_=cst_f[:]), reads=["cst_f"], writes=["cst"])
            P.op("pool", lambda e: e.memset(onesC[:], 1.0 / 512), writes=["onesC"])
            P.op("pool", lambda e: e.memset(ones_bf[:], 1.0), writes=["ones_bf"])
            P.dma("sp", lambda e: e.dma_start(out=gffn[:], in_=bcast_rows(vec["norm_ffn"], 128, 1024)), writes=["gffn"])
            P.dma("sp", lambda e: e.dma_start(out=brt[:], in_=bcast_rows(vec["b_rt"], 128, 36)), writes=["brt"])
            P.dma("sp", lambda e: e.dma_start(out=base_bc[:], in_=bcast_rows(capoff, 128, 32)), writes=["base_bc"])
            P.dma("sp", lambda e: e.dma_start(out=sln[:], in_=col_ap(vec["subln"], 128)), writes=["sln"])
            P.op("dve", lambda e: e.tensor_scalar(out=sln[:], in0=sln[:], scalar1=1.0 - LAMBDA_INIT, scalar2=None, op0=ALU.mult),
                 reads=["sln"], writes=["sln"])

            def colvec(dst, name, nchunk):
                P.dma("sp", lambda e: e.dma_start(out=dst[:, 0:nchunk], in_=bass.AP(vec[name].tensor, 0, [[1, 128], [128, nchunk]])),
                      writes=[name])
            with nc.allow_non_contiguous_dma(reason="tiny per-channel bias columns"):
                pass
            for dst, name, nch in [(bci, "b_conv_in", 8), (bg, "b_gate", 16), (cvb, "conv_b", 4), (lng, "conv_ln_g", 4),
                                   (lnbb, "conv_ln_b", 4), (bco, "b_conv_out", 8)]:
                for c in range(nch):
                    P.dma("sp", lambda e, dst=dst, name=name, c=c: e.dma_start(out=dst[:, c:c + 1], in_=col_ap(vec[name], 128, c * 128)),
                          writes=[(name, c)])
            BIAS = [(n, c) for _, n, nch in [(0, "b_conv_in", 8), (0, "b_gate", 16), (0, "conv_b", 4), (0, "conv_ln_g", 4),
                                             (0, "conv_ln_b", 4), (0, "b_conv_out", 8)] for c in range(nch)]
            for cc in range(4):
                for kk in range(31):
                    pass
            for cc in range(4):
                P.dma("sp", lambda e, cc=cc: e.dma_start(out=cw[:, cc, :], in_=conv_w[cc * 128:(cc + 1) * 128, :]),
                      writes=[("cw", cc)])
            P.dma("sp", lambda e: e.dma_start(out=wrt[:], in_=w_rt.rearrange("(k p) n -> p k n", p=128)), writes=["wrt"])
            wl = [0]

            def load_cast(dst_fn, src_fn, key, eng="pool", scale_col=None):
                sl = wl[0] % 4
                wl[0] += 1
                P.dma("sp", lambda e, sl=sl: e.dma_start(out=xt[:, sl, :], in_=src_fn()), writes=[("xt", sl)])
                if scale_col is None:
                    P.op(eng, lambda e, sl=sl: e.tensor_copy(out=dst_fn(), in_=xt[:, sl, :]), reads=[("xt", sl)], writes=[key])
                else:
                    P.op("dve", lambda e, sl=sl: e.tensor_scalar(out=dst_fn(), in0=xt[:, sl, :], scalar1=scale_col, scalar2=None, op0=ALU.mult),
                         reads=[("xt", sl), "sln"], writes=[key])
            def load_wcg(c3):
                for k in range(8):
                    load_cast(lambda k=k, c3=c3: wcg[:, k, c3 * 1024:(c3 + 1) * 1024],
                              lambda k=k, c3=c3: w_in[k * 128:(k + 1) * 128, 1536 + c3 * 1024:1536 + (c3 + 1) * 1024],
                              ("wcg", k, c3), eng="pool" if (k + c3) % 2 else "dve")
            load_wcg(0)
            for a in range(4):
                load_cast(lambda a=a: wao[:, a, :], lambda a=a: w_ao[a * 128:(a + 1) * 128, :], ("wao", a), scale_col=sln[:, 0:1])
            load_wcg(1)
            load_wcg(2)
            for a in range(4):
                load_cast(lambda a=a: wco[:, a, :], lambda a=a: w_co[a * 128:(a + 1) * 128, :], ("wco", a), eng="pool")
            for k in range(8):
                load_cast(lambda k=k: wo[:, k, :], lambda k=k: w_o[k * 128:(k + 1) * 128, :], ("wo", k), eng="pool" if k % 2 else "dve")
            WCG = [("wcg", k, 0) for k in range(8)]
            WCG12 = [("wcg", k, c3) for k in range(8) for c3 in (1, 2)]

            pa_ = [0]
            pb_ = [0]

            def rbankA():
                i = pa_[0] % 3
                pa_[0] += 1
                return i

            def rbankB():
                i = 3 + pb_[0] % 3
                pb_[0] += 1
                return i

            def zip_run2(ga, gb, ratio=1):
                a_done = ga is None
                b_done = gb is None
                while not (a_done and b_done):
                    if not a_done:
                        try:
                            next(ga)
                        except StopIteration:
                            a_done = True
                    for _ in range(ratio):
                        if not b_done:
                            try:
                                next(gb)
                            except StopIteration:
                                b_done = True

            def p2_front_a(gi):
                b = gi // GPS
                G = gi % GPS
                s0 = G * 512
                c0 = gi * 512
                for cc in range(4):
                    ra = rbankA()
                    for k in range(8):
                        yield P.op("pe", lambda e, cc=cc, k=k, ra=ra: e.matmul(R_ps[:, ra, :], lhsT=wcg[:, k, cc * 128:(cc + 1) * 128], rhs=uT[:, k, :],
                                                                       start=(k == 0), stop=(k == 7)), reads=["uT"] + WCG, writes=[("R", ra)])
                    rg = rbankA()
                    for k in range(8):
                        yield P.op("pe", lambda e, cc=cc, k=k, rg=rg: e.matmul(R_ps[:, rg, :], lhsT=wcg[:, k, 512 + cc * 128:512 + (cc + 1) * 128], rhs=uT[:, k, :],
                                                                       start=(k == 0), stop=(k == 7)), reads=["uT"] + WCG, writes=[("R", rg)])
                    s2 = cc % 2
                    yield P.op("act", lambda e, rg=rg, s2=s2, cc=cc: e.activation(out=sg[:, s2, :], in_=R_ps[:, rg, :], func=AF.Sigmoid, bias=bci[:, 4 + cc:5 + cc]),
                         reads=[("R", rg), ("b_conv_in", 4 + cc)], writes=[("sg", s2)])
                    if G == 0:
                        yield P.op("pool", lambda e, cc=cc: e.memset(glu[:, cc, 0:30], 0.0), writes=[("glu", cc)])
                    yield P.op("dve", lambda e, ra=ra, s2=s2, cc=cc: e.scalar_tensor_tensor(
                        out=glu[:, cc, 30:542], in0=R_ps[:, ra, :], scalar=bci[:, cc:cc + 1], in1=sg[:, s2, :], op0=ALU.add, op1=ALU.mult),
                        reads=[("R", ra), ("sg", s2), ("b_conv_in", cc), ("glu", cc)], writes=[("glu", cc)])

            def p2_front_b(gi):
                b = gi // GPS
                G = gi % GPS
                s0 = G * 512
                c0 = gi * 512
                def gate_front(oc):
                    rya = rbankA()
                    for a in range(4):
                        yield P.op("pe", lambda e, a=a, oc=oc, rya=rya: e.matmul(R_ps[:, rya, :], lhsT=wao[:, a, oc * 128:(oc + 1) * 128], rhs=attnT[:, a, :],
                                                                         start=(a == 0), stop=(a == 3)), reads=["attnT", ("wao", a)], writes=[("R", rya)])
                    rga = rbankA()
                    for k in range(8):
                        yield P.op("pe", lambda e, k=k, oc=oc, rga=rga: e.matmul(R_ps[:, rga, :], lhsT=wcg[:, k, 1024 + oc * 128:1024 + (oc + 1) * 128], rhs=uT[:, k, :],
                                                                         start=(k == 0), stop=(k == 7)), reads=["uT"] + WCG12, writes=[("R", rga)])
                    rgb = rbankA()
                    for k in range(8):
                        yield P.op("pe", lambda e, k=k, oc=oc, rgb=rgb: e.matmul(R_ps[:, rgb, :], lhsT=wcg[:, k, 2048 + oc * 128:2048 + (oc + 1) * 128], rhs=uT[:, k, :],
                                                                         start=(k == 0), stop=(k == 7)), reads=["uT"] + WCG12, writes=[("R", rgb)])
                    yield P.op("act", lambda e, oc=oc, rga=rga: e.activation(out=sga[:], in_=R_ps[:, rga, :], func=AF.Sigmoid, bias=bg[:, oc:oc + 1]),
                         reads=[("R", rga), ("b_gate", oc)], writes=["sga"])
                    yield P.op("act", lambda e, oc=oc, rgb=rgb: e.activation(out=sgbs[:, oc, :], in_=R_ps[:, rgb, :], func=AF.Sigmoid, bias=bg[:, 8 + oc:9 + oc]),
                         reads=[("R", rgb), ("b_gate", 8 + oc)], writes=[("sgbs", oc)])
                    yield P.op("dve", lambda e, rya=rya, oc=oc: e.tensor_tensor(out=t1s[:, oc, :], in0=R_ps[:, rya, :], in1=sga[:], op=ALU.mult),
                         reads=[("R", rya), "sga"], writes=[("t1s", oc)])

                nfront = 0
                for cc in range(4):
                    rcv = rbankA()
                    for k0 in range(0, 31, 8):
                        nt_ = min(8, 31 - k0)
                        hs = dgc[0] % 2
                        dgc[0] += 1
                        ia = cst[:, 0, :]
                        id_b = bass.AP(ia.tensor, ia.offset, [list(ia.ap[0]), [0, nt_], [1, 128]])
                        wa = cw[:, cc, k0:k0 + nt_]
                        w_b = bass.AP(wa.tensor, wa.offset, [list(wa.ap[0]), [1, nt_], [0, 128]])
                        yield P.op("dve", lambda e, hs=hs, nt_=nt_, id_b=id_b, w_b=w_b: e.tensor_tensor(
                            out=dgr[:, hs * 8:hs * 8 + nt_, :], in0=id_b, in1=w_b, op=ALU.mult),
                            reads=["cst", ("cw", cc)], writes=[("dgr", hs)])
                        for i_ in range(nt_):
                            kk = k0 + i_
                            yield P.op("pe", lambda e, ds_=hs * 8 + i_, cc=cc, kk=kk, rcv=rcv: e.matmul(R_ps[:, rcv, :], lhsT=dgr[:, ds_, :], rhs=glu[:, cc, kk:kk + 512],
                                                                                       start=(kk == 0), stop=(kk == 30)),
                                 reads=[("dgr", hs), ("glu", cc)], writes=[("R", rcv)])
                    yield P.op("dve", lambda e, cc=cc, rcv=rcv: e.tensor_scalar(out=cacc[:, cc, :], in0=R_ps[:, rcv, :], scalar1=cvb[:, cc:cc + 1], scalar2=None, op0=ALU.add),
                         reads=[("R", rcv), ("conv_b", cc)], writes=[("cacc", cc)])
                    for _ in range(2):
                        yield from gate_front(nfront)
                        nfront += 1
                if gi + 1 < NG:
                    c1 = (gi + 1) * 512
                    yield P.dma("sp", lambda e, c1=c1: e.dma_start(out=uT[:], in_=uT_d.rearrange("(k p) t -> p k t", p=128)[:, :, c1:c1 + 512]),
                          writes=["uT"])
                    yield P.dma("sp", lambda e, c1=c1: e.dma_start(out=attnT[:], in_=attnT_d.rearrange("(a p) t -> p a t", p=128)[:, :, c1:c1 + 512]),
                          writes=["attnT"])
                for cc in range(4):
                    yield P.op("pool", lambda e, cc=cc: e.tensor_copy(out=glu[:, cc, 0:30], in_=glu[:, cc, 512:542]),
                         reads=[("glu", cc)], writes=[("glu", cc)])

            def p2_mid(gi):
                b = gi // GPS
                G = gi % GPS
                s0 = G * 512
                c0 = gi * 512
                rm = rbankB()
                rv = rbankB()
                for cc in range(4):
                    yield P.op("pe", lambda e, cc=cc, rm=rm: e.matmul(R_ps[:, rm, :], lhsT=onesC[:], rhs=cacc[:, cc, :], start=(cc == 0), stop=(cc == 3)),
                         reads=[("cacc", cc), "onesC"], writes=[("R", rm)])
                for cc in range(4):
                    s2 = cc % 2
                    yield P.op("act", lambda e, cc=cc, s2=s2: e.activation(out=sqf[:, s2, :], in_=cacc[:, cc, :], func=AF.Square),
                         reads=[("cacc", cc)], writes=[("sqf", s2)])
                    yield P.op("pe", lambda e, cc=cc, rv=rv, s2=s2: e.matmul(R_ps[:, rv, :], lhsT=onesC[:], rhs=sqf[:, s2, :], start=(cc == 0), stop=(cc == 3)),
                         reads=[("sqf", s2), "onesC"], writes=[("R", rv)])
                yield P.op("act", lambda e, rm=rm: e.activation(out=mean_sb[:], in_=R_ps[:, rm, :], func=AF.Copy), reads=[("R", rm)], writes=["mean_sb"])
                yield P.op("dve", lambda e: e.tensor_tensor(out=t2[:], in0=mean_sb[:], in1=mean_sb[:], op=ALU.mult), reads=["mean_sb"], writes=["t2"])
                yield P.op("dve", lambda e, rv=rv: e.tensor_tensor(out=t2[:], in0=R_ps[:, rv, :], in1=t2[:], op=ALU.subtract),
                     reads=[("R", rv), "t2"], writes=["t2"])
                yield P.op("act", lambda e: e.activation(out=t2[:], in_=t2[:], func=AF.Ln, bias=EPS), reads=["t2"], writes=["t2"])
                yield P.op("act", lambda e: e.activation(out=sga[:], in_=t2[:], func=AF.Exp, scale=-0.5), reads=["t2"], writes=["sga"])
                for cc in range(4):
                    s2 = cc % 2
                    yield P.op("dve", lambda e, cc=cc, s2=s2: e.tensor_tensor(out=tmp2[:, s2, :], in0=cacc[:, cc, :], in1=mean_sb[:], op=ALU.subtract),
                         reads=[("cacc", cc), "mean_sb"], writes=[("sqf", s2)])
                    yield P.op("dve", lambda e, s2=s2: e.tensor_tensor(out=tmp2[:, s2, :], in0=tmp2[:, s2, :], in1=sga[:], op=ALU.mult),
                         reads=[("sqf", s2), "sga"], writes=[("sqf", s2)])
                    yield P.op("act", lambda e, cc=cc, s2=s2: e.activation(out=cT[:, cc, :], in_=tmp2[:, s2, :], func=AF.Silu,
                                                                     scale=lng[:, cc:cc + 1], bias=lnbb[:, cc:cc + 1]),
                         reads=[("sqf", s2), ("conv_ln_g", cc), ("conv_ln_b", cc)], writes=[("cT", cc)])
                for oc in range(8):
                    ryb = rbankB()
                    for a in range(4):
                        yield P.op("pe", lambda e, a=a, oc=oc, ryb=ryb: e.matmul(R_ps[:, ryb, :], lhsT=wco[:, a, oc * 128:(oc + 1) * 128], rhs=cT[:, a, :],
                                                                         start=(a == 0), stop=(a == 3)), reads=[("cT", a), ("wco", a)], writes=[("R", ryb)])
                    yield P.op("dve", lambda e, ryb=ryb, oc=oc: e.scalar_tensor_tensor(out=t2[:], in0=R_ps[:, ryb, :], scalar=bco[:, oc:oc + 1], in1=sgbs[:, oc, :],
                                                                                op0=ALU.add, op1=ALU.mult),
                         reads=[("R", ryb), ("sgbs", oc), ("b_conv_out", oc)], writes=["t2"])
                    yield P.op("dve", lambda e, oc=oc: e.tensor_tensor(out=mT[:, oc, :], in0=t1s[:, oc, :], in1=t2[:], op=ALU.add),
                         reads=[("t1s", oc), "t2"], writes=[("mT", oc)])
                MT = [("mT", oc) for oc in range(8)]

            def p2_tail(gi):
                b = gi // GPS
                G = gi % GPS
                s0 = G * 512
                c0 = gi * 512
                MT = [("mT", oc) for oc in range(8)]
                for j in range(4):
                    tile_i = gi * 4 + j
                    for n in range(2):
                        rh = rbankB()
                        for oc in range(8):
                            yield P.op("pe", lambda e, j=j, n=n, oc=oc, rh=rh: e.matmul(R_ps[:, rh, :], lhsT=mT[:, oc, j * 128:(j + 1) * 128], rhs=wo[:, oc, n * 512:(n + 1) * 512],
                                                                                 start=(oc == 0), stop=(oc == 7)), reads=MT + [("wo", oc)], writes=[("R", rh)])
                        yield P.op("dve", lambda e, j=j, n=n, rh=rh: e.tensor_tensor(out=h1t[:, j % 2, n * 512:(n + 1) * 512], in0=R_ps[:, rh, :], in1=xt[:, j, n * 512:(n + 1) * 512], op=ALU.add),
                             reads=[("R", rh), ("xt", j)], writes=[("h1t", j % 2, n)])
                    yield P.dma("sp", lambda e, r0=tile_i * 128, j=j: e.dma_start(out=h1_d[r0:r0 + 128, :], in_=h1t[:, j % 2, :]), reads=[("h1t", j % 2, 0), ("h1t", j % 2, 1)])
                    ssq, kssq = stat()
                    yield P.op("act", lambda e, ssq=ssq, j=j: e.activation(out=tb[:, j, :], in_=h1t[:, j % 2, :], func=AF.Square, accum_out=ssq),
                         reads=[("h1t", j % 2, 0), ("h1t", j % 2, 1)], writes=[("tb", j), kssq])
                    lnv, klnv = stat()
                    yield P.op("act", lambda e, ssq=ssq, lnv=lnv: e.activation(out=lnv, in_=ssq, func=AF.Ln, scale=1.0 / 1024, bias=EPS), reads=[kssq], writes=[klnv])
                    rstd, krstd = stat()
                    yield P.op("act", lambda e, lnv=lnv, rstd=rstd: e.activation(out=rstd, in_=lnv, func=AF.Exp, scale=-0.5), reads=[klnv], writes=[krstd])
                    yield P.op("dve", lambda e, rstd=rstd, j=j: e.scalar_tensor_tensor(out=tn[:], in0=h1t[:, j % 2, :], scalar=rstd, in1=gffn[:], op0=ALU.mult, op1=ALU.mult),
                         reads=[("h1t", j % 2, 0), ("h1t", j % 2, 1), krstd, "gffn"], writes=["tn"])
                    yield P.op("act", lambda e, j=j: e.activation(out=tb[:, j, :], in_=tn[:], func=AF.Copy), reads=["tn"], writes=[("tb", j)])
                    for k in range(8):
                        yield P.op("pe", lambda e, k=k: e.transpose(out=TF_ps[:, k * 128:(k + 1) * 128], in_=tn[:, k * 128:(k + 1) * 128], identity=cst_f[:, 0, :]),
                             reads=["tn", "cst_f"], writes=["TF_ps"])
                    yield P.op("act", lambda e: e.activation(out=tT[:], in_=TF_ps[:].rearrange("p (k t) -> p k t", k=8), func=AF.Copy), reads=["TF_ps"], writes=["tT"])
                    rl = rbankB()
                    for k in range(8):
                        yield P.op("pe", lambda e, k=k, rl=rl: e.matmul(R_ps[:, rl, 0:36], lhsT=tT[:, k, :], rhs=wrt[:, k, :], start=(k == 0), stop=(k == 7)),
                             reads=["tT", "wrt"], writes=[("R", rl)])
                    yield P.op("dve", lambda e, rl=rl, j=j: e.tensor_tensor(out=L4[:, j, :], in0=R_ps[:, rl, 0:36], in1=brt[:], op=ALU.add), reads=[("R", rl), "brt"], writes=[("L4", j)])

            def p2_route(gi):
                b = gi // GPS
                G = gi % GPS
                s0 = G * 512
                c0 = gi * 512
                KL = [("L4", j) for j in range(4)]
                Lg4 = L4[:, :, 0:4]
                Lx = L4[:, :, 4:36].rearrange("p t (g j) -> p t g j", g=4)

                def bc_last(ap2, n):
                    return bass.AP(ap2.tensor, ap2.offset, [list(ap2.ap[0]), list(ap2.ap[1]), [0, n]])

                def col(buf3, c):
                    a = buf3[:, :, c:c + 1]
                    return bass.AP(a.tensor, a.offset, [list(a.ap[0]), list(a.ap[1])])
                P.op("dve", lambda e: e.tensor_reduce(out=gmax4[:], in_=Lg4, axis=AX.X, op=ALU.max), reads=KL, writes=["gmax4"])
                P.op("dve", lambda e: e.tensor_tensor(out=ohg4[:], in0=Lg4, in1=bc_last(gmax4[:], 4), op=ALU.is_equal), reads=KL + ["gmax4"], writes=["ohg4"])
                P.op("dve", lambda e: e.tensor_tensor(out=ge4[:], in0=Lg4, in1=bc_last(gmax4[:], 4), op=ALU.subtract), reads=KL + ["gmax4"], writes=["ge4"])
                P.op("act", lambda e: e.activation(out=ge4[:], in_=ge4[:], func=AF.Exp), reads=["ge4"], writes=["ge4"])
                P.op("dve", lambda e: e.tensor_reduce(out=gsum4[:], in_=ge4[:], axis=AX.X, op=ALU.add), reads=["ge4"], writes=["gsum4"])
                P.op("dve", lambda e: e.reciprocal(out=gsum4[:], in_=gsum4[:]), reads=["gsum4"], writes=["gsum4"])
                ohg_bj = bass.AP(ohg4[:].tensor, ohg4[:].offset, [list(ohg4[:].ap[0]), [4, 4], [1, 4], [0, 8]])
                P.op("dve", lambda e: e.tensor_tensor(out=prod4[:], in0=Lx, in1=ohg_bj, op=ALU.mult), reads=KL + ["ohg4"], writes=["prod4"])
                P.op("dve", lambda e: e.tensor_reduce(out=els4[:], in_=prod4[:].rearrange("p t g j -> p t j g"), axis=AX.X, op=ALU.add), reads=["prod4"], writes=["els4"])
                for t in range(4):
                    P.op("dve", lambda e, t=t: e.max(out=top84[:, t, :], in_=els4[:, t, :]), reads=["els4"], writes=[("top84", t)])
                T8 = [("top84", t) for t in range(4)]
                for i in range(2):
                    P.op("dve", lambda e, i=i: e.tensor_tensor(out=m124[:, i, :, :], in0=els4[:], in1=bc_last(col(top84, i), 8), op=ALU.is_equal),
                         reads=["els4"] + T8, writes=[("m124", i)])
                P.op("dve", lambda e: e.tensor_tensor(out=d214[:], in0=col(top84, 1), in1=col(top84, 0), op=ALU.subtract), reads=T8, writes=["d214"])
                P.op("act", lambda e: e.activation(out=d214[:], in_=d214[:], func=AF.Exp), reads=["d214"], writes=["d214"])
                P.op("dve", lambda e: e.tensor_scalar(out=den4[:], in0=d214[:], scalar1=1.0, scalar2=None, op0=ALU.add), reads=["d214"], writes=["den4"])
                P.op("dve", lambda e: e.reciprocal(out=den4[:], in_=den4[:]), reads=["den4"], writes=["den4"])
                P.op("dve", lambda e: e.tensor_tensor(out=col(rinfo4, 2), in0=gsum4[:], in1=den4[:], op=ALU.mult), reads=["gsum4", "den4", "rinfo4"], writes=["rinfo4"])
                P.op("dve", lambda e: e.tensor_tensor(out=col(rinfo4, 3), in0=col(rinfo4, 2), in1=d214[:], op=ALU.mult), reads=["rinfo4", "d214"], writes=["rinfo4"])
                for i in range(2):
                    mi = m124[:, i, :, :]
                    m_bg = bass.AP(mi.tensor, mi.offset, [list(mi.ap[0]), [8, 4], [0, 4], [1, 8]])
                    P.op("dve", lambda e, i=i, m_bg=m_bg: e.tensor_tensor(out=s124[:, i, :, :].rearrange("p t (g j) -> p t g j", g=4), in0=m_bg, in1=ohg_bj, op=ALU.mult),
                         reads=[("m124", i), "ohg4"], writes=[("s124", i)])
                P.op("dve", lambda e: e.tensor_tensor(out=sel4[:], in0=s124[:, 0, :, :], in1=s124[:, 1, :, :], op=ALU.add), reads=[("s124", 0), ("s124", 1)], writes=["sel4"])
                rc = rbankB()
                for t in range(4):
                    P.op("pe", lambda e, rc=rc, t=t: e.matmul(R_ps[:, rc, t * 32:(t + 1) * 32], lhsT=cst[:, 2, :], rhs=sel4[:, t, :], start=True, stop=(t == 0), skip_group_check=True),
                         reads=["sel4", "cst"], writes=[("R", rc)])
                    for tq in range(t):
                        P.op("pe", lambda e, rc=rc, t=t, tq=tq: e.matmul(R_ps[:, rc, t * 32:(t + 1) * 32], lhsT=ones_bf[:], rhs=sel4[:, tq, :], start=False, stop=(tq == t - 1), skip_group_check=True),
                             reads=["sel4", "ones_bf"], writes=[("R", rc)])
                rt_ = rbankB()
                for t in range(4):
                    P.op("pe", lambda e, rt_=rt_, t=t: e.matmul(R_ps[:, rt_, 0:32], lhsT=ones_bf[:], rhs=sel4[:, t, :], start=(t == 0), stop=(t == 3)),
                         reads=["sel4", "ones_bf"], writes=[("R", rt_)])
                base_b = bass.AP(base_bc[:].tensor, base_bc[:].offset, [list(base_bc[:].ap[0]), [0, 4], [1, 32]])
                P.op("dve", lambda e, rc=rc: e.tensor_tensor(out=dest4[:], in0=R_ps[:, rc, 0:128].rearrange("p (t e) -> p t e", t=4), in1=base_b, op=ALU.add),
                     reads=[("R", rc), "base_bc"], writes=["dest4"])
                P.op("dve", lambda e, rt_=rt_: e.tensor_tensor(out=base_bc[:], in0=R_ps[:, rt_, 0:32], in1=base_bc[:], op=ALU.add), reads=[("R", rt_), "base_bc"], writes=["base_bc"])
                for i in range(2):
                    P.op("dve", lambda e, i=i: e.tensor_tensor(out=tmp324[:], in0=s124[:, i, :, :], in1=dest4[:], op=ALU.mult), reads=[("s124", i), "dest4"], writes=["tmp324"])
                    P.op("dve", lambda e, i=i: e.tensor_reduce(out=col(rinfo4, i), in_=tmp324[:], axis=AX.X, op=ALU.add), reads=["tmp324", "rinfo4"], writes=["rinfo4"])
                P.op("dve", lambda e: e.tensor_copy(out=idx4[:], in_=rinfo4[:, :, 0:2]), reads=["rinfo4"], writes=["idx4"])
                for t in range(4):
                    for i in range(2):
                        P.dma("pool", lambda e, t=t, i=i: e.indirect_dma_start(
                            out=xs_d[:, :], out_offset=bass.IndirectOffsetOnAxis(ap=idx4[:, t, i:i + 1], axis=0), in_=tb[:, t, :], in_offset=None),
                            reads=["idx4", ("tb", t)], writes=["xs_d"])
                P.dma("sp", lambda e, r0=gi * 512: e.dma_start(out=rt_d[r0:r0 + 512, :].rearrange("(t p) c -> p t c", p=128), in_=rinfo4[:]), reads=["rinfo4"])

            P.dma("sp", lambda e: e.dma_start(out=uT[:], in_=uT_d.rearrange("(k p) t -> p k t", p=128)[:, :, 0:512]), writes=["uT"])
            P.dma("sp", lambda e: e.dma_start(out=attnT[:], in_=attnT_d.rearrange("(a p) t -> p a t", p=128)[:, :, 0:512]), writes=["attnT"])
            zip_run2(p2_front_a(0), None)
            zip_run2(p2_front_b(0), None)
            for gi in range(NG):
                b = gi // GPS
                s0 = (gi % GPS) * 512
                for j in range(4):
                    P.dma("sp", lambda e, j=j, b=b, r0=s0 + j * 128: e.dma_start(out=xt[:, j, :], in_=x[b, r0:r0 + 128, :]),
                          writes=[("xt", j)])
                zip_run2(p2_front_a(gi + 1) if gi + 1 < NG else None, p2_mid(gi), 1)
                zip_run2(p2_tail(gi), p2_front_b(gi + 1) if gi + 1 < NG else None, 3)
                p2_route(gi)
            P.flush()


        if 3 in phases:
          with ExitStack() as st:
            def sb(n, s_, d):
                return st.enter_context(nc.sbuf_tensor(n, s_, d))

            def ps(n, s_, d):
                return st.enter_context(nc.psum_tensor(n, s_, d))

            cst_f = sb("cst_f3", [128, 4, 128], F32)
            cst = sb("cst3", [128, 4, 128], BF16)
            wb1 = sb("wb1", [128, 2, 8, 512], BF16)
            wb3 = sb("wb3", [128, 2, 8, 512], BF16)
            wb2 = sb("wb2", [128, 2, 4, 1024], BF16)
            stage = sb("stage3", [128, 4, 1024], F32)
            stage2 = sb("stage3b", [128, 4, 1024], F32)
            xsb = sb("xsb", [128, 2, NBLK, 1024], BF16)
            XT = sb("XT", [128, 2, 8, CAP], BF16)
            hT = sb("hT", [128, 4, CAP], BF16)
            sil = sb("sil", [128, 2, 512], F32)
            ysb = sb("ysb", [128, NBLK, 1024], BF16)
            R_ps = ps("R_ps3", [128, 6, 512], F32)
            T_ps = ps("T_ps3", [128, 2, 1024], BF16)
            rrot = [0]

            def rbank():
                i = rrot[0] % 6
                rrot[0] += 1
                return i

            P.dma("sp", lambda e: e.dma_start(out=cst_f[:], in_=consts[:, :, :]), writes=["cst_f"])
            P.op("dve", lambda e: e.tensor_copy(out=cst[:], in_=cst_f[:]), reads=["cst_f"], writes=["cst"])
            wl = [0]
            blkc = [0]

            def xs_dma(ex):
                eb = ex % 2
                P.dma("sp", lambda e, eb=eb, ex=ex: e.dma_start(
                    out=xsb[:, eb, :, :], in_=xs_d[ex * CAP:(ex + 1) * CAP, :].rearrange("(n p) c -> p n c", p=128)), writes=[("xsb", eb)])

            def load_w13(ex):
                eb = ex % 2
                for kk in range(4):
                    for (wsrc, wdst, nm) in ((w1, wb1, "wb1"), (w3, wb3, "wb3")):
                        sl = wl[0] % 4
                        wl[0] += 1
                        P.dma("sp", lambda e, sl=sl, wsrc=wsrc, ex=ex, kk=kk: e.dma_start(
                            out=stage[:, sl, :].rearrange("p (k f) -> p k f", k=2),
                            in_=wsrc[ex, kk * 256:(kk + 1) * 256, :].rearrange("(k p) f -> p k f", p=128)), writes=[("stage", sl)])
                        P.op("pool", lambda e, sl=sl, wdst=wdst, eb=eb, kk=kk: e.tensor_copy(
                            out=wdst[:, eb, 2 * kk:2 * kk + 2, :], in_=stage[:, sl, :].rearrange("p (k f) -> p k f", k=2)),
                            reads=[("stage", sl)], writes=[(nm, eb, kk)])

            def load_w2_dma(ex):
                for f in range(4):
                    P.dma("sp", lambda e, ex=ex, f=f: e.dma_start(out=stage2[:, f, :], in_=w2[ex, f * 128:(f + 1) * 128, :]), writes=[("stage2", f)])

            def cast_w2(ex, eng):
                eb = ex % 2
                for f in range(4):
                    if eng == "act":
                        P.op("act", lambda e, eb=eb, f=f: e.activation(out=wb2[:, eb, f, :], in_=stage2[:, f, :], func=AF.Copy),
                             reads=[("stage2", f)], writes=[("wb2", eb, f)])
                    else:
                        P.op("pool", lambda e, eb=eb, f=f: e.tensor_copy(out=wb2[:, eb, f, :], in_=stage2[:, f, :]),
                             reads=[("stage2", f)], writes=[("wb2", eb, f)])

            def zip_run3(ga, gb, ratio):
                a_done = ga is None
                b_done = gb is None
                while not (a_done and b_done):
                    if not a_done:
                        try:
                            next(ga)
                        except StopIteration:
                            a_done = True
                    for _ in range(ratio):
                        if not b_done:
                            try:
                                next(gb)
                            except StopIteration:
                                b_done = True

            def gather_T(ex):
                eb = ex % 2
                for blk in range(NBLK):
                    bs = blkc[0] % 2
                    blkc[0] += 1
                    for k in range(8):
                        yield P.op("pe", lambda e, bs=bs, k=k, blk=blk, eb=eb: e.transpose(out=T_ps[:, bs, k * 128:(k + 1) * 128], in_=xsb[:, eb, blk, k * 128:(k + 1) * 128], identity=cst[:, 0, :]),
                                   reads=[("xsb", eb), "cst"], writes=[("T_ps", bs)])
                    if blk % 2 == 0:
                        yield P.op("dve", lambda e, bs=bs, blk=blk, eb=eb: e.tensor_copy(out=XT[:, eb, :, blk * 128:(blk + 1) * 128], in_=T_ps[:, bs, :].rearrange("p (k t) -> p k t", k=8)),
                                   reads=[("T_ps", bs)], writes=[("XT", eb, blk)])
                    else:
                        yield P.op("act", lambda e, bs=bs, blk=blk, eb=eb: e.activation(out=XT[:, eb, :, blk * 128:(blk + 1) * 128], in_=T_ps[:, bs, :].rearrange("p (k t) -> p k t", k=8), func=AF.Copy),
                                   reads=[("T_ps", bs)], writes=[("XT", eb, blk)])

            def expert_compute(ex):
                eb = ex % 2
                W1 = [("wb1", eb, kk) for kk in range(4)]
                W3 = [("wb3", eb, kk) for kk in range(4)]
                W2 = [("wb2", eb, f) for f in range(4)]
                for nt in range(CAP // 512):
                    XTK = [("XT", eb, nt * 4 + i) for i in range(4)]
                    for f in range(4):
                        r1 = rbank()
                        for k in range(8):
                            yield P.op("pe", lambda e, f=f, k=k, nt=nt, r1=r1, eb=eb: e.matmul(R_ps[:, r1, :], lhsT=wb1[:, eb, k, f * 128:(f + 1) * 128], rhs=XT[:, eb, k, nt * 512:(nt + 1) * 512],
                                                                                        start=(k == 0), stop=(k == 7)), reads=W1 + XTK, writes=[("R", r1)])
                        r3 = rbank()
                        for k in range(8):
                            yield P.op("pe", lambda e, f=f, k=k, nt=nt, r3=r3, eb=eb: e.matmul(R_ps[:, r3, :], lhsT=wb3[:, eb, k, f * 128:(f + 1) * 128], rhs=XT[:, eb, k, nt * 512:(nt + 1) * 512],
                                                                                        start=(k == 0), stop=(k == 7)), reads=W3 + XTK, writes=[("R", r3)])
                        ss = f % 2
                        yield P.op("act", lambda e, r1=r1, ss=ss: e.activation(out=sil[:, ss, :], in_=R_ps[:, r1, :], func=AF.Silu), reads=[("R", r1)], writes=[("sil", ss)])
                        yield P.op("dve", lambda e, r3=r3, ss=ss, f=f, nt=nt: e.tensor_tensor(out=hT[:, f, nt * 512:(nt + 1) * 512], in0=R_ps[:, r3, :], in1=sil[:, ss, :], op=ALU.mult),
                                   reads=[("R", r3), ("sil", ss)], writes=[("hT", f, nt)])
                if ex + 1 < NE:
                    cast_w2(ex + 1, "act")
                for blk in range(NBLK):
                    for n in range(2):
                        ry = rbank()
                        for f in range(4):
                            yield P.op("pe", lambda e, f=f, n=n, blk=blk, ry=ry, eb=eb: e.matmul(R_ps[:, ry, :], lhsT=hT[:, f, blk * 128:(blk + 1) * 128], rhs=wb2[:, eb, f, n * 512:(n + 1) * 512],
                                                                                          start=(f == 0), stop=(f == 3)), reads=W2 + [("hT", f, blk // 4)], writes=[("R", ry)])
                        if n == 0:
                            yield P.op("act", lambda e, ry=ry, blk=blk, n=n: e.activation(out=ysb[:, blk, n * 512:(n + 1) * 512], in_=R_ps[:, ry, :], func=AF.Copy),
                                       reads=[("R", ry)], writes=[("ysb", blk, n)])
                        else:
                            yield P.op("dve", lambda e, ry=ry, blk=blk, n=n: e.tensor_copy(out=ysb[:, blk, n * 512:(n + 1) * 512], in_=R_ps[:, ry, :]),
                                       reads=[("R", ry)], writes=[("ysb", blk, n)])
                yield P.dma("sp", lambda e, ex=ex: e.dma_start(out=ys_d[ex * CAP:(ex + 1) * CAP, :].rearrange("(n p) c -> p n c", p=128), in_=ysb[:]),
                            reads=[("ysb", blk, n) for blk in range(NBLK) for n in range(2)])

            xs_dma(0)
            load_w13(0)
            load_w2_dma(0)
            cast_w2(0, "pool")
            xs_dma(1)
            zip_run3(gather_T(0), None, 1)
            for ex in range(NE):
                if ex + 1 < NE:
                    load_w13(ex + 1)
                    load_w2_dma(ex + 1)
                if ex + 2 < NE:
                    xs_dma(ex + 2)
                zip_run3(gather_T(ex + 1) if ex + 1 < NE else None, expert_compute(ex), 4)
            P.flush()

        if 4 in phases:
          with ExitStack() as st:
            def sb(n, s_, d):
                return st.enter_context(nc.sbuf_tensor(n, s_, d))

            def ps(n, s_, d):
                return st.enter_context(nc.psum_tensor(n, s_, d))

            cst_f = sb("cst_f4", [128, 4, 128], F32)
            cst = sb("cst4", [128, 4, 128], BF16)
            wpg = sb("wpg", [128, 8, 1024], BF16)
            wpp = sb("wpp", [128, 2, 1024], BF16)
            stage = sb("stage4", [128, 2, 1024], F32)
            gple = sb("gple", [128, 1024], F32)
            bple = sb("bple", [128, 1024], F32)
            h1t = sb("h1t4", [128, 3, 1024], F32)
            yA = sb("yA", [128, 3, 1024], BF16)
            yB = sb("yB", [128, 3, 1024], BF16)
            h2 = sb("h2", [128, 2, 1024], F32)
            hn = sb("hn", [128, 2, 1024], BF16)
            hnT = sb("hnT", [128, 2, 8, 128], BF16)
            pt = sb("pt", [128, 3, 256], F32)
            pb = sb("pb", [128, 2, 256], BF16)
            pT = sb("pT4", [128, 2, 2, 128], BF16)
            gl = sb("gl", [128, 2, 1024], F32)
            ot = sb("ot", [128, 2, 1024], F32)
            rinfo = sb("rinfo4", [128, NTILE, 4], F32)
            idx = sb("idx4", [128, NTILE, 2], I32)
            junk = sb("junk4", [128, 1024], BF16)
            rs_ = sb("rs4", [128, 16], F32)
            R_ps = ps("R_ps4", [128, 5, 512], F32)
            T_ps = ps("T_ps4", [128, 3, 1024], BF16)
            rrot = [0]

            def rbank():
                i = rrot[0] % 5
                rrot[0] += 1
                return i
            stc = [0]

            def stat():
                i = stc[0] % 16
                stc[0] += 1
                return rs_[:, i:i + 1], ("rs_", i)

            P.dma("sp", lambda e: e.dma_start(out=cst_f[:], in_=consts[:, :, :]), writes=["cst_f"])
            P.op("dve", lambda e: e.tensor_copy(out=cst[:], in_=cst_f[:]), reads=["cst_f"], writes=["cst"])
            P.dma("sp", lambda e: e.dma_start(out=gple[:], in_=bcast_rows(vec["norm_ple"], 128, 1024)), writes=["gple"])
            P.dma("sp", lambda e: e.dma_start(out=bple[:], in_=bcast_rows(vec["b_ple_gate"], 128, 1024)), writes=["bple"])
            wl = [0]
            for k in range(8):
                sl = wl[0] % 2
                wl[0] += 1
                P.dma("sp", lambda e, sl=sl, k=k: e.dma_start(out=stage[:, sl, :], in_=w_pg[k * 128:(k + 1) * 128, :]), writes=[("stage", sl)])
                P.op("pool", lambda e, sl=sl, k=k: e.tensor_copy(out=wpg[:, k, :], in_=stage[:, sl, :]), reads=[("stage", sl)], writes=[("wpg", k)])
            for k in range(2):
                sl = wl[0] % 2
                wl[0] += 1
                P.dma("sp", lambda e, sl=sl, k=k: e.dma_start(out=stage[:, sl, :], in_=w_pp[k * 128:(k + 1) * 128, :]), writes=[("stage", sl)])
                P.op("pool", lambda e, sl=sl, k=k: e.tensor_copy(out=wpp[:, k, :], in_=stage[:, sl, :]), reads=[("stage", sl)], writes=[("wpp", k)])
            WPG = [("wpg", k) for k in range(8)]
            WPP = [("wpp", k) for k in range(2)]
            def p4_loads(ti):
                b = ti // TPS
                r0 = (ti % TPS) * 128
                s = ti % 3
                P.dma("pool", lambda e, s=s, ti=ti: e.indirect_dma_start(out=yA[:, s, :], out_offset=None, in_=ys_d[:, :],
                                                                 in_offset=bass.IndirectOffsetOnAxis(ap=idx[:, ti, 0:1], axis=0)), reads=["idx"], writes=[("yA", s)])
                P.dma("pool", lambda e, s=s, ti=ti: e.indirect_dma_start(out=yB[:, s, :], out_offset=None, in_=ys_d[:, :],
                                                                 in_offset=bass.IndirectOffsetOnAxis(ap=idx[:, ti, 1:2], axis=0)), reads=["idx"], writes=[("yB", s)])
                P.dma("sp", lambda e, s=s, ti=ti: e.dma_start(out=h1t[:, s, :], in_=h1_d[ti * 128:(ti + 1) * 128, :]), writes=[("h1t", s)])
                P.dma("sp", lambda e, s=s, b=b, r0=r0: e.dma_start(out=pt[:, s, :], in_=pin[b, r0:r0 + 128, :]), writes=[("pt", s)])

            P.dma("sp", lambda e: e.dma_start(out=rinfo[:], in_=rt_d.rearrange("(t p) c -> p t c", p=128)), writes=["rinfo"])
            P.op("pool", lambda e: e.tensor_copy(out=idx[:], in_=rinfo[:, :, 0:2]), reads=["rinfo"], writes=["idx"])
            p4_loads(0)
            p4_loads(1)
            def p4_front(ti):
                b = ti // TPS
                r0 = (ti % TPS) * 128
                s = ti % 2
                sl = ti % 3
                yield P.op("dve", lambda e, s=s, sl=sl, ti=ti: e.scalar_tensor_tensor(out=h2[:, s, :], in0=yA[:, sl, :], scalar=rinfo[:, ti, 2:3], in1=h1t[:, sl, :], op0=ALU.mult, op1=ALU.add),
                     reads=[("yA", sl), "rinfo", ("h1t", sl)], writes=[("h2", s)])
                yield P.op("dve", lambda e, s=s, sl=sl, ti=ti: e.scalar_tensor_tensor(out=h2[:, s, :], in0=yB[:, sl, :], scalar=rinfo[:, ti, 3:4], in1=h2[:, s, :], op0=ALU.mult, op1=ALU.add),
                     reads=[("yB", sl), "rinfo", ("h2", s)], writes=[("h2", s)])
                ssq, kssq = stat()
                yield P.op("act", lambda e, s=s, ssq=ssq: e.activation(out=junk[:], in_=h2[:, s, :], func=AF.Square, accum_out=ssq), reads=[("h2", s)], writes=["junk", kssq])
                lnv, klnv = stat()
                yield P.op("act", lambda e, ssq=ssq, lnv=lnv: e.activation(out=lnv, in_=ssq, func=AF.Ln, scale=1.0 / 1024, bias=EPS), reads=[kssq], writes=[klnv])
                rstd, krstd = stat()
                yield P.op("act", lambda e, lnv=lnv, rstd=rstd: e.activation(out=rstd, in_=lnv, func=AF.Exp, scale=-0.5), reads=[klnv], writes=[krstd])
                yield P.op("dve", lambda e, s=s, rstd=rstd: e.scalar_tensor_tensor(out=hn[:, s, :], in0=h2[:, s, :], scalar=rstd, in1=gple[:], op0=ALU.mult, op1=ALU.mult),
                     reads=[("h2", s), krstd, "gple"], writes=[("hn", s)])
                for k in range(8):
                    yield P.op("pe", lambda e, k=k, s=s: e.transpose(out=T_ps[:, s, k * 128:(k + 1) * 128], in_=hn[:, s, k * 128:(k + 1) * 128], identity=cst[:, 0, :]),
                         reads=[("hn", s), "cst"], writes=[("T_ps", s)])
                yield P.op("act", lambda e, s=s: e.activation(out=hnT[:, s, :, :], in_=T_ps[:, s, :].rearrange("p (k t) -> p k t", k=8), func=AF.Copy), reads=[("T_ps", s)], writes=[("hnT", s)])
                yield P.op("act", lambda e, s=s, sl=sl: e.activation(out=pb[:, s, :], in_=pt[:, sl, :], func=AF.Copy), reads=[("pt", sl)], writes=[("pb", s)])
                for k in range(2):
                    yield P.op("pe", lambda e, k=k, s=s: e.transpose(out=T_ps[:, 2, s * 256 + k * 128:s * 256 + (k + 1) * 128], in_=pb[:, s, k * 128:(k + 1) * 128], identity=cst[:, 0, :]),
                         reads=[("pb", s), "cst"], writes=[("T_psp", s)])
                yield P.op("act", lambda e, s=s: e.activation(out=pT[:, s, :, :], in_=T_ps[:, 2, s * 256:(s + 1) * 256].rearrange("p (k t) -> p k t", k=2), func=AF.Copy), reads=[("T_psp", s)], writes=[("pT", s)])

            def p4_back(ti):
                b = ti // TPS
                r0 = (ti % TPS) * 128
                s = ti % 2
                sl = ti % 3
                for n in range(2):
                    rg = rbank()
                    for k in range(8):
                        yield P.op("pe", lambda e, k=k, n=n, rg=rg, s=s: e.matmul(R_ps[:, rg, :], lhsT=hnT[:, s, k, :], rhs=wpg[:, k, n * 512:(n + 1) * 512], start=(k == 0), stop=(k == 7)),
                             reads=[("hnT", s)] + WPG, writes=[("R", rg)])
                    rp = rbank()
                    for k in range(2):
                        yield P.op("pe", lambda e, k=k, n=n, rp=rp, s=s: e.matmul(R_ps[:, rp, :], lhsT=pT[:, s, k, :], rhs=wpp[:, k, n * 512:(n + 1) * 512], start=(k == 0), stop=(k == 1)),
                             reads=[("pT", s)] + WPP, writes=[("R", rp)])
                    yield P.op("dve", lambda e, n=n, rg=rg, s=s: e.tensor_tensor(out=gl[:, s, n * 512:(n + 1) * 512], in0=R_ps[:, rg, :], in1=bple[:, n * 512:(n + 1) * 512], op=ALU.add),
                         reads=[("R", rg), "bple"], writes=[("gl", s, n)])
                    yield P.op("act", lambda e, n=n, s=s: e.activation(out=gl[:, s, n * 512:(n + 1) * 512], in_=gl[:, s, n * 512:(n + 1) * 512], func=AF.Sigmoid),
                         reads=[("gl", s, n)], writes=[("gl", s, n)])
                    yield P.op("dve", lambda e, n=n, rp=rp, s=s: e.tensor_tensor(out=gl[:, s, n * 512:(n + 1) * 512], in0=R_ps[:, rp, :], in1=gl[:, s, n * 512:(n + 1) * 512], op=ALU.mult),
                         reads=[("R", rp), ("gl", s, n)], writes=[("gl", s, n)])
                    yield P.op("dve", lambda e, n=n, s=s: e.tensor_tensor(out=ot[:, s, n * 512:(n + 1) * 512], in0=gl[:, s, n * 512:(n + 1) * 512], in1=h2[:, s, n * 512:(n + 1) * 512], op=ALU.add),
                         reads=[("gl", s, n), ("h2", s)], writes=[("ot", s, n)])
                yield P.dma("sp", lambda e, s=s, b=b, r0=r0: e.dma_start(out=out[b, r0:r0 + 128, :], in_=ot[:, s, :]), reads=[("ot", s, 0), ("ot", s, 1)])

            def zip_run(ga, gb, ratio=2):
                a_done = ga is None
                b_done = gb is None
                while not (a_done and b_done):
                    if not a_done:
                        try:
                            next(ga)
                        except StopIteration:
                            a_done = True
                    for _ in range(ratio):
                        if not b_done:
                            try:
                                next(gb)
                            except StopIteration:
                                b_done = True

            zip_run(p4_front(0), None)
            for ti in range(NTILE):
                if ti + 2 < NTILE:
                    p4_loads(ti + 2)
                zip_run(p4_front(ti + 1) if ti + 1 < NTILE else None, p4_back(ti))
            P.flush()
    return nc


def make_consts(CAP):
    k = np.arange(128)
    c = np.zeros((128, 4, 128), np.float32)
    c[:, 0, :] = np.eye(128, dtype=np.float32)
    c[:, 1, :] = (k[:, None] <= k[None, :]).astype(np.float32)
    c[:, 2, :] = (k[:, None] < k[None, :]).astype(np.float32)
    blk = (k[:, None] // 64 == k[None, :] // 64).astype(np.float32) / 64.0
    c[:, 3, :] = blk
    capoff = (np.arange(32, dtype=np.float32) * CAP).reshape(1, 32)
    return c, capoff


def core_inputs(inputs, core, NB, CAP):
    f = lambda a: np.ascontiguousarray(a, dtype=np.float32)
    L = 0
    consts, capoff = make_consts(CAP)
    m = {
        "x": f(inputs["x"][core * NB:(core + 1) * NB]),
        "p": f(inputs["p"][L, core * NB:(core + 1) * NB]),
        "w_in": f(inputs["w_in"][L]),
        "w_attn_out": f(inputs["w_attn_out"][L]),
        "w_conv_out": f(inputs["w_conv_out"][L]),
        "w_o": f(inputs["w_o"][L]),
        "w_rt": f(np.concatenate([inputs["w_router_group"][L], inputs["w_router_expert"][L]], axis=1)),
        "b_rt": f(np.concatenate([inputs["b_router_group"][L], inputs["b_router_expert"][L]])[None, :]),
        "w1": f(inputs["w1"][L]), "w3": f(inputs["w3"][L]), "w2": f(inputs["w2"][L]),
        "w_ple_gate": f(inputs["w_ple_gate"][L]), "w_ple_proj": f(inputs["w_ple_proj"][L]),
        "consts": consts, "capoff": capoff,
        "conv_w": f(inputs["conv_w"][L].T),
    }
    for n in ["norm_mix", "norm_ffn", "norm_ple", "b_ple_gate", "lambda_q1", "lambda_k1", "lambda_q2",
              "lambda_k2", "subln", "b_conv_in", "b_gate", "conv_b", "conv_ln_g", "conv_ln_b", "b_conv_out"]:
        m[n] = f(inputs[n][L]).reshape(1, -1)
    m["q_norm"] = f(inputs["q_norm"][L]).reshape(1, 128)
    m["k_norm"] = f(inputs["k_norm"][L]).reshape(1, 128)
    return m


_CACHE = {}


def kernel(**inputs):
    NB, CAP = 2, 1024
    if "nc" not in _CACHE:
        _CACHE["nc"] = build_program(S=4096, NB=NB, CAP=CAP)
    nc = _CACHE["nc"]
    in_maps = [core_inputs(inputs, c, NB, CAP) for c in range(8)]
    res = run_bass_kernel_spmd(nc, in_maps, core_ids=list(range(8)))
    return np.concatenate([np.asarray(r["out"]) for r in res.results], axis=0).astype(np.float32)
```
